# Optimizing a Trainium2 kernel written in Bass

```python
import math
import jax, jax.numpy as jnp
from jax import lax
import numpy as np

D_MODEL = 1024
BATCH = 32
SEQ = 2048
DEPTH = 2

GRID_W = 64
HEAD_DIM = 64
N_Q_HEADS = 8
N_KV_HEADS = 2
GROUP = N_Q_HEADS // N_KV_HEADS
ATTN_WIDTH = N_Q_HEADS * HEAD_DIM
KV_WIDTH = N_KV_HEADS * HEAD_DIM
CONV_CH = D_MODEL - ATTN_WIDTH
MIX_WIDTH = ATTN_WIDTH + CONV_CH
IN_WIDTH = ATTN_WIDTH + 2 * KV_WIDTH + 2 * CONV_CH
Q_BLOCK = 128
ROPE_THETA = 10000.0
ROPE_HALF = HEAD_DIM // 2
ROPE_FREQS = ROPE_HALF // 2
CONV_WIDTH = 31
CONV_PAD = CONV_WIDTH // 2
N_EXPERTS = 32
TOP_K = 4
D_EXPERT = 1024
SWIGLU_LIMIT = 7.0
SWIGLU_ALPHA = 1.702
MOE_BLOCK = 512
EPS = 1e-6

kernel_name = "hymba_style_attn_conformer_moe_encoder"


def rmsnorm(x, g):
    xf = x.astype(jnp.float32)
    y = xf * lax.rsqrt(jnp.mean(xf * xf, axis=-1, keepdims=True) + EPS)
    return (y * g.astype(jnp.float32)).astype(x.dtype)


def layernorm(x, g, b):
    xf = x.astype(jnp.float32)
    mu = jnp.mean(xf, axis=-1, keepdims=True)
    var = jnp.mean(jnp.square(xf - mu), axis=-1, keepdims=True)
    y = (xf - mu) * lax.rsqrt(var + EPS)
    return (y * g.astype(jnp.float32) + b.astype(jnp.float32)).astype(x.dtype)


def axial_rope_tables(seq_len):
    rows = seq_len // GRID_W
    freqs = ROPE_THETA ** (-jnp.arange(ROPE_FREQS, dtype=jnp.float32) / ROPE_FREQS)
    row_ang = jnp.arange(rows, dtype=jnp.float32)[:, None] * freqs
    col_ang = jnp.arange(GRID_W, dtype=jnp.float32)[:, None] * freqs
    row_full = jnp.broadcast_to(row_ang[:, None, :], (rows, GRID_W, ROPE_FREQS)).reshape(seq_len, ROPE_FREQS)
    col_full = jnp.broadcast_to(col_ang[None, :, :], (rows, GRID_W, ROPE_FREQS)).reshape(seq_len, ROPE_FREQS)
    return jnp.cos(row_full), jnp.sin(row_full), jnp.cos(col_full), jnp.sin(col_full)


def _rotate(x, cos, sin):
    x1, x2 = x[..., :ROPE_FREQS], x[..., ROPE_FREQS:]
    return jnp.concatenate([x1 * cos - x2 * sin, x2 * cos + x1 * sin], axis=-1)


def apply_axial_rope(x, tables):
    cr, sr, cc, sc = [t.astype(x.dtype)[None, :, None, :] for t in tables]
    return jnp.concatenate([_rotate(x[..., :ROPE_HALF], cr, sr),
                            _rotate(x[..., ROPE_HALF:], cc, sc)], axis=-1)


def blocked_gqa(q, k, v):
    B, S, _, Dh = q.shape
    nblk = S // Q_BLOCK
    scale = Dh ** -0.5
    qb = q.reshape(B, nblk, Q_BLOCK, N_KV_HEADS, GROUP, Dh).transpose(1, 0, 2, 3, 4, 5)

    def one_block(qblk):
        s = jnp.einsum('bqkgd,bskd->bkgqs', qblk, k).astype(jnp.float32) * scale
        p = jax.nn.softmax(s, axis=-1).astype(v.dtype)
        return jnp.einsum('bkgqs,bskd->bqkgd', p, v)

    o = lax.map(one_block, qb)
    return o.transpose(1, 0, 2, 3, 4, 5).reshape(B, S, N_Q_HEADS * Dh)


def conformer_conv(a, gate, w_dw, b_dw, g_cn, b_cn):
    u = a * jax.nn.sigmoid(gate)
    u = lax.conv_general_dilated(u, w_dw[:, None, :].astype(u.dtype), window_strides=(1,),
                                 padding=[(CONV_PAD, CONV_PAD)],
                                 dimension_numbers=('NWC', 'WIO', 'NWC'),
                                 feature_group_count=CONV_CH) + b_dw
    return jax.nn.silu(layernorm(u, g_cn, b_cn))


def moe_ffn(h, w_router, b_router, w1, b1, w2, b2):
    B, S, D = h.shape
    N = B * S
    NK = N * TOP_K
    hf = h.reshape(N, D)
    logits = (hf @ w_router).astype(jnp.float32) + b_router.astype(jnp.float32)
    top_vals, top_idx = lax.top_k(logits, TOP_K)
    gates = jax.nn.softmax(top_vals, axis=-1)
    flat_e = top_idx.reshape(NK)
    flat_t = jnp.repeat(jnp.arange(N, dtype=jnp.int32), TOP_K)
    flat_g = gates.reshape(NK)
    order = jnp.argsort(flat_e)
    se, st, sg = flat_e[order], flat_t[order], flat_g[order]
    counts = jnp.bincount(flat_e, length=N_EXPERTS)
    starts = jnp.cumsum(counts) - counts
    padded = (counts + MOE_BLOCK - 1) // MOE_BLOCK * MOE_BLOCK
    pstarts = jnp.cumsum(padded) - padded
    pends = pstarts + padded
    dest = pstarts[se] + (jnp.arange(NK, dtype=jnp.int32) - starts[se])
    P = (NK + MOE_BLOCK - 1) // MOE_BLOCK * MOE_BLOCK + N_EXPERTS * MOE_BLOCK
    n_blocks = P // MOE_BLOCK
    pad_t = jnp.zeros((P,), jnp.int32).at[dest].set(st)
    pad_g = jnp.zeros((P,), jnp.float32).at[dest].set(sg)
    block_start = jnp.arange(n_blocks, dtype=jnp.int32) * MOE_BLOCK
    block_e = jnp.minimum(jnp.searchsorted(pends, block_start, side='right'), N_EXPERTS - 1)

    def step(acc, blk):
        tok, g, e = blk
        xb = hf[tok]
        hu = xb @ w1[e] + b1[e]
        x_glu = jnp.minimum(hu[:, :D_EXPERT], SWIGLU_LIMIT)
        x_lin = jnp.clip(hu[:, D_EXPERT:], -SWIGLU_LIMIT, SWIGLU_LIMIT)
        act = (x_lin + 1.0) * (x_glu * jax.nn.sigmoid(SWIGLU_ALPHA * x_glu))
        y = act @ w2[e] + b2[e]
        return acc.at[tok].add(y * g[:, None].astype(y.dtype)), None

    out, _ = lax.scan(step, jnp.zeros_like(hf),
                      (pad_t.reshape(n_blocks, MOE_BLOCK), pad_g.reshape(n_blocks, MOE_BLOCK), block_e))
    return out.reshape(B, S, D)


def setup_inputs(seed: int = 0) -> dict:
    key = jax.random.key(seed)
    ks = jax.random.split(key, 26)
    L, D = DEPTH, D_MODEL

    def nrm(k, shape, scale):
        return jax.random.normal(k, shape, jnp.float32) * scale

    return {
        "x": nrm(ks[0], (BATCH, SEQ, D), 1.0),
        "c": nrm(ks[1], (BATCH, D), 1.0),
        "w_mod": nrm(ks[2], (L, D, 6 * D), 0.5 * D ** -0.5),
        "b_mod": nrm(ks[3], (L, 6 * D), 0.01),
        "g_mix": 1.0 + nrm(ks[4], (L, D), 0.02),
        "w_in": nrm(ks[5], (L, D, IN_WIDTH), D ** -0.5),
        "g_q": 1.0 + nrm(ks[6], (L, HEAD_DIM), 0.02),
        "g_k": 1.0 + nrm(ks[7], (L, HEAD_DIM), 0.02),
        "w_dw": nrm(ks[8], (L, CONV_WIDTH, CONV_CH), CONV_WIDTH ** -0.5),
        "b_dw": nrm(ks[9], (L, CONV_CH), 0.01),
        "g_cn": 1.0 + nrm(ks[10], (L, CONV_CH), 0.02),
        "b_cn": nrm(ks[11], (L, CONV_CH), 0.01),
        "w_out": nrm(ks[12], (L, MIX_WIDTH, D), MIX_WIDTH ** -0.5),
        "g_ffn": 1.0 + nrm(ks[13], (L, D), 0.02),
        "w_router": nrm(ks[14], (L, D, N_EXPERTS), D ** -0.5),
        "b_router": nrm(ks[15], (L, N_EXPERTS), 0.01),
        "w1": nrm(ks[16], (L, N_EXPERTS, D, 2 * D_EXPERT), D ** -0.5),
        "b1": nrm(ks[17], (L, N_EXPERTS, 2 * D_EXPERT), 0.01),
        "w2": nrm(ks[18], (L, N_EXPERTS, D_EXPERT, D), D_EXPERT ** -0.5),
        "b2": nrm(ks[19], (L, N_EXPERTS, D), 0.01),
        "g_final": 1.0 + nrm(ks[20], (D,), 0.02),
    }


def reference(x, c, w_mod, b_mod, g_mix, w_in, g_q, g_k, w_dw, b_dw, g_cn, b_cn,
              w_out, g_ffn, w_router, b_router, w1, b1, w2, b2, g_final):
    B, S, D = x.shape
    rope = axial_rope_tables(S)
    c_act = jax.nn.silu(c)
    split_at = [ATTN_WIDTH, ATTN_WIDTH + KV_WIDTH, ATTN_WIDTH + 2 * KV_WIDTH,
                ATTN_WIDTH + 2 * KV_WIDTH + CONV_CH]
    for l in range(DEPTH):
        mod = c_act @ w_mod[l] + b_mod[l]
        sh1, sc1, gt1, sh2, sc2, gt2 = [m[:, None, :] for m in jnp.split(mod, 6, axis=-1)]

        h = rmsnorm(x, g_mix[l]) * (1.0 + sc1) + sh1
        proj = h @ w_in[l]
        q, k, v, ca, cg = jnp.split(proj, split_at, axis=-1)
        q = apply_axial_rope(rmsnorm(q.reshape(B, S, N_Q_HEADS, HEAD_DIM), g_q[l]), rope)
        k = apply_axial_rope(rmsnorm(k.reshape(B, S, N_KV_HEADS, HEAD_DIM), g_k[l]), rope)
        v = v.reshape(B, S, N_KV_HEADS, HEAD_DIM)
        attn = blocked_gqa(q, k, v)
        conv = conformer_conv(ca, cg, w_dw[l], b_dw[l], g_cn[l], b_cn[l])
        mix = jnp.concatenate([attn, conv], axis=-1) @ w_out[l]
        x = x + gt1 * mix

        h = rmsnorm(x, g_ffn[l]) * (1.0 + sc2) + sh2
        x = x + gt2 * moe_ffn(h, w_router[l], b_router[l], w1[l], b1[l], w2[l], b2[l])
    return rmsnorm(x, g_final)
```

```python
import contextlib
import numpy as np
import concourse.bass as bass
import concourse.mybir as mybir
from concourse.bass_utils import run_bass_kernel_spmd

F32 = mybir.dt.float32
BF16 = mybir.dt.bfloat16
AF = mybir.ActivationFunctionType
ALU = mybir.AluOpType

D = 1024
SEQ = 2048
NBLK = 4
NT = 16
NE = 32
EPS = 1e-6
WIN = 1920


class Sched:
    ENG = ("pe", "dve", "act", "pool", "sp")
    LIMIT = 30000

    def __init__(self, nc, stack):
        self.nc = nc
        self.stack = stack
        self.engs = {"pe": nc.tensor, "dve": nc.vector, "act": nc.scalar,
                     "pool": nc.gpsimd, "sp": nc.sync}
        self.tick_sem = {e: stack.enter_context(nc.semaphore("tk_" + e)) for e in self.ENG}
        self.tick = {e: 0 for e in self.ENG}
        self.tick_sid = {e: "tk_" + e for e in self.ENG}
        self.epoch = {}
        self.seen = {e: {} for e in self.ENG}
        self.sems = {}
        for e in self.ENG:
            self.sems["tk_" + e] = self.tick_sem[e]
        self.dma_sem = {}
        self.dma_gen = 0
        self.state = {}
        self.n_wait = 0
        self.n_ops = 0

    def _need(self, e, ev):
        sid, val = ev
        if self.seen[e].get(sid, 0) >= val:
            return
        self.seen[e][sid] = val
        self.engs[e].wait_ge(self.sems[sid], val)
        self.n_wait += 1

    def _deps(self, e, reads, writes):
        for k in reads:
            st = self.state.get(k)
            if st:
                for ev in st["w"]:
                    self._need(e, ev)
        for k in writes:
            st = self.state.get(k)
            if st:
                for ev in st["w"]:
                    self._need(e, ev)
                for ev in st["r"]:
                    self._need(e, ev)

    def _record(self, ev, reads, writes):
        for k in writes:
            self.state[k] = {"w": [ev], "r": []}
        for k in reads:
            st = self.state.setdefault(k, {"w": [], "r": []})
            st["r"].append(ev)
            if len(st["r"]) > 16:
                best = {}
                for s, v in st["r"]:
                    if best.get(s, 0) < v:
                        best[s] = v
                st["r"] = list(best.items())

    def _roll(self, e):
        if self.tick[e] >= self.LIMIT:
            self.epoch[e] = self.epoch.get(e, 0) + 1
            sid = "tk_%s_%d" % (e, self.epoch[e])
            self.sems[sid] = self.stack.enter_context(self.nc.semaphore(sid))
            self.tick_sem[e] = self.sems[sid]
            self.tick_sid[e] = sid
            self.tick[e] = 0

    def barrier(self):
        cur = [(self.tick_sid[e], self.tick[e]) for e in self.ENG if self.tick[e] > 0]
        for e in self.ENG:
            for ev in cur:
                self._need(e, ev)
        for k, st in self.state.items():
            st["w"] = [ev for ev in st["w"] if not ev[0].startswith("tk_")]
            st["r"] = [ev for ev in st["r"] if not ev[0].startswith("tk_")]

    def op(self, e, fn, reads=(), writes=()):
        self._deps(e, reads, writes)
        self._roll(e)
        self.tick[e] += 1
        ev = (self.tick_sid[e], self.tick[e])
        fn(self.engs[e]).then_inc(self.tick_sem[e], 1)
        self._record(ev, reads, writes)
        self.n_ops += 1
        return ev

    def ops(self, e, fns, reads=(), writes=()):
        self._deps(e, reads, writes)
        self._roll(e)
        self.tick[e] += 1
        ev = (self.tick_sid[e], self.tick[e])
        for fn in fns[:-1]:
            fn(self.engs[e])
        fns[-1](self.engs[e]).then_inc(self.tick_sem[e], 1)
        self._record(ev, reads, writes)
        self.n_ops += len(fns)
        return ev

    def dma(self, q, fn, key, reads=(), writes=()):
        self._deps(q, reads, writes)
        if key not in self.dma_sem:
            sid = "dma_" + str(key)
            self.sems[sid] = self.stack.enter_context(self.nc.semaphore(sid))
            self.dma_sem[key] = [sid, 0]
        sid, cnt = self.dma_sem[key]
        if cnt >= self.LIMIT:
            self.dma_gen += 1
            sid = "dma_%s_g%d" % (key, self.dma_gen)
            self.sems[sid] = self.stack.enter_context(self.nc.semaphore(sid))
            self.dma_sem[key] = [sid, 0]
            cnt = 0
        r = fn(self.engs[q])
        if not isinstance(r, (list, tuple)):
            r = [r]
        for ins in r:
            ins.then_inc(self.sems[sid], 16)
        cnt += 16 * len(r)
        self.dma_sem[key][1] = cnt
        ev = (sid, cnt)
        self._record(ev, reads, writes)
        return ev

    def finish(self, keys):
        for k in keys:
            st = self.state.get(k)
            if st:
                for ev in st["w"] + st["r"]:
                    self._need("sp", ev)


class Rot:
    def __init__(self, alloc, name, n, shape, dt):
        self.t = [alloc(name + str(i), shape, dt) for i in range(n)]
        self.k = [name + str(i) for i in range(n)]
        self.i = 0

    def get(self):
        j = self.i % len(self.t)
        self.i += 1
        return self.t[j], self.k[j]


def _plan(NB, NL, NEXP):
    plan = []
    for l in range(NL):
        for j in range(12):
            plan.append(("mod", l, j))
    for b in range(NB):
        for l in range(NL):
            plan.append(("q", l))
            plan.append(("kkv", l))
            for c in range(4):
                plan.append(("conv", l, c))
            for dh in range(2):
                plan.append(("o", l, dh))
    return plan


def build(NB=4, NL=2, NEXP=NE, stop=None):
    nc = bass.Bass("TRN2", target_bir_lowering=False)

    def din(name, shape, dt=F32):
        return nc.dram_tensor(name, list(shape), dt, kind="ExternalInput").ap()

    x_d = din("x", [NB, SEQ, D])
    cT_d = din("cT", [128, 8, NB])
    wmod_d = din("w_mod", [2, D, 6 * D])
    bmodT_d = din("b_modT", [2, 128, 48])
    gmixT_d = din("g_mixT", [2, 128, 8])
    gffnT_d = din("g_ffnT", [2, 128, 8])
    gfinT_d = din("g_finT", [128, 8])
    win_d = din("w_in_r", [2, D, WIN])
    gq2_d = din("gq2", [2, 128, 1])
    gk2_d = din("gk2", [2, 128, 1])
    wdwT_d = din("wdwT", [2, 128, 4 * 31])
    bdwT_d = din("b_dwT", [2, 128, 4])
    gcnT_d = din("g_cnT", [2, 128, 4])
    bcnT_d = din("b_cnT", [2, 128, 4])
    wout_d = din("w_out", [2, D, D])
    wr_d = din("w_router", [2, D, NE])
    br_d = din("b_router", [2, NE])
    w1_d = din("w1", [2, NEXP, D, 2 * D])
    b1R_d = din("b1R", [2, NE * 128, 16])
    b2R_d = din("b2R", [2, NE * 128, 8])
    hs_d = nc.dram_tensor("hs_scratch", [64 * 256, D], BF16, kind="Internal").ap()
    ys_d = nc.dram_tensor("ys_scratch", [64 * 256, D], F32, kind="Internal").ap()
    w2_d = din("w2", [2, NEXP, D, D])
    cos_d = din("cosT", [128, SEQ])
    sin_d = din("sinT", [128, SEQ])
    pm_d = din("pmat", [128, 128])
    out_d = nc.dram_tensor("out", [NB, SEQ, D], F32, kind="ExternalOutput").ap()

    with contextlib.ExitStack() as st:
        S = Sched(nc, st)

        uniq = [0]

        def sb(name, shape, dt, stack=st):
            uniq[0] += 1
            return stack.enter_context(nc.sbuf_tensor("%s_s%d" % (name, uniq[0]), list(shape), dt))

        xT = sb("xT", [128, 8, SEQ], F32)
        A = sb("A", [128, 8, SEQ], BF16)
        wslot = [sb("wslot%d" % i, [128, 8, 512], BF16) for i in range(3)]
        cosT = sb("cos", [128, SEQ], BF16)
        sinT = sb("sin", [128, SEQ], BF16)
        pmat = sb("pmat_s", [128, 128], BF16)
        ident = sb("ident", [128, 128], F32)
        identb = sb("identb", [128, 128], BF16)
        onesb = sb("onesb", [128, 128], BF16)
        onesbd = sb("onesbd", [128, 128], BF16)
        ones32 = sb("ones32", [128, 128], F32)
        cvec = sb("cvec", [128, 8], F32)
        cact = sb("cact", [128, 8, NB], F32)
        cactb = sb("cactb", [128, 8, NB], BF16)
        modT = [sb("modT%d" % l, [128, 48, NB], F32) for l in range(NL)]
        bmodT = [sb("bmodT%d" % l, [128, 48], F32) for l in range(NL)]
        gmixT = [sb("gmixT%d" % l, [128, 8], F32) for l in range(NL)]
        gffnT = [sb("gffnT%d" % l, [128, 8], F32) for l in range(NL)]
        gfinT = sb("gfinT", [128, 8], F32)
        gq2 = [sb("gq2_%d" % l, [128, 1], F32) for l in range(NL)]
        gk2 = [sb("gk2_%d" % l, [128, 1], F32) for l in range(NL)]
        wdwT = [sb("wdwT%d" % l, [128, 4 * 31], F32) for l in range(NL)]
        bdwT = [sb("bdwT%d" % l, [128, 4], F32) for l in range(NL)]
        gcnT = [sb("gcnT%d" % l, [128, 4], F32) for l in range(NL)]
        bcnT = [sb("bcnT%d" % l, [128, 4], F32) for l in range(NL)]
        wr32 = [sb("wr32_%d" % l, [128, 8, NE], F32) for l in range(NL)]
        brow = [sb("brow%d" % l, [1, NE], F32) for l in range(NL)]
        utri = sb("utri", [128, 128], BF16)
        utri32 = sb("utri32", [128, 128], F32)
        pcol = sb("pcol", [128, 1], F32)
        bigcol = sb("bigcol", [128, 1], F32)
        basepc = sb("basepc", [128, 8], F32)
        ipc = sb("ipc", [128, 8], mybir.dt.int32)
        gmod = sb("gmod", [128, 8], F32)
        pb = [st.enter_context(nc.psum_tensor("pb%d" % i, [128, 512], F32)) for i in range(8)]
        PB = ["pb%d" % i for i in range(8)]

        const_keys = []

        def cload(q, dst, src, key):
            S.dma(q, lambda e: e.dma_start(out=dst, in_=src), "const", writes=[key])
            const_keys.append(key)

        cload("pool", cosT[:], cos_d, "cos")
        cload("pool", sinT[:], sin_d, "sin")
        cload("pool", pmat[:], pm_d, "pmat")
        cload("sp", cact[:], cT_d, "cact")
        cload("sp", gfinT[:], gfinT_d, "gfinT")
        for l in range(NL):
            cload("sp", bmodT[l][:], bmodT_d[l], "bmodT%d" % l)
            cload("sp", gmixT[l][:], gmixT_d[l], "gmixT%d" % l)
            cload("sp", gffnT[l][:], gffnT_d[l], "gffnT%d" % l)
            cload("sp", gq2[l][:], gq2_d[l], "gq2%d" % l)
            cload("sp", gk2[l][:], gk2_d[l], "gk2%d" % l)
            cload("sp", wdwT[l][:], wdwT_d[l], "wdwT%d" % l)
            cload("sp", bdwT[l][:], bdwT_d[l], "bdwT%d" % l)
            cload("sp", gcnT[l][:], gcnT_d[l], "gcnT%d" % l)
            cload("sp", bcnT[l][:], bcnT_d[l], "bcnT%d" % l)
            cload("sp", wr32[l][:], wr_d[l].rearrange("(c p) n -> p c n", p=128), "wr%d" % l)
            cload("sp", brow[l][:], br_d[l:l + 1, :], "brow%d" % l)
        fin = (S.dma_sem["const"][0], S.dma_sem["const"][1])
        for k in const_keys:
            S.state[k] = {"w": [fin], "r": []}

        S.op("pool", lambda e: e.memset(ident[:], 0.0), writes=["ident"])
        S.op("pool", lambda e: e.affine_select(out=ident[:], in_=ident[:], pattern=[[-1, 128]],
                                               compare_op=ALU.not_equal, fill=1.0, base=0,
                                               channel_multiplier=1),
             reads=["ident"], writes=["ident"])
        S.op("dve", lambda e: e.tensor_copy(out=identb[:], in_=ident[:]), reads=["ident"], writes=["identb"])
        S.op("pool", lambda e: e.memset(utri32[:], 1.0), writes=["utri32"])
        S.op("pool", lambda e: e.affine_select(out=utri32[:], in_=utri32[:], pattern=[[1, 128]],
                                               compare_op=ALU.is_gt, fill=0.0, base=0, channel_multiplier=-1),
             reads=["utri32"], writes=["utri32"])
        S.op("dve", lambda e: e.tensor_copy(out=utri[:], in_=utri32[:]), reads=["utri32"], writes=["utri"])
        S.op("pool", lambda e: e.iota(ipc[:], pattern=[[128, 8]], base=0, channel_multiplier=1), writes=["ipc"])
        S.op("dve", lambda e: e.tensor_copy(out=basepc[:], in_=ipc[:]), reads=["ipc"], writes=["basepc"])
        S.op("dve", lambda e: e.tensor_copy(out=pcol[:], in_=ipc[:, 0:1]), reads=["ipc"], writes=["pcol"])
        S.op("dve", lambda e: e.tensor_scalar(out=bigcol[:], in0=pcol[:], scalar1=0.5, scalar2=1048576.0, op0=ALU.is_gt, op1=ALU.mult),
             reads=["pcol"], writes=["bigcol"])
        S.op("dve", lambda e: e.memset(onesb[:], 1.0), writes=["onesb"])
        S.op("dve", lambda e: e.memset(ones32[:], 1.0), writes=["ones32"])
        S.op("dve", lambda e: e.memset(onesbd[:], 0.0), writes=["onesbd"])
        S.op("dve", lambda e: e.memset(onesbd[0:64, 0:64], 1.0), reads=["onesbd"], writes=["onesbd"])
        S.op("dve", lambda e: e.memset(onesbd[64:128, 64:128], 1.0), reads=["onesbd"], writes=["onesbd"])
        S.op("dve", lambda e: e.memset(cvec[:, 0:1], EPS), writes=["cvec"])
        S.op("dve", lambda e: e.memset(cvec[:, 1:2], 64.0 * EPS), reads=["cvec"], writes=["cvec"])
        S.op("dve", lambda e: e.memset(cvec[:, 2:3], 0.0), reads=["cvec"], writes=["cvec"])
        eps_c = cvec[:, 0:1]
        eps64_c = cvec[:, 1:2]
        zero_c = cvec[:, 2:3]
        S.op("act", lambda e: e.activation(out=cactb[:], in_=cact[:], func=AF.Silu),
             reads=["cact"], writes=["cactb"])
        for l in range(NL):
            S.op("dve", lambda e, l=l: e.tensor_scalar(out=gk2[l][:], in0=gk2[l][:], scalar1=8.0,
                                                       scalar2=None, op0=ALU.mult),
                 reads=["gk2%d" % l], writes=["gk2%d" % l])

        plan = _plan(NB, NL, NEXP)
        wstate = {"next": 0, "cons": 0}
        pgrp = []
        g = 0
        for idx_, d_ in enumerate(plan):
            if d_[0] == "q":
                g += 1
            pgrp.append(g)

        def piece_dma(desc, slot):
            kind = desc[0]
            l = desc[1]
            if kind == "mod":
                j = desc[2]
                src = wmod_d[l].rearrange("(c p) n -> p c n", p=128)[:, :, j * 512:(j + 1) * 512]
                dst = slot[:, :, 0:512]
            elif kind == "conv":
                c = desc[2]
                src = win_d[l].rearrange("(c p) n -> p c n", p=128)[:, :, 896 + 256 * c:896 + 256 * (c + 1)]
                dst = slot[:, :, 0:256]
            elif kind == "q":
                src = win_d[l].rearrange("(c p) n -> p c n", p=128)[:, :, 0:512]
                dst = slot[:, :, 0:512]
            elif kind == "kkv":
                src = win_d[l].rearrange("(c p) n -> p c n", p=128)[:, :, 512:896]
                dst = slot[:, :, 0:384]
            elif kind == "o":
                dh = desc[2]
                src = wout_d[l].rearrange("(c p) n -> p c n", p=128)[:, :, dh * 512:(dh + 1) * 512]
                dst = slot[:, :, 0:512]
            elif kind == "w1":
                e, pg = desc[2], desc[3]
                v = w1_d[l, e].rearrange("(c p) n -> p c n", p=128)
                src = [v[:, :, 256 * pg:256 * (pg + 1)], v[:, :, 1024 + 256 * pg:1024 + 256 * (pg + 1)]]
                dst = [slot[:, :, 0:256], slot[:, :, 256:512]]
            elif kind == "w2":
                e, dh = desc[2], desc[3]
                src = w2_d[l, e].rearrange("(c p) n -> p c n", p=128)[:, :, dh * 512:(dh + 1) * 512]
                dst = slot[:, :, 0:512]
            if not isinstance(dst, list):
                dst, src = [dst], [src]
            return dst, src

        def wget(desc):
            i = wstate["cons"]
            assert plan[i] == desc, (plan[i], desc)
            while wstate["next"] < len(plan) and wstate["next"] <= i + 2 and pgrp[wstate["next"]] == pgrp[i]:
                j = wstate["next"]
                dst, src = piece_dma(plan[j], wslot[j % 3])
                S.dma("pool", lambda e, dst=dst, src=src: [e.dma_start(out=d_, in_=s_) for d_, s_ in zip(dst, src)],
                      "ws%d" % (j % 3), writes=["ws%d" % (j % 3)])
                wstate["next"] += 1
            wstate["cons"] += 1
            return wslot[i % 3], "ws%d" % (i % 3)

        def wprefetch():
            i = wstate["cons"]
            while wstate["next"] < len(plan) and wstate["next"] <= i + 2 and pgrp[wstate["next"]] == pgrp[max(i - 1, 0)]:
                j = wstate["next"]
                dst, src = piece_dma(plan[j], wslot[j % 3])
                S.dma("pool", lambda e, dst=dst, src=src: [e.dma_start(out=d_, in_=s_) for d_, s_ in zip(dst, src)],
                      "ws%d" % (j % 3), writes=["ws%d" % (j % 3)])
                wstate["next"] += 1

        def blk(j):
            return slice(j * 512, (j + 1) * 512)

        def mm(out, lhsT, rhs, start, stop):
            return lambda e: e.matmul(out, lhsT=lhsT, rhs=rhs, start=start, stop=stop)

        for l in range(NL):
            for j in range(12):
                wt, wk = wget(("mod", l, j))
                for jj in range(4):
                    col = j * 4 + jj
                    bank = pb[col % 2]
                    fns = [mm(bank[:, 0:NB], wt[:, k, jj * 128:(jj + 1) * 128], cactb[:, k, :], k == 0, k == 7)
                           for k in range(8)]
                    S.ops("pe", fns, reads=[wk, "cactb"], writes=[PB[col % 2]])
                    S.op("dve", lambda e, bank=bank, l=l, col=col: e.tensor_scalar(
                        out=modT[l][:, col, :], in0=bank[:, 0:NB], scalar1=bmodT[l][:, col:col + 1],
                        scalar2=None, op0=ALU.add),
                        reads=[PB[col % 2], "bmodT%d" % l], writes=[("modT", l, col)])
                wprefetch()
        S.barrier()

        def mod_ap(l, which, c, b):
            return modT[l][:, which * 8 + c, b:b + 1]

        def norm_stats(stk, j, sqr, sdr, scale):
            sq, sqk = sqr.get()
            S.op("act", lambda e: e.activation(out=sq[:], in_=xT[:, :, blk(j)], func=AF.Square),
                 reads=[("x", c, j) for c in range(8)], writes=[sqk])
            bank = pb[j % 2]
            S.ops("pe", [mm(bank[:], onesb[:], sq[:, k, :], k == 0, k == 7) for k in range(8)],
                  reads=[sqk, "onesb"], writes=[PB[j % 2]])
            sd, sdk = sdr.get()
            S.op("act", lambda e: e.activation(out=sd[:], in_=bank[:], func=AF.Ln, bias=eps_c, scale=scale),
                 reads=[PB[j % 2], "cvec"], writes=[sdk])
            S.op("act", lambda e: e.activation(out=sd[:], in_=sd[:], func=AF.Exp, scale=-0.5), reads=[sdk], writes=[sdk])
            return sd, sdk

        def load_x(b):
            with contextlib.ExitStack() as ph:
                xin = Rot(lambda n, s, d: sb(n, s, d, ph), "xin", 2, [128, D], F32)
                for t in range(NT):
                    xi, xk = xin.get()
                    S.dma("sp", lambda e, xi=xi, t=t: e.dma_start(out=xi[:], in_=x_d[b, t * 128:(t + 1) * 128, :]),
                          xk, writes=[xk])
                    for half in range(2):
                        bi = (2 * t + half) % 4
                        bank = pb[bi]
                        fns = [(lambda e, cc=cc, bank=bank, xi=xi, half=half: e.transpose(
                            out=bank[:, cc * 128:(cc + 1) * 128],
                            in_=xi[:, (half * 4 + cc) * 128:(half * 4 + cc + 1) * 128], identity=ident[:]))
                            for cc in range(4)]
                        S.ops("pe", fns, reads=[xk, "ident"], writes=[PB[bi]])
                        eng = "dve" if half == 0 else "act"
                        dst = xT[:, half * 4:half * 4 + 4, t * 128:(t + 1) * 128]
                        src = bank[:].rearrange("p (c t) -> p c t", c=4)
                        wr = [("x", half * 4 + cc, t // 4) for cc in range(4)]
                        if eng == "dve":
                            S.op("dve", lambda e, dst=dst, src=src: e.tensor_copy(out=dst, in_=src),
                                 reads=[PB[bi]], writes=wr)
                        else:
                            S.op("act", lambda e, dst=dst, src=src: e.copy(out=dst, in_=src),
                                 reads=[PB[bi]], writes=wr)
                S.barrier()

        def store_x(b, do_norm):
            with contextlib.ExitStack() as ph:
                al = lambda n, s, d: sb(n, s, d, ph)
                sqr = Rot(al, "f_sq", 2, [128, 8, 512], BF16)
                sdr = Rot(al, "f_sd", 2, [128, 512], F32)
                y32 = Rot(al, "f_y", 1, [128, 8, 512], F32)
                ost = Rot(al, "f_o", 2, [128, D], F32)
                for j in range(NBLK):
                    y, yk = y32.get()
                    if do_norm:
                        rs, rsk = norm_stats(ph, j, sqr, sdr, 1.0 / D)
                        for c in range(8):
                            S.op("dve", lambda e, c=c, y=y, rs=rs: e.scalar_tensor_tensor(
                                out=y[:, c, :], in0=xT[:, c, blk(j)], scalar=gfinT[:, c:c + 1], in1=rs[:],
                                op0=ALU.mult, op1=ALU.mult),
                                reads=[("x", c, j), rsk, "gfinT"], writes=[(yk, c)])
                    else:
                        for c in range(8):
                            S.op("dve", lambda e, c=c, y=y: e.tensor_copy(out=y[:, c, :], in_=xT[:, c, blk(j)]),
                                 reads=[("x", c, j)], writes=[(yk, c)])
                    for t in range(4):
                        o, ok = ost.get()
                        for half in range(2):
                            bi = (2 * t + half) % 4
                            bank = pb[bi]
                            fns = [(lambda e, cc=cc, bank=bank, y=y, half=half, t=t: e.transpose(
                                out=bank[:, cc * 128:(cc + 1) * 128],
                                in_=y[:, half * 4 + cc, t * 128:(t + 1) * 128], identity=ident[:]))
                                for cc in range(4)]
                            S.ops("pe", fns, reads=[(yk, half * 4 + cc) for cc in range(4)] + ["ident"],
                                  writes=[PB[bi]])
                            if half == 0:
                                S.op("dve", lambda e, o=o, bank=bank: e.tensor_copy(out=o[:, 0:512], in_=bank[:]),
                                     reads=[PB[bi]], writes=[(ok, 0)])
                            else:
                                S.op("act", lambda e, o=o, bank=bank: e.copy(out=o[:, 512:1024], in_=bank[:]),
                                     reads=[PB[bi]], writes=[(ok, 1)])
                        row = (j * 4 + t) * 128
                        S.dma("sp", lambda e, o=o, row=row: e.dma_start(out=out_d[b, row:row + 128, :], in_=o[:]),
                              "st_" + ok, reads=[(ok, 0), (ok, 1)], writes=[("out", ok)])
                S.barrier()

        def norm_mod(b, l, sub, ph, h32r=None, router=None):
            al = lambda n, s, d: sb(n, s, d, ph)
            sqr = Rot(al, "n_sq", 2, [128, 8, 512], BF16)
            sdr = Rot(al, "n_sd", 2, [128, 512], F32)
            h32r = Rot(al, "n_h", 1, [128, 8, 512], F32)
            gsrc = gmixT[l] if sub == 0 else gffnT[l]
            gkey = ("gmixT%d" if sub == 0 else "gffnT%d") % l
            S.op("dve", lambda e: e.scalar_tensor_tensor(
                out=gmod[:], in0=modT[l][:, (1 + 3 * sub) * 8:(2 + 3 * sub) * 8, b], scalar=1.0, in1=gsrc[:],
                op0=ALU.add, op1=ALU.mult),
                reads=[("modT", l, (1 + 3 * sub) * 8 + c) for c in range(8)] + [gkey], writes=["gmod"])
            for j in range(NBLK):
                rs, rsk = norm_stats(ph, j, sqr, sdr, 1.0 / D)
                h, hk = h32r.get()
                for c in range(8):
                    S.op("dve", lambda e, c=c, h=h, rs=rs: e.scalar_tensor_tensor(
                        out=h[:, c, :], in0=xT[:, c, blk(j)], scalar=gmod[:, c:c + 1], in1=rs[:],
                        op0=ALU.mult, op1=ALU.mult),
                        reads=[("x", c, j), rsk, "gmod"], writes=[(hk, c)])
                    sh = mod_ap(l, 3 * sub, c, b)
                    if router is None:
                        S.op("act", lambda e, c=c, h=h, sh=sh: e.activation(
                            out=A[:, c, blk(j)], in_=h[:, c, :], func=AF.Identity, bias=sh, scale=1.0),
                            reads=[(hk, c), ("modT", l, 3 * sub * 8 + c)], writes=[("A", c, j)])
                    else:
                        S.op("act", lambda e, c=c, h=h, sh=sh: e.activation(
                            out=h[:, c, :], in_=h[:, c, :], func=AF.Identity, bias=sh, scale=1.0),
                            reads=[(hk, c), ("modT", l, 3 * sub * 8 + c)], writes=[(hk, c)])
                        S.op("dve", lambda e, c=c, h=h: e.tensor_copy(out=A[:, c, blk(j)], in_=h[:, c, :]),
                             reads=[(hk, c)], writes=[("A", c, j)])
                if router is not None:
                    router(j, h, hk)

        def mixer(b, l):
            with contextlib.ExitStack() as ph:
                norm_mod(b, l, 0, ph)
                S.barrier()
            with contextlib.ExitStack() as ph_q:
                qT = sb("qT", [128, 4, SEQ], BF16, ph_q)
                with contextlib.ExitStack() as ph_kv:
                    al0 = lambda n, s, d: sb(n, s, d, ph_kv)
                    kz = al0("kz", [128, 4, SEQ], BF16)
                    S.op("dve", lambda e: e.memset(kz[:], 0.0), writes=[("k", kc_, j_) for kc_ in range(2) for j_ in range(NBLK)])
                    v1 = al0("v1", [128, NT, 320], BF16)
                    S.op("dve", lambda e: e.memset(v1[:], 1.0), writes=[("v1", t) for t in range(NT)])
                    with contextlib.ExitStack() as ph:
                        al = lambda n, s, d: sb(n, s, d, ph)
                        sqr = Rot(al, "p_sq", 2, [128, 512], BF16)
                        sdr = Rot(al, "p_sd", 2, [128, 512], F32)
                        qnr = Rot(al, "p_qn", 2, [128, 512], BF16)
                        ar = Rot(al, "p_a", 2, [128, 512], F32)
                        br = Rot(al, "p_b", 2, [128, 512], F32)
                        cnt = [0]

                        def stageA(cs):
                            wt, wk, col0, j = cs["wt"], cs["wk"], cs["col0"], cs["j"]
                            i = cs["i"]
                            pq, kq = pb[i % 2], PB[i % 2]
                            S.ops("pe", [mm(pq[:], wt[:, k, col0:col0 + 128], A[:, k, blk(j)], k == 0, k == 7) for k in range(8)],
                                  reads=[wk] + [("A", k, j) for k in range(8)], writes=[kq])
                            sq, sqk = sqr.get()
                            S.op("act", lambda e: e.activation(out=sq[:], in_=pq[:], func=AF.Square), reads=[kq], writes=[sqk])
                            cs["sq"], cs["sqk"] = sq, sqk

                        def stageB(cs):
                            i = cs["i"]
                            pq, kq = pb[i % 2], PB[i % 2]
                            pss, kss = pb[2 + i % 2], PB[2 + i % 2]
                            sq, sqk = cs["sq"], cs["sqk"]
                            S.ops("pe", [mm(pss[:], onesbd[:], sq[:], True, True)], reads=[sqk, "onesbd"], writes=[kss])
                            sd, sdk = sdr.get()
                            S.op("act", lambda e: e.activation(out=sd[:], in_=pss[:], func=AF.Ln, bias=eps64_c, scale=1.0),
                                 reads=[kss, "cvec"], writes=[sdk])
                            S.op("act", lambda e: e.activation(out=sd[:], in_=sd[:], func=AF.Exp, scale=-0.5), reads=[sdk], writes=[sdk])
                            qn, qnk = qnr.get()
                            gain, gkey = cs["gain"], cs["gkey"]
                            S.op("dve", lambda e: e.scalar_tensor_tensor(out=qn[:], in0=pq[:], scalar=gain[:, 0:1], in1=sd[:],
                                                                         op0=ALU.mult, op1=ALU.mult),
                                 reads=[kq, sdk, gkey], writes=[qnk])
                            cs["qn"], cs["qnk"] = qn, qnk

                        def stageC(cs):
                            i, j = cs["i"], cs["j"]
                            ppq, kpq = pb[4 + i % 2], PB[4 + i % 2]
                            qn, qnk = cs["qn"], cs["qnk"]
                            S.ops("pe", [mm(ppq[:], pmat[:], qn[:], True, True)], reads=[qnk, "pmat"], writes=[kpq])
                            a, ak = ar.get()
                            bb, bk = br.get()
                            S.op("dve", lambda e: e.tensor_tensor(out=a[:], in0=qn[:], in1=cosT[:, blk(j)], op=ALU.mult),
                                 reads=[qnk, "cos"], writes=[ak])
                            S.op("dve", lambda e: e.tensor_tensor(out=bb[:], in0=ppq[:], in1=sinT[:, blk(j)], op=ALU.mult),
                                 reads=[kpq, "sin"], writes=[bk])
                            if cs.get("kc") is None:
                                S.op("dve", lambda e: e.tensor_tensor(out=cs["dst"], in0=a[:], in1=bb[:], op=ALU.add),
                                     reads=[ak, bk], writes=[cs["dkey"]])
                            else:
                                kc_ = cs["kc"]
                                S.op("dve", lambda e: e.tensor_tensor(out=kz[0:64, 2 * kc_, blk(j)], in0=a[0:64, :], in1=bb[0:64, :], op=ALU.add),
                                     reads=[ak, bk, cs["dkey"]], writes=[cs["dkey"]])
                                S.op("dve", lambda e: e.tensor_tensor(out=kz[64:128, 2 * kc_ + 1, blk(j)], in0=a[64:128, :], in1=bb[64:128, :],
                                                                      op=ALU.add),
                                     reads=[ak, bk, cs["dkey"]], writes=[cs["dkey"]])

                        wtq, wkq = wget(("q", l))
                        wtk, wkk = None, None
                        chunks = []
                        for j in range(NBLK):
                            for c in range(4):
                                chunks.append(dict(wt=wtq, wk=wkq, col0=c * 128, j=j, dst=qT[:, c, blk(j)], dkey=("q", c, j),
                                                   gain=gq2[l], gkey="gq2%d" % l))
                        for j in range(NBLK):
                            for kc in range(2):
                                chunks.append(dict(wt=wtk, wk=wkk, col0=kc * 128, j=j, dst=None, kc=kc, dkey=("k", kc, j),
                                                   gain=gk2[l], gkey="gk2%d" % l))
                        for i_, cs in enumerate(chunks):
                            cs["i"] = i_
                        nch_ = len(chunks)
                        for step in range(nch_ + 2):
                            if step == 16:
                                wtk, wkk = wget(("kkv", l))
                                for cs in chunks[16:]:
                                    cs["wt"], cs["wk"] = wtk, wkk
                            if step < nch_:
                                stageA(chunks[step])
                            if 0 <= step - 1 < nch_:
                                stageB(chunks[step - 1])
                            if 0 <= step - 2 < nch_:
                                stageC(chunks[step - 2])
                        wt, wk = wtk, wkk
                        for j in range(NBLK):
                            for tt in range(4):
                                t = j * 4 + tt
                                bank = pb[6 + tt % 2]
                                S.ops("pe", [mm(bank[:, 0:128], A[:, k, t * 128:(t + 1) * 128], wt[:, k, 256:384], k == 0, k == 7)
                                             for k in range(8)],
                                      reads=[wk] + [("A", k, j) for k in range(8)], writes=[PB[6 + tt % 2]])
                                dst = v1[:, t, 64:320].rearrange("p (a b) -> p a b", b=128)[:, :, 0:64]
                                src = bank[:, 0:128].rearrange("p (a b) -> p a b", b=64)
                                S.op("act", lambda e, dst=dst, src=src: e.copy(out=dst, in_=src),
                                     reads=[PB[6 + tt % 2]], writes=[("v1", t)])
                        wprefetch()
                        S.barrier()
                    with contextlib.ExitStack() as ph:
                        al = lambda n, s, d: sb(n, s, d, ph)
                        ptr = Rot(al, "a_pt", 4, [128, 512], BF16)
                        recr = Rot(al, "a_rec", 2, [128, 512], F32)
                        seq = [(h, qj, kt) for h in range(8) for qj in range(NBLK) for kt in range(NT)]
                        pend = []

                        def score(idx):
                            h, qj, kt = seq[idx]
                            c, hp, kv = h // 2, h % 2, h // 4
                            bank = pb[idx % 3]
                            rows = slice(hp * 64, hp * 64 + 64)
                            S.ops("pe", [mm(bank[:], kz[:, 2 * kv + hp, kt * 128:(kt + 1) * 128], qT[:, c, blk(qj)], True, True)],
                                  reads=[("k", kv, kt // 4), ("q", c, qj)], writes=[PB[idx % 3]])
                            pt, ptk = ptr.get()
                            S.op("act", lambda e: e.activation(out=pt[:], in_=bank[:], func=AF.Exp),
                                 reads=[PB[idx % 3]], writes=[ptk])
                            return pt, ptk

                        def pv(idx, pt, ptk):
                            h, qj, kt = seq[idx]
                            c, hp, kv = h // 2, h % 2, h // 4
                            g = idx // NT
                            po = pb[4 + g % 2]
                            pok = PB[4 + g % 2]
                            col0 = 128 * kv + (64 if hp == 0 else 0)
                            S.ops("pe", [mm(po[:], v1[:, kt, col0:col0 + 128], pt[:], kt == 0, kt == NT - 1)],
                                  reads=[("v1", kt), ptk], writes=[pok])
                            if kt == NT - 1:
                                rec, reck = recr.get()
                                if hp == 0:
                                    S.op("dve", lambda e: e.reciprocal(out=rec[0:64, :], in_=po[64:128, :]), reads=[pok], writes=[reck])
                                    S.op("dve", lambda e: e.tensor_tensor(out=qT[0:64, c, blk(qj)], in0=po[0:64, :], in1=rec[0:64, :],
                                                                          op=ALU.mult),
                                         reads=[pok, reck], writes=[("q", c, qj)])
                                else:
                                    S.op("dve", lambda e: e.reciprocal(out=rec[64:128, :], in_=po[0:64, :]), reads=[pok], writes=[reck])
                                    S.op("dve", lambda e: e.tensor_tensor(out=qT[64:128, c, blk(qj)], in0=po[64:128, :], in1=rec[64:128, :],
                                                                          op=ALU.mult),
                                         reads=[pok, reck], writes=[("q", c, qj)])

                        n = len(seq)
                        look = 2
                        for i in range(n + look):
                            if i < n:
                                pend.append((i,) + score(i))
                            if i >= look:
                                idx, pt, ptk = pend.pop(0)
                                pv(idx, pt, ptk)
                        S.barrier()
                with contextlib.ExitStack() as ph:
                    al = lambda n, s, d: sb(n, s, d, ph)
                    upad = al("upad", [128, 4, SEQ + 30], BF16)
                    diag = al("diag", [128, 31, 128], BF16)
                    sigr = Rot(al, "c_sig", 2, [128, 512], F32)
                    S.op("dve", lambda e: e.memset(upad[:, :, 0:15], 0.0), writes=["upad_l"])
                    S.op("dve", lambda e: e.memset(upad[:, :, SEQ + 15:SEQ + 30], 0.0), writes=["upad_r"])
                    for c in range(4):
                        wt, wk = wget(("conv", l, c))
                        for j in range(NBLK):
                            ba, bg = pb[j % 2], pb[2 + j % 2]
                            S.ops("pe", [mm(ba[:], wt[:, k, 0:128], A[:, k, blk(j)], k == 0, k == 7) for k in range(8)],
                                  reads=[wk] + [("A", k, j) for k in range(8)], writes=[PB[j % 2]])
                            S.ops("pe", [mm(bg[:], wt[:, k, 128:256], A[:, k, blk(j)], k == 0, k == 7) for k in range(8)],
                                  reads=[wk] + [("A", k, j) for k in range(8)], writes=[PB[2 + j % 2]])
                            sg, sgk = sigr.get()
                            S.op("act", lambda e, sg=sg, bg=bg: e.activation(out=sg[:], in_=bg[:], func=AF.Sigmoid),
                                 reads=[PB[2 + j % 2]], writes=[sgk])
                            S.op("dve", lambda e, sg=sg, ba=ba, c=c, j=j: e.tensor_tensor(
                                out=upad[:, c, 15 + j * 512:15 + (j + 1) * 512], in0=ba[:], in1=sg[:], op=ALU.mult),
                                reads=[PB[j % 2], sgk], writes=[("u", c, j)])
                        wprefetch()
                    S.barrier()
                    for c in range(4):
                        for tp in range(31):
                            S.op("dve", lambda e, tp=tp, c=c: e.tensor_scalar(
                                out=diag[:, tp, :], in0=identb[:], scalar1=wdwT[l][:, c * 31 + tp:c * 31 + tp + 1],
                                scalar2=None, op0=ALU.mult),
                                reads=["identb", "wdwT%d" % l], writes=["diag"])
                        for j in range(NBLK):
                            bank = pb[4 + j % 2]
                            rd = ["diag", "upad_l", "upad_r"] + [("u", c, jj) for jj in range(max(0, j - 1), min(NBLK, j + 2))]
                            S.ops("pe", [mm(bank[:], diag[:, tp, :], upad[:, c, j * 512 + tp:j * 512 + tp + 512],
                                            tp == 0, tp == 30) for tp in range(31)],
                                  reads=rd, writes=[PB[4 + j % 2]])
                            S.op("act", lambda e, bank=bank, c=c, j=j: e.activation(
                                out=A[:, c, blk(j)], in_=bank[:], func=AF.Identity, bias=bdwT[l][:, c:c + 1], scale=1.0),
                                reads=[PB[4 + j % 2], "bdwT%d" % l], writes=[("A", c, j)])
                    S.barrier()
                with contextlib.ExitStack() as ph:
                    al = lambda n, s, d: sb(n, s, d, ph)
                    vsq = Rot(al, "l_vsq", 1, [128, 4, 512], BF16)
                    mur = Rot(al, "l_mu", 1, [128, 512], F32)
                    msr = Rot(al, "l_ms", 1, [128, 512], F32)
                    sdr = Rot(al, "l_sd", 1, [128, 512], F32)
                    tr = Rot(al, "l_t", 2, [128, 512], F32)
                    for j in range(NBLK):
                        vq, vqk = vsq.get()
                        S.op("act", lambda e, vq=vq: e.activation(out=vq[:], in_=A[:, 0:4, blk(j)], func=AF.Square),
                             reads=[("A", c, j) for c in range(4)], writes=[vqk])
                        S.ops("pe", [mm(pb[6][:], onesb[:], A[:, c, blk(j)], c == 0, c == 3) for c in range(4)],
                              reads=[("A", c, j) for c in range(4)] + ["onesb"], writes=[PB[6]])
                        S.ops("pe", [mm(pb[7][:], onesb[:], vq[:, c, :], c == 0, c == 3) for c in range(4)],
                              reads=[vqk, "onesb"], writes=[PB[7]])
                        mu, muk = mur.get()
                        ms, msk = msr.get()
                        sd, sdk = sdr.get()
                        S.op("dve", lambda e, mu=mu: e.tensor_scalar(out=mu[:], in0=pb[6][:], scalar1=1.0 / 512,
                                                                     scalar2=None, op0=ALU.mult),
                             reads=[PB[6]], writes=[muk])
                        S.op("dve", lambda e, mu=mu, ms=ms: e.tensor_tensor(out=ms[:], in0=mu[:], in1=mu[:], op=ALU.mult),
                             reads=[muk], writes=[msk])
                        S.op("dve", lambda e, ms=ms, sd=sd: e.scalar_tensor_tensor(
                            out=sd[:], in0=pb[7][:], scalar=1.0 / 512, in1=ms[:], op0=ALU.mult, op1=ALU.subtract),
                            reads=[PB[7], msk], writes=[sdk])
                        S.op("act", lambda e, sd=sd: e.activation(out=sd[:], in_=sd[:], func=AF.Ln, bias=eps_c, scale=1.0),
                             reads=[sdk, "cvec"], writes=[sdk])
                        S.op("act", lambda e, sd=sd: e.activation(out=sd[:], in_=sd[:], func=AF.Exp, scale=-0.5), reads=[sdk], writes=[sdk])
                        for c in range(4):
                            t, tk = tr.get()
                            S.op("dve", lambda e, t=t, c=c, mu=mu: e.tensor_tensor(
                                out=t[:], in0=A[:, c, blk(j)], in1=mu[:], op=ALU.subtract),
                                reads=[("A", c, j), muk], writes=[tk])
                            S.op("dve", lambda e, t=t, sd=sd: e.tensor_tensor(out=t[:], in0=t[:], in1=sd[:], op=ALU.mult),
                                 reads=[tk, sdk], writes=[tk])
                            S.op("act", lambda e, t=t, c=c: e.activation(
                                out=A[:, c, blk(j)], in_=t[:], func=AF.Silu, bias=bcnT[l][:, c:c + 1],
                                scale=gcnT[l][:, c:c + 1]),
                                reads=[tk, "gcnT%d" % l, "bcnT%d" % l], writes=[("A", c, j)])
                    S.barrier()
                for dh in range(2):
                    wt, wk = wget(("o", l, dh))
                    for j in range(NBLK):
                        for dc in range(4):
                            i = (j * 4 + dc) % 4
                            cch = dh * 4 + dc
                            fns = [mm(pb[i][:], wt[:, k, dc * 128:(dc + 1) * 128],
                                      qT[:, k, blk(j)] if k < 4 else A[:, k - 4, blk(j)], k == 0, k == 7) for k in range(8)]
                            S.ops("pe", fns,
                                  reads=[wk] + [("q", k, j) for k in range(4)] + [("A", k, j) for k in range(4)], writes=[PB[i]])
                            S.op("dve", lambda e, i=i, cch=cch, j=j: e.scalar_tensor_tensor(
                                out=xT[:, cch, blk(j)], in0=pb[i][:], scalar=mod_ap(l, 2, cch, b), in1=xT[:, cch, blk(j)],
                                op0=ALU.mult, op1=ALU.add),
                                reads=[PB[i], ("x", cch, j), ("modT", l, 16 + cch)], writes=[("x", cch, j)])
                    if dh == 0:
                        wprefetch()
                S.barrier()

        TS = 256
        NTILE = 64
        U32 = mybir.dt.uint32
        I32 = mybir.dt.int32

        def moe(b, l):
            w1rows = w1_d.rearrange("l e k n -> (l e k) n")
            w2rows = w2_d.rearrange("l e k n -> (l e k) n")
            b1rows = b1R_d.rearrange("l r n -> (l r) n")
            b2rows = b2R_d.rearrange("l r n -> (l r) n")
            with contextlib.ExitStack() as ph_g:
                alg = lambda n, s, d: sb(n, s, d, ph_g)
                gate4 = alg("gate4", [128, NT, 4], F32)
                slotu = alg("slotu", [128, NT * 4], U32)
                widxu = alg("widxu", [128, NTILE, 8], U32)
                bidxu = alg("bidxu", [128, NTILE], U32)
                ph_r = contextlib.ExitStack()
                alr = lambda n, s, d: sb(n, s, d, ph_r)
                lgs_all = alr("lgs_all", [128, NT, NE], F32)
                m8_all = alr("m8_all", [128, NT, 8], F32)
                maskb = alr("maskb", [128, NT, NE], BF16)
                slotf = alr("slotf", [128, NT * 4], F32)
                cnt = alr("cnt", [128, NE], F32)
                ntl = alr("ntl", [128, NE], F32)
                incl = alr("incl", [128, NE], F32)
                pst = alr("pst", [128, NE], F32)
                onesf = alr("onesf", [128, NE], F32)
                te = alr("te", [128, NTILE], F32)
                te128 = alr("te128", [128, NTILE], F32)
                tflag = alr("tflag", [128, NTILE], F32)
                widxf = alr("widxf", [128, NTILE, 8], F32)
                bidxf = alr("bidxf", [128, NTILE], F32)
                with contextlib.ExitStack() as ph:
                    al = lambda n, s, d: sb(n, s, d, ph)
                    nmr = Rot(al, "r_nm", 2, [128, 1], F32)
                    ssr = Rot(al, "r_ss", 2, [128, 1], F32)

                    def router(j, h, hk):
                        lg = pb[2]
                        for tt in range(4):
                            fns = [mm(lg[:, tt * NE:(tt + 1) * NE], h[:, k, tt * 128:(tt + 1) * 128], wr32[l][:, k, :], k == 0, False)
                                   for k in range(8)]
                            fns.append(mm(lg[:, tt * NE:(tt + 1) * NE], ones32[0:1, :], brow[l][0:1, :], False, True))
                            S.ops("pe", fns, reads=[(hk, k) for k in range(8)] + ["wr%d" % l, "brow%d" % l, "ones32"],
                                  writes=[(PB[2], tt)])
                        S.op("act", lambda e: e.copy(out=lgs_all[:, 4 * j:4 * j + 4, :],
                                                     in_=lg[:, 0:4 * NE].rearrange("p (t e) -> p t e", e=NE)),
                             reads=[(PB[2], tt) for tt in range(4)], writes=[("lgs", 4 * j + tt) for tt in range(4)])
                        for tt in range(4):
                            t = 4 * j + tt
                            lgt = lgs_all[:, t, :]
                            m8 = m8_all[:, t, :]
                            nm, nmk = nmr.get()
                            ss, ssk = ssr.get()
                            S.op("dve", lambda e: e.max(out=m8, in_=lgt), reads=[("lgs", t)], writes=[("m8", t)])
                            S.op("dve", lambda e: e.tensor_scalar(out=nm[:], in0=m8[:, 0:1], scalar1=-1.0, scalar2=None, op0=ALU.mult),
                                 reads=[("m8", t)], writes=[nmk])
                            S.op("act", lambda e: e.activation(out=gate4[:, t, :], in_=m8[:, 0:4], func=AF.Exp, bias=nm[:, 0:1], scale=1.0),
                                 reads=[("m8", t), nmk], writes=[("g4", t)])
                            S.op("dve", lambda e: e.tensor_reduce(out=ss[:], in_=gate4[:, t, :], axis=mybir.AxisListType.X, op=ALU.add),
                                 reads=[("g4", t)], writes=[ssk])
                            S.op("dve", lambda e: e.reciprocal(out=ss[:], in_=ss[:]), reads=[ssk], writes=[ssk])
                            S.op("dve", lambda e: e.tensor_scalar(out=gate4[:, t, :], in0=gate4[:, t, :], scalar1=ss[:, 0:1], scalar2=None,
                                                                  op0=ALU.mult),
                                 reads=[("g4", t), ssk], writes=[("g4", t)])
                            S.op("dve", lambda e: e.tensor_scalar(out=maskb[:, t, :], in0=lgt, scalar1=m8[:, 3:4], scalar2=None, op0=ALU.is_ge),
                                 reads=[("lgs", t), ("m8", t)], writes=[("mask", t)])

                    norm_mod(b, l, 1, ph, router=router)
                    S.barrier()
                with contextlib.ExitStack() as ph:
                    al = lambda n, s, d: sb(n, s, d, ph)
                    tmpr = Rot(al, "q_tmp", 2, [128, NE], F32)
                    Sr = Rot(al, "q_S", 2, [128, NE], F32)
                    S.ops("pe", [mm(pb[0][:, 0:NE], onesb[:], maskb[:, t, :], t == 0, t == NT - 1) for t in range(NT)],
                          reads=[("mask", t) for t in range(NT)] + ["onesb"], writes=[PB[0]])
                    S.op("dve", lambda e: e.tensor_copy(out=cnt[:], in_=pb[0][:, 0:NE]), reads=[PB[0]], writes=["cnt"])
                    S.op("dve", lambda e: e.tensor_scalar(out=ntl[:], in0=cnt[:], scalar1=0.0, scalar2=None, op0=ALU.is_gt),
                         reads=["cnt"], writes=["ntl"])
                    for jj in range(1, SEQ // TS):
                        S.op("dve", lambda e, jj=jj: e.scalar_tensor_tensor(out=ntl[:], in0=cnt[:], scalar=float(TS * jj), in1=ntl[:],
                                                                            op0=ALU.is_gt, op1=ALU.add),
                             reads=["cnt", "ntl"], writes=["ntl"])
                    S.op("dve", lambda e: e.memset(onesf[:], 1.0), writes=["onesf"])
                    S.op("dve", lambda e: e.tensor_tensor_scan(out=incl[:], data0=onesf[:], data1=ntl[:], initial=0.0,
                                                               op0=ALU.mult, op1=ALU.add),
                         reads=["onesf", "ntl"], writes=["incl"])
                    S.op("dve", lambda e: e.tensor_tensor(out=pst[:], in0=incl[:], in1=ntl[:], op=ALU.subtract),
                         reads=["incl", "ntl"], writes=["pst"])
                    for i in range(NTILE):
                        tmp, tmpk = tmpr.get()
                        S.op("dve", lambda e, i=i, tmp=tmp: e.tensor_scalar(out=tmp[:], in0=incl[:], scalar1=float(i), scalar2=None,
                                                                            op0=ALU.is_le),
                             reads=["incl"], writes=[tmpk])
                        S.op("dve", lambda e, i=i, tmp=tmp: e.tensor_reduce(out=te[:, i:i + 1], in_=tmp[:], axis=mybir.AxisListType.X,
                                                                            op=ALU.add),
                             reads=[tmpk], writes=[("te", i)])
                    tek = [("te", i) for i in range(NTILE)]
                    S.op("dve", lambda e: e.tensor_scalar(out=tflag[:], in0=te[:], scalar1=float(NE), scalar2=bigcol[:, 0:1],
                                                          op0=ALU.is_ge, op1=ALU.mult),
                         reads=tek + ["bigcol"], writes=["tflag"])
                    S.op("dve", lambda e: e.tensor_scalar(out=te[:], in0=te[:], scalar1=float(NE - 1), scalar2=None, op0=ALU.min),
                         reads=tek, writes=["te_all"])
                    S.op("dve", lambda e: e.tensor_scalar(out=te128[:], in0=te[:], scalar1=128.0, scalar2=float(l * NE * 128), op0=ALU.mult, op1=ALU.add),
                         reads=["te_all"], writes=["te128"])
                    S.op("dve", lambda e: e.tensor_scalar(out=bidxf[:], in0=te128[:], scalar1=pcol[:, 0:1], scalar2=None, op0=ALU.add),
                         reads=["te128", "pcol"], writes=["bidxf"])
                    S.op("dve", lambda e: e.tensor_copy(out=bidxu[:], in_=bidxf[:]), reads=["bidxf"], writes=["bidxu"])
                    for c in range(8):
                        S.op("dve", lambda e, c=c: e.tensor_scalar(out=widxf[:, :, c], in0=te128[:], scalar1=8.0, scalar2=basepc[:, c:c + 1],
                                                                   op0=ALU.mult, op1=ALU.add),
                             reads=["te128", "basepc"], writes=[("widxf", c)])

                    S.op("dve", lambda e: e.tensor_copy(out=widxu[:], in_=widxf[:]), reads=[("widxf", c) for c in range(8)], writes=["widxu"])
                    for t in range(NT):
                        bank = pb[1 + t % 2]
                        fns = [mm(bank[:, 0:NE], onesb[:], maskb[:, tp, :], tp == 0, False) for tp in range(t)]
                        fns.append(mm(bank[:, 0:NE], utri[:], maskb[:, t, :], t == 0, True))
                        S.ops("pe", fns, reads=[("mask", tp) for tp in range(t + 1)] + ["onesb", "utri"], writes=[PB[1 + t % 2]])
                        Sv, Sk = Sr.get()
                        S.op("dve", lambda e, Sv=Sv, bank=bank: e.scalar_tensor_tensor(out=Sv[:], in0=pst[:], scalar=float(TS), in1=bank[:, 0:NE],
                                                                                       op0=ALU.mult, op1=ALU.add),
                             reads=["pst", PB[1 + t % 2]], writes=[Sk])
                        for k in range(4):
                            tmp, tmpk = tmpr.get()
                            S.op("dve", lambda e, t=t, k=k, tmp=tmp, Sv=Sv: e.scalar_tensor_tensor(
                                out=tmp[:], in0=lgs_all[:, t, :], scalar=m8_all[:, t, k:k + 1], in1=Sv[:], op0=ALU.is_equal, op1=ALU.mult),
                                reads=[("lgs", t), ("m8", t), Sk], writes=[tmpk])
                            S.op("dve", lambda e, t=t, k=k, tmp=tmp: e.tensor_reduce(out=slotf[:, 4 * t + k:4 * t + k + 1], in_=tmp[:],
                                                                                    axis=mybir.AxisListType.X, op=ALU.add),
                                 reads=[tmpk], writes=[("slotf", t, k)])
                    S.op("dve", lambda e: e.tensor_copy(out=slotu[:], in_=slotf[:]),
                         reads=[("slotf", t, k) for t in range(NT) for k in range(4)], writes=["slotu"])
                    S.barrier()
                ph_r.close()
                with contextlib.ExitStack() as ph:
                    al = lambda n, s, d: sb(n, s, d, ph)
                    htr = Rot(al, "h_tok", 2, [128, D], BF16)
                    hs_ev = {}
                    for t in range(NT):
                        ht, htk = htr.get()
                        bank = pb[t % 2]
                        bview = bank[:].bitcast(BF16)
                        fns = [(lambda e, c=c, bview=bview, t=t: e.transpose(out=bview[:, c * 128:(c + 1) * 128],
                                                                              in_=A[:, c, t * 128:(t + 1) * 128], identity=identb[:]))
                               for c in range(8)]
                        S.ops("pe", fns, reads=[("A", c, t // 4) for c in range(8)] + ["identb"], writes=[PB[t % 2]])
                        if t % 2 == 0:
                            S.op("dve", lambda e, ht=ht, bview=bview: e.tensor_copy(out=ht[:], in_=bview), reads=[PB[t % 2]], writes=[htk])
                        else:
                            S.op("act", lambda e, ht=ht, bview=bview: e.copy(out=ht[:], in_=bview), reads=[PB[t % 2]], writes=[htk])

                        def sc(e, ht=ht, t=t):
                            return [e.indirect_dma_start(out=hs_d[:, :], out_offset=bass.IndirectOffsetOnAxis(
                                ap=slotu[:, 4 * t + k:4 * t + k + 1], axis=0), in_=ht[:, :], in_offset=None) for k in range(4)]
                        hs_ev[htk] = S.dma("pool", sc, "sc_" + htk, reads=[htk, "slotu"], writes=[("HS", t)])
                    S.state["HS"] = {"w": list(hs_ev.values()), "r": []}
                    S.barrier()
                with contextlib.ExitStack() as ph:
                    al = lambda n, s, d: sb(n, s, d, ph)
                    wA0 = al("wA0", [128, 8, 2 * D], BF16)
                    wA1b = al("wA1b", [128, 4, 2 * D], BF16)
                    wAs = [[wA0[:, c, :] for c in range(8)],
                           [A[:, 4 + c, :] for c in range(4)] + [wA1b[:, c, :] for c in range(4)]]
                    wBl = [A[:, c // 2, (c % 2) * D:(c % 2 + 1) * D] for c in range(8)]
                    hgr = Rot(al, "e_hg", 2, [128, 8, TS], BF16)
                    actr = Rot(al, "e_act", 1, [128, 8, TS], BF16)
                    glr = Rot(al, "e_gl", 2, [128, TS], F32)
                    tr = Rot(al, "e_t", 2, [128, TS], F32)
                    b1r = Rot(al, "e_b1", 2, [128, 16], F32)
                    b1pr = Rot(al, "e_b1p", 2, [128, 16], F32)
                    b2r = Rot(al, "e_b2", 2, [128, 8], F32)
                    hsts = [wslot[2][:].rearrange("p a b -> p (a b)").rearrange("p (s d) -> p s d", s=4)[:, 2 * q_:2 * q_ + 2, :]
                            for q_ in range(2)]

                    def hs_load(i_):
                        S.dma("sp", lambda e: e.dma_start(out=hsts[i_ % 2],
                                                          in_=hs_d[i_ * TS:(i_ + 1) * TS, :].rearrange("(s p) d -> p s d", p=128)),
                              "hsld%d" % (i_ % 2), reads=["HS"], writes=[("ws2", i_ % 2)])
                    hs_load(0)
                    yT = wslot[0][:].rearrange("p a b -> p (a b)").bitcast(F32).rearrange("p (c s) -> p c s", c=8)
                    ytok = [wslot[1][:].rearrange("p a b -> p (a b)").bitcast(F32).rearrange("p (s d) -> p s d", s=2)[:, s, :]
                            for s in range(2)]
                    ys_ev = {}
                    cntp = 0
                    for i in range(NTILE):
                        par = i % 2
                        wA = wAs[par]

                        def gw1f(i_, dstl):
                            def f(e):
                                return [e.indirect_dma_start(out=dstl[c], out_offset=None, in_=w1rows,
                                                             in_offset=bass.IndirectOffsetOnAxis(ap=widxu[:, i_, c:c + 1], axis=0)) for c in range(8)]
                            return f

                        def gbf(i_, b1t_, b2t_):
                            def f(e):
                                return [e.indirect_dma_start(out=b1t_[:, :], out_offset=None, in_=b1rows,
                                                             in_offset=bass.IndirectOffsetOnAxis(ap=bidxu[:, i_:i_ + 1], axis=0)),
                                        e.indirect_dma_start(out=b2t_[:, :], out_offset=None, in_=b2rows,
                                                             in_offset=bass.IndirectOffsetOnAxis(ap=bidxu[:, i_:i_ + 1], axis=0))]
                            return f

                        if i == 0:
                            bias_buf = {}
                            b1t, b1k = b1r.get()
                            b2t, b2k = b2r.get()
                            S.dma("pool", gbf(0, b1t, b2t), "gb0", reads=["bidxu"], writes=[b1k, b2k])
                            bias_buf[0] = (b1t, b1k, b2t, b2k)
                            S.dma("pool", gw1f(0, wAs[0]), "gwA0", reads=["widxu"], writes=[("wA", 0)])

                        def gw2(e, i=i):
                            return [e.indirect_dma_start(out=wBl[c], out_offset=None, in_=w2rows,
                                                         in_offset=bass.IndirectOffsetOnAxis(ap=widxu[:, i, c:c + 1], axis=0)) for c in range(8)]
                        S.dma("pool", gw2, "gwB", reads=["widxu"], writes=["wB"])
                        if i + 1 < NTILE:
                            nb1t, nb1k = b1r.get()
                            nb2t, nb2k = b2r.get()
                            S.dma("pool", gbf(i + 1, nb1t, nb2t), "gb%d" % ((i + 1) % 2), reads=["bidxu"], writes=[nb1k, nb2k])
                            bias_buf[i + 1] = (nb1t, nb1k, nb2t, nb2k)
                            S.dma("pool", gw1f(i + 1, wAs[1 - par]), "gwA%d" % (1 - par), reads=["widxu"], writes=[("wA", 1 - par)])
                        b1t, b1k, b2t, b2k = bias_buf.pop(i)
                        b1p, b1pk = b1pr.get()
                        S.op("dve", lambda e, b1p=b1p, b1t=b1t: e.tensor_scalar(out=b1p[:], in0=b1t[:], scalar1=1.0, scalar2=None, op0=ALU.add),
                             reads=[b1k], writes=[b1pk])
                        hstok = hsts[par]
                        if i + 1 < NTILE:
                            hs_load(i + 1)
                        hg, hgk = hgr.get()
                        for half in range(2):
                            bank = pb[4 + half]
                            bview = bank[:].bitcast(BF16)
                            fns = [(lambda e, cc=cc, s=s, bview=bview, half=half: e.transpose(
                                out=bview[:, cc * TS + s * 128:cc * TS + (s + 1) * 128],
                                in_=hstok[:, s, (half * 4 + cc) * 128:(half * 4 + cc + 1) * 128], identity=identb[:]))
                                for cc in range(4) for s in range(2)]
                            S.ops("pe", fns, reads=[("ws2", par), "identb"], writes=[PB[4 + half]])
                            dst = hg[:, half * 4:half * 4 + 4, :]
                            src = bview.rearrange("p (c s) -> p c s", c=4)
                            if half == 0:
                                S.op("dve", lambda e, dst=dst, src=src: e.tensor_copy(out=dst, in_=src), reads=[PB[4 + half]], writes=[(hgk, half)])
                            else:
                                S.op("dve", lambda e, dst=dst, src=src: e.tensor_copy(out=dst, in_=src), reads=[PB[4 + half]], writes=[(hgk, half)])
                        act, actk = actr.get()
                        for nch in range(8):
                            bank = pb[cntp % 2]
                            bk = PB[cntp % 2]
                            cntp += 1
                            fns = [mm(bank[:, 0:TS], wA[k][:, nch * 128:(nch + 1) * 128], hg[:, k, :], k == 0, k == 7) for k in range(8)]
                            fns += [mm(bank[:, TS:2 * TS], wA[k][:, D + nch * 128:D + (nch + 1) * 128], hg[:, k, :], k == 0, k == 7) for k in range(8)]
                            S.ops("pe", fns, reads=[("wA", par), (hgk, 0), (hgk, 1)], writes=[bk])
                            gl, glk = glr.get()
                            tq, tk = tr.get()
                            S.op("dve", lambda e, gl=gl, bank=bank, b1t=b1t, nch=nch: e.tensor_scalar(
                                out=gl[:], in0=bank[:, 0:TS], scalar1=b1t[:, nch:nch + 1], scalar2=7.0, op0=ALU.add, op1=ALU.min),
                                reads=[bk, b1k], writes=[glk])
                            S.op("act", lambda e, gl=gl: e.activation(out=gl[:], in_=gl[:], func=AF.Silu, scale=1.702), reads=[glk], writes=[glk])
                            S.op("dve", lambda e, tq=tq, bank=bank, b1p=b1p, nch=nch: e.tensor_scalar(
                                out=tq[:], in0=bank[:, TS:2 * TS], scalar1=b1p[:, 8 + nch:9 + nch], scalar2=8.0, op0=ALU.add, op1=ALU.min),
                                reads=[bk, b1pk], writes=[tk])
                            S.op("dve", lambda e, tq=tq, gl=gl, act=act, nch=nch: e.scalar_tensor_tensor(
                                out=act[:, nch, :], in0=tq[:], scalar=-6.0, in1=gl[:], op0=ALU.max, op1=ALU.mult),
                                reads=[tk, glk], writes=[(actk, nch)])
                        for dc in range(8):
                            bank = pb[2 + dc % 2]
                            bk = PB[2 + dc % 2]
                            S.ops("pe", [mm(bank[:, 0:TS], wBl[k][:, dc * 128:(dc + 1) * 128], act[:, k, :], k == 0, k == 7)
                                         for k in range(8)],
                                  reads=["wB"] + [(actk, k) for k in range(8)], writes=[bk])
                            S.op("dve", lambda e, bank=bank, dc=dc, b2t=b2t: e.tensor_scalar(
                                out=yT[:, dc, :], in0=bank[:, 0:TS], scalar1=1.0 / 1.702, scalar2=b2t[:, dc:dc + 1], op0=ALU.mult, op1=ALU.add),
                                reads=[bk, b2k], writes=[("ws0", dc)])
                        for s in range(2):
                            for half in range(2):
                                bank = pb[6 + half]
                                fns = [(lambda e, cc=cc, bank=bank, s=s, half=half: e.transpose(
                                    out=bank[:, cc * 128:(cc + 1) * 128], in_=yT[:, half * 4 + cc, s * 128:(s + 1) * 128], identity=ident[:]))
                                    for cc in range(4)]
                                S.ops("pe", fns, reads=[("ws0", half * 4 + cc) for cc in range(4)] + ["ident"], writes=[PB[6 + half]])
                                if half == 0:
                                    S.op("dve", lambda e, bank=bank, s=s: e.tensor_copy(out=ytok[s][:, 0:512], in_=bank[:]),
                                         reads=[PB[6 + half]], writes=[("ws1", s, 0)])
                                else:
                                    S.op("dve", lambda e, bank=bank, s=s: e.tensor_copy(out=ytok[s][:, 512:1024], in_=bank[:]),
                                         reads=[PB[6 + half]], writes=[("ws1", s, 1)])
                            row = i * TS + s * 128
                            ys_ev[s] = S.dma("sp", lambda e, s=s, row=row: e.dma_start(out=ys_d[row:row + 128, :], in_=ytok[s]),
                                             "yst%d" % s, reads=[("ws1", s, 0), ("ws1", s, 1)], writes=[("YS", i, s)])
                    S.state["YS"] = {"w": list(ys_ev.values()), "r": []}
                    S.barrier()
                    for kk in ("ws0", "ws1", "ws2"):
                        evs = []
                        for key, stt in S.state.items():
                            if isinstance(key, tuple) and key[0] == kk:
                                evs += stt["w"] + stt["r"]
                        base = S.state.setdefault(kk, {"w": [], "r": []})
                        base["r"] = base["r"] + evs
                with contextlib.ExitStack() as ph:
                    al = lambda n, s, d: sb(n, s, d, ph)
                    ykr = Rot(al, "c_yk", 8, [128, D], F32)
                    accr = Rot(al, "c_acc", 2, [128, D], F32)
                    gt2 = lambda cch: mod_ap(l, 5, cch, b)
                    for t in range(NT):
                        yks = []
                        for k in range(4):
                            yk, ykk = ykr.get()
                            S.dma("pool", lambda e, yk=yk, t=t, k=k: e.indirect_dma_start(
                                out=yk[:, :], out_offset=None, in_=ys_d[:, :],
                                in_offset=bass.IndirectOffsetOnAxis(ap=slotu[:, 4 * t + k:4 * t + k + 1], axis=0)),
                                "g_" + ykk, reads=["YS", "slotu"], writes=[ykk])
                            yks.append((yk, ykk))
                        acc, acck = accr.get()
                        S.op("dve", lambda e, acc=acc, yk=yks[0][0], t=t: e.tensor_scalar(
                            out=acc[:], in0=yk[:], scalar1=gate4[:, t, 0:1], scalar2=None, op0=ALU.mult),
                            reads=[yks[0][1], ("g4", t)], writes=[acck])
                        for k in range(1, 4):
                            S.op("dve", lambda e, acc=acc, yk=yks[k][0], t=t, k=k: e.scalar_tensor_tensor(
                                out=acc[:], in0=yk[:], scalar=gate4[:, t, k:k + 1], in1=acc[:], op0=ALU.mult, op1=ALU.add),
                                reads=[yks[k][1], ("g4", t), acck], writes=[acck])
                        for half in range(2):
                            bank = pb[(2 * t + half) % 4]
                            bk = PB[(2 * t + half) % 4]
                            fns = [(lambda e, cc=cc, bank=bank, acc=acc, half=half: e.transpose(
                                out=bank[:, cc * 128:(cc + 1) * 128], in_=acc[:, (half * 4 + cc) * 128:(half * 4 + cc + 1) * 128],
                                identity=ident[:])) for cc in range(4)]
                            S.ops("pe", fns, reads=[acck, "ident"], writes=[bk])
                            for cc in range(4):
                                cch = half * 4 + cc
                                S.op("dve", lambda e, bank=bank, cc=cc, cch=cch, t=t: e.scalar_tensor_tensor(
                                    out=xT[:, cch, t * 128:(t + 1) * 128], in0=bank[:, cc * 128:(cc + 1) * 128], scalar=gt2(cch),
                                    in1=xT[:, cch, t * 128:(t + 1) * 128], op0=ALU.mult, op1=ALU.add),
                                    reads=[bk, ("x", cch, t // 4), ("modT", l, 40 + cch)], writes=[("x", cch, t // 4)])
                    S.barrier()

        for b in range(NB):
            load_x(b)
            done = (stop == ("load", 0))
            for l in range(NL):
                if done:
                    break
                mixer(b, l)
                if stop == ("mixer", l):
                    done = True
                    break
                moe(b, l)
                if stop == ("moe", l):
                    done = True
                    break
            if done:
                store_x(b, False)
                break
            store_x(b, stop is None)
        S.finish([("out", "f_o0"), ("out", "f_o1")])
        build.stats = dict(ticks=dict(S.tick), waits=S.n_wait, ops=S.n_ops, nsem=len(S.sems))
    return nc


def _rope_tables():
    freqs = (np.float32(10000.0) ** (-np.arange(16, dtype=np.float32) / np.float32(16))).astype(np.float32)
    tok = np.arange(SEQ)
    row = (tok // 64).astype(np.float32)
    col = (tok % 64).astype(np.float32)
    cosT = np.zeros((128, SEQ), np.float32)
    sinT = np.zeros((128, SEQ), np.float32)
    for p in range(128):
        d = p % 64
        ang = (row if d < 32 else col) * freqs[d % 16]
        cosT[p] = np.cos(ang.astype(np.float32))
        sinT[p] = np.sin(ang.astype(np.float32))
    pm = np.zeros((128, 128), np.float32)
    for m in range(128):
        i = m % 32
        if i < 16:
            pm[m + 16, m] = -1.0
        else:
            pm[m - 16, m] = 1.0
    return cosT, sinT, pm


def _prep_shared(inp):
    f = lambda a: np.ascontiguousarray(np.asarray(a, dtype=np.float32))
    w_in = f(inp["w_in"])
    q = w_in[:, :, 0:512]
    k0 = w_in[:, :, 512:576]
    k1 = w_in[:, :, 576:640]
    v = w_in[:, :, 640:768]
    ca = w_in[:, :, 768:1280]
    cg = w_in[:, :, 1280:1792]
    parts = [q, k0, k0, k1, k1, v]
    for c in range(4):
        parts += [ca[:, :, c * 128:(c + 1) * 128], cg[:, :, c * 128:(c + 1) * 128]]
    w_in_r = np.ascontiguousarray(np.concatenate(parts, axis=2))
    assert w_in_r.shape[2] == WIN

    def fm(a, nch):
        a = f(a)
        return np.ascontiguousarray(a.reshape(a.shape[0], nch, 128).transpose(0, 2, 1))

    cosT, sinT, pm = _rope_tables()
    sh = dict(
        w_mod=f(inp["w_mod"]), b_modT=fm(inp["b_mod"], 48), g_mixT=fm(inp["g_mix"], 8), g_ffnT=fm(inp["g_ffn"], 8),
        g_finT=fm(f(inp["g_final"])[None], 8)[0], w_in_r=w_in_r,
        gq2=np.ascontiguousarray(np.tile(f(inp["g_q"]), (1, 2))[:, :, None]),
        gk2=np.ascontiguousarray(np.tile(f(inp["g_k"]), (1, 2))[:, :, None]),
        wdwT=np.ascontiguousarray(f(inp["w_dw"]).reshape(2, 31, 4, 128).transpose(0, 3, 2, 1).reshape(2, 128, 124)),
        b_dwT=fm(inp["b_dw"], 4), g_cnT=fm(inp["g_cn"], 4), b_cnT=fm(inp["b_cn"], 4),
        w_out=f(inp["w_out"]), w_router=f(inp["w_router"]), b_router=f(inp["b_router"]),
        w1=f(inp["w1"]),
        b1R=np.ascontiguousarray(f(inp["b1"]).reshape(2, NE, 16, 128).transpose(0, 1, 3, 2).reshape(2, NE * 128, 16)),
        w2=f(inp["w2"]),
        b2R=np.ascontiguousarray(f(inp["b2"]).reshape(2, NE, 8, 128).transpose(0, 1, 3, 2).reshape(2, NE * 128, 8)),
        cosT=cosT, sinT=sinT, pmat=pm,
    )
    return sh


def kernel(**inputs):
    n = 8
    NB = 4
    sh = _prep_shared(inputs)
    x = np.asarray(inputs["x"], dtype=np.float32)
    c = np.asarray(inputs["c"], dtype=np.float32)
    nc = build(NB=NB, NL=2)
    in_maps = []
    for i in range(n):
        m = dict(sh)
        m["x"] = np.ascontiguousarray(x[i * NB:(i + 1) * NB])
        cc = c[i * NB:(i + 1) * NB]
        m["cT"] = np.ascontiguousarray(cc.reshape(NB, 8, 128).transpose(2, 1, 0))
        in_maps.append(m)
    res = run_bass_kernel_spmd(nc, in_maps, core_ids=list(range(n)))
    return np.concatenate([np.asarray(r["out"]) for r in res.results], axis=0).astype(np.float32)
```

```python
import contextlib
import numpy as np
import concourse.bass as bass
import concourse.mybir as mybir
from concourse.bass_utils import run_bass_kernel_spmd

F32 = mybir.dt.float32
BF16 = mybir.dt.bfloat16
AF = mybir.ActivationFunctionType
ALU = mybir.AluOpType

D = 1024
SEQ = 2048
NBLK = 4
NT = 16
NE = 32
EPS = 1e-6
WIN = 1920


class Sched:
    ENG = ("pe", "dve", "act", "pool", "sp")
    LIMIT = 30000

    def __init__(self, nc, stack):
        self.nc = nc
        self.stack = stack
        self.engs = {"pe": nc.tensor, "dve": nc.vector, "act": nc.scalar,
                     "pool": nc.gpsimd, "sp": nc.sync}
        self.tick_sem = {e: stack.enter_context(nc.semaphore("tk_" + e)) for e in self.ENG}
        self.tick = {e: 0 for e in self.ENG}
        self.tick_sid = {e: "tk_" + e for e in self.ENG}
        self.epoch = {}
        self.seen = {e: {} for e in self.ENG}
        self.sems = {}
        for e in self.ENG:
            self.sems["tk_" + e] = self.tick_sem[e]
        self.dma_sem = {}
        self.dma_gen = 0
        self.state = {}
        self.n_wait = 0
        self.n_ops = 0

    def _need(self, e, ev):
        sid, val = ev
        if self.seen[e].get(sid, 0) >= val:
            return
        self.seen[e][sid] = val
        self.engs[e].wait_ge(self.sems[sid], val)
        self.n_wait += 1

    def _deps(self, e, reads, writes):
        for k in reads:
            st = self.state.get(k)
            if st:
                for ev in st["w"]:
                    self._need(e, ev)
        for k in writes:
            st = self.state.get(k)
            if st:
                for ev in st["w"]:
                    self._need(e, ev)
                for ev in st["r"]:
                    self._need(e, ev)

    def _record(self, ev, reads, writes):
        for k in writes:
            self.state[k] = {"w": [ev], "r": []}
        for k in reads:
            st = self.state.setdefault(k, {"w": [], "r": []})
            st["r"].append(ev)
            if len(st["r"]) > 16:
                best = {}
                for s, v in st["r"]:
                    if best.get(s, 0) < v:
                        best[s] = v
                st["r"] = list(best.items())

    def _roll(self, e):
        if self.tick[e] >= self.LIMIT:
            self.epoch[e] = self.epoch.get(e, 0) + 1
            sid = "tk_%s_%d" % (e, self.epoch[e])
            self.sems[sid] = self.stack.enter_context(self.nc.semaphore(sid))
            self.tick_sem[e] = self.sems[sid]
            self.tick_sid[e] = sid
            self.tick[e] = 0

    def barrier(self):
        cur = [(self.tick_sid[e], self.tick[e]) for e in self.ENG if self.tick[e] > 0]
        for e in self.ENG:
            for ev in cur:
                self._need(e, ev)
        for k, st in self.state.items():
            st["w"] = [ev for ev in st["w"] if not ev[0].startswith("tk_")]
            st["r"] = [ev for ev in st["r"] if not ev[0].startswith("tk_")]

    def op(self, e, fn, reads=(), writes=()):
        self._deps(e, reads, writes)
        self._roll(e)
        self.tick[e] += 1
        ev = (self.tick_sid[e], self.tick[e])
        fn(self.engs[e]).then_inc(self.tick_sem[e], 1)
        self._record(ev, reads, writes)
        self.n_ops += 1
        return ev

    def ops(self, e, fns, reads=(), writes=()):
        self._deps(e, reads, writes)
        self._roll(e)
        self.tick[e] += 1
        ev = (self.tick_sid[e], self.tick[e])
        for fn in fns[:-1]:
            fn(self.engs[e])
        fns[-1](self.engs[e]).then_inc(self.tick_sem[e], 1)
        self._record(ev, reads, writes)
        self.n_ops += len(fns)
        return ev

    def dma(self, q, fn, key, reads=(), writes=()):
        self._deps(q, reads, writes)
        if key not in self.dma_sem:
            sid = "dma_" + str(key)
            self.sems[sid] = self.stack.enter_context(self.nc.semaphore(sid))
            self.dma_sem[key] = [sid, 0]
        sid, cnt = self.dma_sem[key]
        if cnt >= self.LIMIT:
            self.dma_gen += 1
            sid = "dma_%s_g%d" % (key, self.dma_gen)
            self.sems[sid] = self.stack.enter_context(self.nc.semaphore(sid))
            self.dma_sem[key] = [sid, 0]
            cnt = 0
        r = fn(self.engs[q])
        if not isinstance(r, (list, tuple)):
            r = [r]
        for ins in r:
            ins.then_inc(self.sems[sid], 16)
        cnt += 16 * len(r)
        self.dma_sem[key][1] = cnt
        ev = (sid, cnt)
        self._record(ev, reads, writes)
        return ev

    def finish(self, keys):
        for k in keys:
            st = self.state.get(k)
            if st:
                for ev in st["w"] + st["r"]:
                    self._need("sp", ev)


class Rot:
    def __init__(self, alloc, name, n, shape, dt):
        self.t = [alloc(name + str(i), shape, dt) for i in range(n)]
        self.k = [name + str(i) for i in range(n)]
        self.i = 0

    def get(self):
        j = self.i % len(self.t)
        self.i += 1
        return self.t[j], self.k[j]


def _plan(NB, NL, NEXP):
    plan = []
    for l in range(NL):
        for j in range(12):
            plan.append(("mod", l, j))
    for b in range(NB):
        for l in range(NL):
            plan.append(("q", l))
            plan.append(("kkv", l))
            for c in range(4):
                plan.append(("conv", l, c))
            for dh in range(2):
                plan.append(("o", l, dh))
    return plan


def build(NB=4, NL=2, NEXP=NE, stop=None):
    nc = bass.Bass("TRN2", target_bir_lowering=False)

    def din(name, shape, dt=F32):
        return nc.dram_tensor(name, list(shape), dt, kind="ExternalInput").ap()

    x_d = din("x", [NB, SEQ, D])
    cT_d = din("cT", [128, 8, NB])
    wmod_d = din("w_mod", [2, D, 6 * D])
    bmodT_d = din("b_modT", [2, 128, 48])
    gmixT_d = din("g_mixT", [2, 128, 8])
    gffnT_d = din("g_ffnT", [2, 128, 8])
    gfinT_d = din("g_finT", [128, 8])
    win_d = din("w_in_r", [2, D, WIN])
    gq2_d = din("gq2", [2, 128, 1])
    gk2_d = din("gk2", [2, 128, 1])
    wdwT_d = din("wdwT", [2, 128, 4 * 31])
    bdwT_d = din("b_dwT", [2, 128, 4])
    gcnT_d = din("g_cnT", [2, 128, 4])
    bcnT_d = din("b_cnT", [2, 128, 4])
    wout_d = din("w_out", [2, D, D])
    wr_d = din("w_router", [2, D, NE])
    br_d = din("b_router", [2, NE])
    w1_d = din("w1", [2, NEXP, D, 2 * D])
    b1R_d = din("b1R", [2, NE * 128, 16])
    b2R_d = din("b2R", [2, NE * 128, 8])
    hs_d = nc.dram_tensor("hs_scratch", [64 * 256, D], BF16, kind="Internal").ap()
    ys_d = nc.dram_tensor("ys_scratch", [64 * 256, D], F32, kind="Internal").ap()
    w1b_d = nc.dram_tensor("w1_bf16", [2 * NE * D, 2 * D], BF16, kind="Internal").ap()
    w2b_d = nc.dram_tensor("w2_bf16", [2 * NE * D, D], BF16, kind="Internal").ap()
    w2_d = din("w2", [2, NEXP, D, D])
    cos_d = din("cosT", [128, SEQ])
    sin_d = din("sinT", [128, SEQ])
    pm_d = din("pmat", [128, 128])
    out_d = nc.dram_tensor("out", [NB, SEQ, D], F32, kind="ExternalOutput").ap()

    with contextlib.ExitStack() as st:
        S = Sched(nc, st)

        uniq = [0]

        def sb(name, shape, dt, stack=st):
            uniq[0] += 1
            return stack.enter_context(nc.sbuf_tensor("%s_s%d" % (name, uniq[0]), list(shape), dt))

        xT = sb("xT", [128, 8, SEQ], F32)
        A = sb("A", [128, 8, SEQ], BF16)
        wslot = [sb("wslot%d" % i, [128, 8, 512], BF16) for i in range(3)]
        cosT = sb("cos", [128, SEQ], BF16)
        sinT = sb("sin", [128, SEQ], BF16)
        pmat = sb("pmat_s", [128, 128], BF16)
        ident = sb("ident", [128, 128], F32)
        identb = sb("identb", [128, 128], BF16)
        onesb = sb("onesb", [128, 128], BF16)
        onesbd = sb("onesbd", [128, 128], BF16)
        ones32 = sb("ones32", [128, 128], F32)
        cvec = sb("cvec", [128, 8], F32)
        cact = sb("cact", [128, 8, NB], F32)
        cactb = sb("cactb", [128, 8, NB], BF16)
        modT = [sb("modT%d" % l, [128, 48, NB], F32) for l in range(NL)]
        bmodT = [sb("bmodT%d" % l, [128, 48], F32) for l in range(NL)]
        gmixT = [sb("gmixT%d" % l, [128, 8], F32) for l in range(NL)]
        gffnT = [sb("gffnT%d" % l, [128, 8], F32) for l in range(NL)]
        gfinT = sb("gfinT", [128, 8], F32)
        gq2 = [sb("gq2_%d" % l, [128, 1], F32) for l in range(NL)]
        gk2 = [sb("gk2_%d" % l, [128, 1], F32) for l in range(NL)]
        wdwT = [sb("wdwT%d" % l, [128, 4 * 31], F32) for l in range(NL)]
        bdwT = [sb("bdwT%d" % l, [128, 4], F32) for l in range(NL)]
        gcnT = [sb("gcnT%d" % l, [128, 4], F32) for l in range(NL)]
        bcnT = [sb("bcnT%d" % l, [128, 4], F32) for l in range(NL)]
        wr32 = [sb("wr32_%d" % l, [128, 8, NE], F32) for l in range(NL)]
        brow = [sb("brow%d" % l, [1, NE], F32) for l in range(NL)]
        utri = sb("utri", [128, 128], BF16)
        utri32 = sb("utri32", [128, 128], F32)
        pcol = sb("pcol", [128, 1], F32)
        bigcol = sb("bigcol", [128, 1], F32)
        basepc = sb("basepc", [128, 8], F32)
        ipc = sb("ipc", [128, 8], mybir.dt.int32)
        gmod = sb("gmod", [128, 8], F32)
        pb = [st.enter_context(nc.psum_tensor("pb%d" % i, [128, 512], F32)) for i in range(8)]
        PB = ["pb%d" % i for i in range(8)]

        const_keys = []

        def cload(q, dst, src, key):
            S.dma(q, lambda e: e.dma_start(out=dst, in_=src), "const", writes=[key])
            const_keys.append(key)

        cload("pool", cosT[:], cos_d, "cos")
        cload("pool", sinT[:], sin_d, "sin")
        cload("pool", pmat[:], pm_d, "pmat")
        cload("sp", cact[:], cT_d, "cact")
        cload("sp", gfinT[:], gfinT_d, "gfinT")
        for l in range(NL):
            cload("sp", bmodT[l][:], bmodT_d[l], "bmodT%d" % l)
            cload("sp", gmixT[l][:], gmixT_d[l], "gmixT%d" % l)
            cload("sp", gffnT[l][:], gffnT_d[l], "gffnT%d" % l)
            cload("sp", gq2[l][:], gq2_d[l], "gq2%d" % l)
            cload("sp", gk2[l][:], gk2_d[l], "gk2%d" % l)
            cload("sp", wdwT[l][:], wdwT_d[l], "wdwT%d" % l)
            cload("sp", bdwT[l][:], bdwT_d[l], "bdwT%d" % l)
            cload("sp", gcnT[l][:], gcnT_d[l], "gcnT%d" % l)
            cload("sp", bcnT[l][:], bcnT_d[l], "bcnT%d" % l)
            cload("sp", wr32[l][:], wr_d[l].rearrange("(c p) n -> p c n", p=128), "wr%d" % l)
            cload("sp", brow[l][:], br_d[l:l + 1, :], "brow%d" % l)
        fin = (S.dma_sem["const"][0], S.dma_sem["const"][1])
        for k in const_keys:
            S.state[k] = {"w": [fin], "r": []}

        S.op("pool", lambda e: e.memset(ident[:], 0.0), writes=["ident"])
        S.op("pool", lambda e: e.affine_select(out=ident[:], in_=ident[:], pattern=[[-1, 128]],
                                               compare_op=ALU.not_equal, fill=1.0, base=0,
                                               channel_multiplier=1),
             reads=["ident"], writes=["ident"])
        S.op("dve", lambda e: e.tensor_copy(out=identb[:], in_=ident[:]), reads=["ident"], writes=["identb"])
        S.op("pool", lambda e: e.memset(utri32[:], 1.0), writes=["utri32"])
        S.op("pool", lambda e: e.affine_select(out=utri32[:], in_=utri32[:], pattern=[[1, 128]],
                                               compare_op=ALU.is_gt, fill=0.0, base=0, channel_multiplier=-1),
             reads=["utri32"], writes=["utri32"])
        S.op("dve", lambda e: e.tensor_copy(out=utri[:], in_=utri32[:]), reads=["utri32"], writes=["utri"])
        S.op("pool", lambda e: e.iota(ipc[:], pattern=[[128, 8]], base=0, channel_multiplier=1), writes=["ipc"])
        S.op("dve", lambda e: e.tensor_copy(out=basepc[:], in_=ipc[:]), reads=["ipc"], writes=["basepc"])
        S.op("dve", lambda e: e.tensor_copy(out=pcol[:], in_=ipc[:, 0:1]), reads=["ipc"], writes=["pcol"])
        S.op("dve", lambda e: e.tensor_scalar(out=bigcol[:], in0=pcol[:], scalar1=0.5, scalar2=1048576.0, op0=ALU.is_gt, op1=ALU.mult),
             reads=["pcol"], writes=["bigcol"])
        S.op("dve", lambda e: e.memset(onesb[:], 1.0), writes=["onesb"])
        S.op("dve", lambda e: e.memset(ones32[:], 1.0), writes=["ones32"])
        S.op("dve", lambda e: e.memset(onesbd[:], 0.0), writes=["onesbd"])
        S.op("dve", lambda e: e.memset(onesbd[0:64, 0:64], 1.0), reads=["onesbd"], writes=["onesbd"])
        S.op("dve", lambda e: e.memset(onesbd[64:128, 64:128], 1.0), reads=["onesbd"], writes=["onesbd"])
        S.op("dve", lambda e: e.memset(cvec[:, 0:1], EPS), writes=["cvec"])
        S.op("dve", lambda e: e.memset(cvec[:, 1:2], 64.0 * EPS), reads=["cvec"], writes=["cvec"])
        S.op("dve", lambda e: e.memset(cvec[:, 2:3], 0.0), reads=["cvec"], writes=["cvec"])
        eps_c = cvec[:, 0:1]
        eps64_c = cvec[:, 1:2]
        zero_c = cvec[:, 2:3]
        S.op("act", lambda e: e.activation(out=cactb[:], in_=cact[:], func=AF.Silu),
             reads=["cact"], writes=["cactb"])
        for l in range(NL):
            S.op("dve", lambda e, l=l: e.tensor_scalar(out=gk2[l][:], in0=gk2[l][:], scalar1=8.0,
                                                       scalar2=None, op0=ALU.mult),
                 reads=["gk2%d" % l], writes=["gk2%d" % l])

        plan = _plan(NB, NL, NEXP)
        wstate = {"next": 0, "cons": 0}
        pgrp = []
        g = 0
        for idx_, d_ in enumerate(plan):
            if d_[0] == "q":
                g += 1
            pgrp.append(g)

        def piece_dma(desc, slot):
            kind = desc[0]
            l = desc[1]
            if kind == "mod":
                j = desc[2]
                src = wmod_d[l].rearrange("(c p) n -> p c n", p=128)[:, :, j * 512:(j + 1) * 512]
                dst = slot[:, :, 0:512]
            elif kind == "conv":
                c = desc[2]
                src = win_d[l].rearrange("(c p) n -> p c n", p=128)[:, :, 896 + 256 * c:896 + 256 * (c + 1)]
                dst = slot[:, :, 0:256]
            elif kind == "q":
                src = win_d[l].rearrange("(c p) n -> p c n", p=128)[:, :, 0:512]
                dst = slot[:, :, 0:512]
            elif kind == "kkv":
                src = win_d[l].rearrange("(c p) n -> p c n", p=128)[:, :, 512:896]
                dst = slot[:, :, 0:384]
            elif kind == "o":
                dh = desc[2]
                src = wout_d[l].rearrange("(c p) n -> p c n", p=128)[:, :, dh * 512:(dh + 1) * 512]
                dst = slot[:, :, 0:512]
            elif kind == "w1":
                e, pg = desc[2], desc[3]
                v = w1_d[l, e].rearrange("(c p) n -> p c n", p=128)
                src = [v[:, :, 256 * pg:256 * (pg + 1)], v[:, :, 1024 + 256 * pg:1024 + 256 * (pg + 1)]]
                dst = [slot[:, :, 0:256], slot[:, :, 256:512]]
            elif kind == "w2":
                e, dh = desc[2], desc[3]
                src = w2_d[l, e].rearrange("(c p) n -> p c n", p=128)[:, :, dh * 512:(dh + 1) * 512]
                dst = slot[:, :, 0:512]
            if not isinstance(dst, list):
                dst, src = [dst], [src]
            return dst, src

        def wget(desc):
            i = wstate["cons"]
            assert plan[i] == desc, (plan[i], desc)
            while wstate["next"] < len(plan) and wstate["next"] <= i + 2 and pgrp[wstate["next"]] == pgrp[i]:
                j = wstate["next"]
                dst, src = piece_dma(plan[j], wslot[j % 3])
                S.dma("pool", lambda e, dst=dst, src=src: [e.dma_start(out=d_, in_=s_) for d_, s_ in zip(dst, src)],
                      "ws%d" % (j % 3), writes=["ws%d" % (j % 3)])
                wstate["next"] += 1
            wstate["cons"] += 1
            return wslot[i % 3], "ws%d" % (i % 3)

        def wprefetch():
            i = wstate["cons"]
            while wstate["next"] < len(plan) and wstate["next"] <= i + 2 and pgrp[wstate["next"]] == pgrp[max(i - 1, 0)]:
                j = wstate["next"]
                dst, src = piece_dma(plan[j], wslot[j % 3])
                S.dma("pool", lambda e, dst=dst, src=src: [e.dma_start(out=d_, in_=s_) for d_, s_ in zip(dst, src)],
                      "ws%d" % (j % 3), writes=["ws%d" % (j % 3)])
                wstate["next"] += 1

        def blk(j):
            return slice(j * 512, (j + 1) * 512)

        def mm(out, lhsT, rhs, start, stop):
            return lambda e: e.matmul(out, lhsT=lhsT, rhs=rhs, start=start, stop=stop)

        for l in range(NL):
            for j in range(12):
                wt, wk = wget(("mod", l, j))
                for jj in range(4):
                    col = j * 4 + jj
                    bank = pb[col % 2]
                    fns = [mm(bank[:, 0:NB], wt[:, k, jj * 128:(jj + 1) * 128], cactb[:, k, :], k == 0, k == 7)
                           for k in range(8)]
                    S.ops("pe", fns, reads=[wk, "cactb"], writes=[PB[col % 2]])
                    S.op("dve", lambda e, bank=bank, l=l, col=col: e.tensor_scalar(
                        out=modT[l][:, col, :], in0=bank[:, 0:NB], scalar1=bmodT[l][:, col:col + 1],
                        scalar2=None, op0=ALU.add),
                        reads=[PB[col % 2], "bmodT%d" % l], writes=[("modT", l, col)])
                wprefetch()
        S.barrier()

        def mod_ap(l, which, c, b):
            return modT[l][:, which * 8 + c, b:b + 1]

        def norm_stats(stk, j, sqr, sdr, scale):
            sq, sqk = sqr.get()
            S.op("act", lambda e: e.activation(out=sq[:], in_=xT[:, :, blk(j)], func=AF.Square),
                 reads=[("x", c, j) for c in range(8)], writes=[sqk])
            bank = pb[j % 2]
            S.ops("pe", [mm(bank[:], onesb[:], sq[:, k, :], k == 0, k == 7) for k in range(8)],
                  reads=[sqk, "onesb"], writes=[PB[j % 2]])
            sd, sdk = sdr.get()
            S.op("act", lambda e: e.activation(out=sd[:], in_=bank[:], func=AF.Ln, bias=eps_c, scale=scale),
                 reads=[PB[j % 2], "cvec"], writes=[sdk])
            S.op("act", lambda e: e.activation(out=sd[:], in_=sd[:], func=AF.Exp, scale=-0.5), reads=[sdk], writes=[sdk])
            return sd, sdk

        def load_x(b):
            with contextlib.ExitStack() as ph:
                xin = Rot(lambda n, s, d: sb(n, s, d, ph), "xin", 2, [128, D], F32)
                for t in range(NT):
                    xi, xk = xin.get()
                    S.dma("sp", lambda e, xi=xi, t=t: e.dma_start(out=xi[:], in_=x_d[b, t * 128:(t + 1) * 128, :]),
                          xk, writes=[xk])
                    for half in range(2):
                        bi = (2 * t + half) % 4
                        bank = pb[bi]
                        fns = [(lambda e, cc=cc, bank=bank, xi=xi, half=half: e.transpose(
                            out=bank[:, cc * 128:(cc + 1) * 128],
                            in_=xi[:, (half * 4 + cc) * 128:(half * 4 + cc + 1) * 128], identity=ident[:]))
                            for cc in range(4)]
                        S.ops("pe", fns, reads=[xk, "ident"], writes=[PB[bi]])
                        eng = "dve" if half == 0 else "act"
                        dst = xT[:, half * 4:half * 4 + 4, t * 128:(t + 1) * 128]
                        src = bank[:].rearrange("p (c t) -> p c t", c=4)
                        wr = [("x", half * 4 + cc, t // 4) for cc in range(4)]
                        if eng == "dve":
                            S.op("dve", lambda e, dst=dst, src=src: e.tensor_copy(out=dst, in_=src),
                                 reads=[PB[bi]], writes=wr)
                        else:
                            S.op("act", lambda e, dst=dst, src=src: e.copy(out=dst, in_=src),
                                 reads=[PB[bi]], writes=wr)
                S.barrier()

        def store_x(b, do_norm):
            with contextlib.ExitStack() as ph:
                al = lambda n, s, d: sb(n, s, d, ph)
                sqr = Rot(al, "f_sq", 2, [128, 8, 512], BF16)
                sdr = Rot(al, "f_sd", 2, [128, 512], F32)
                y32 = Rot(al, "f_y", 1, [128, 8, 512], F32)
                ost = Rot(al, "f_o", 2, [128, D], F32)
                for j in range(NBLK):
                    y, yk = y32.get()
                    if do_norm:
                        rs, rsk = norm_stats(ph, j, sqr, sdr, 1.0 / D)
                        for c in range(8):
                            S.op("dve", lambda e, c=c, y=y, rs=rs: e.scalar_tensor_tensor(
                                out=y[:, c, :], in0=xT[:, c, blk(j)], scalar=gfinT[:, c:c + 1], in1=rs[:],
                                op0=ALU.mult, op1=ALU.mult),
                                reads=[("x", c, j), rsk, "gfinT"], writes=[(yk, c)])
                    else:
                        for c in range(8):
                            S.op("dve", lambda e, c=c, y=y: e.tensor_copy(out=y[:, c, :], in_=xT[:, c, blk(j)]),
                                 reads=[("x", c, j)], writes=[(yk, c)])
                    for t in range(4):
                        o, ok = ost.get()
                        for half in range(2):
                            bi = (2 * t + half) % 4
                            bank = pb[bi]
                            fns = [(lambda e, cc=cc, bank=bank, y=y, half=half, t=t: e.transpose(
                                out=bank[:, cc * 128:(cc + 1) * 128],
                                in_=y[:, half * 4 + cc, t * 128:(t + 1) * 128], identity=ident[:]))
                                for cc in range(4)]
                            S.ops("pe", fns, reads=[(yk, half * 4 + cc) for cc in range(4)] + ["ident"],
                                  writes=[PB[bi]])
                            if half == 0:
                                S.op("dve", lambda e, o=o, bank=bank: e.tensor_copy(out=o[:, 0:512], in_=bank[:]),
                                     reads=[PB[bi]], writes=[(ok, 0)])
                            else:
                                S.op("act", lambda e, o=o, bank=bank: e.copy(out=o[:, 512:1024], in_=bank[:]),
                                     reads=[PB[bi]], writes=[(ok, 1)])
                        row = (j * 4 + t) * 128
                        S.dma("sp", lambda e, o=o, row=row: e.dma_start(out=out_d[b, row:row + 128, :], in_=o[:]),
                              "st_" + ok, reads=[(ok, 0), (ok, 1)], writes=[("out", ok)])
                S.barrier()

        def norm_mod(b, l, sub, ph, h32r=None, router=None):
            al = lambda n, s, d: sb(n, s, d, ph)
            sqr = Rot(al, "n_sq", 2, [128, 8, 512], BF16)
            sdr = Rot(al, "n_sd", 2, [128, 512], F32)
            h32r = Rot(al, "n_h", 1, [128, 8, 512], F32)
            gsrc = gmixT[l] if sub == 0 else gffnT[l]
            gkey = ("gmixT%d" if sub == 0 else "gffnT%d") % l
            S.op("dve", lambda e: e.scalar_tensor_tensor(
                out=gmod[:], in0=modT[l][:, (1 + 3 * sub) * 8:(2 + 3 * sub) * 8, b], scalar=1.0, in1=gsrc[:],
                op0=ALU.add, op1=ALU.mult),
                reads=[("modT", l, (1 + 3 * sub) * 8 + c) for c in range(8)] + [gkey], writes=["gmod"])
            for j in range(NBLK):
                rs, rsk = norm_stats(ph, j, sqr, sdr, 1.0 / D)
                h, hk = h32r.get()
                for c in range(8):
                    S.op("dve", lambda e, c=c, h=h, rs=rs: e.scalar_tensor_tensor(
                        out=h[:, c, :], in0=xT[:, c, blk(j)], scalar=gmod[:, c:c + 1], in1=rs[:],
                        op0=ALU.mult, op1=ALU.mult),
                        reads=[("x", c, j), rsk, "gmod"], writes=[(hk, c)])
                    sh = mod_ap(l, 3 * sub, c, b)
                    if router is None:
                        S.op("act", lambda e, c=c, h=h, sh=sh: e.activation(
                            out=A[:, c, blk(j)], in_=h[:, c, :], func=AF.Identity, bias=sh, scale=1.0),
                            reads=[(hk, c), ("modT", l, 3 * sub * 8 + c)], writes=[("A", c, j)])
                    else:
                        S.op("act", lambda e, c=c, h=h, sh=sh: e.activation(
                            out=h[:, c, :], in_=h[:, c, :], func=AF.Identity, bias=sh, scale=1.0),
                            reads=[(hk, c), ("modT", l, 3 * sub * 8 + c)], writes=[(hk, c)])
                        S.op("dve", lambda e, c=c, h=h: e.tensor_copy(out=A[:, c, blk(j)], in_=h[:, c, :]),
                             reads=[(hk, c)], writes=[("A", c, j)])
                if router is not None:
                    router(j, h, hk)

        def mixer(b, l):
            with contextlib.ExitStack() as ph:
                norm_mod(b, l, 0, ph)
                S.barrier()
            with contextlib.ExitStack() as ph_q:
                qT = sb("qT", [128, 4, SEQ], BF16, ph_q)
                with contextlib.ExitStack() as ph_kv:
                    al0 = lambda n, s, d: sb(n, s, d, ph_kv)
                    kz = al0("kz", [128, 4, SEQ], BF16)
                    S.op("dve", lambda e: e.memset(kz[:], 0.0), writes=[("k", kc_, j_) for kc_ in range(2) for j_ in range(NBLK)])
                    v1 = al0("v1", [128, NT, 320], BF16)
                    S.op("dve", lambda e: e.memset(v1[:], 1.0), writes=[("v1", t) for t in range(NT)])
                    with contextlib.ExitStack() as ph:
                        al = lambda n, s, d: sb(n, s, d, ph)
                        sqr = Rot(al, "p_sq", 2, [128, 512], BF16)
                        sdr = Rot(al, "p_sd", 2, [128, 512], F32)
                        qnr = Rot(al, "p_qn", 2, [128, 512], BF16)
                        ar = Rot(al, "p_a", 2, [128, 512], F32)
                        br = Rot(al, "p_b", 2, [128, 512], F32)
                        cnt = [0]

                        def stageA(cs):
                            wt, wk, col0, j = cs["wt"], cs["wk"], cs["col0"], cs["j"]
                            i = cs["i"]
                            pq, kq = pb[i % 2], PB[i % 2]
                            S.ops("pe", [mm(pq[:], wt[:, k, col0:col0 + 128], A[:, k, blk(j)], k == 0, k == 7) for k in range(8)],
                                  reads=[wk] + [("A", k, j) for k in range(8)], writes=[kq])
                            sq, sqk = sqr.get()
                            S.op("act", lambda e: e.activation(out=sq[:], in_=pq[:], func=AF.Square), reads=[kq], writes=[sqk])
                            cs["sq"], cs["sqk"] = sq, sqk

                        def stageB(cs):
                            i = cs["i"]
                            pq, kq = pb[i % 2], PB[i % 2]
                            pss, kss = pb[2 + i % 2], PB[2 + i % 2]
                            sq, sqk = cs["sq"], cs["sqk"]
                            S.ops("pe", [mm(pss[:], onesbd[:], sq[:], True, True)], reads=[sqk, "onesbd"], writes=[kss])
                            sd, sdk = sdr.get()
                            S.op("act", lambda e: e.activation(out=sd[:], in_=pss[:], func=AF.Ln, bias=eps64_c, scale=1.0),
                                 reads=[kss, "cvec"], writes=[sdk])
                            S.op("act", lambda e: e.activation(out=sd[:], in_=sd[:], func=AF.Exp, scale=-0.5), reads=[sdk], writes=[sdk])
                            qn, qnk = qnr.get()
                            gain, gkey = cs["gain"], cs["gkey"]
                            S.op("dve", lambda e: e.scalar_tensor_tensor(out=qn[:], in0=pq[:], scalar=gain[:, 0:1], in1=sd[:],
                                                                         op0=ALU.mult, op1=ALU.mult),
                                 reads=[kq, sdk, gkey], writes=[qnk])
                            cs["qn"], cs["qnk"] = qn, qnk

                        def stageC(cs):
                            i, j = cs["i"], cs["j"]
                            ppq, kpq = pb[4 + i % 2], PB[4 + i % 2]
                            qn, qnk = cs["qn"], cs["qnk"]
                            S.ops("pe", [mm(ppq[:], pmat[:], qn[:], True, True)], reads=[qnk, "pmat"], writes=[kpq])
                            a, ak = ar.get()
                            bb, bk = br.get()
                            S.op("dve", lambda e: e.tensor_tensor(out=a[:], in0=qn[:], in1=cosT[:, blk(j)], op=ALU.mult),
                                 reads=[qnk, "cos"], writes=[ak])
                            S.op("dve", lambda e: e.tensor_tensor(out=bb[:], in0=ppq[:], in1=sinT[:, blk(j)], op=ALU.mult),
                                 reads=[kpq, "sin"], writes=[bk])
                            if cs.get("kc") is None:
                                S.op("dve", lambda e: e.tensor_tensor(out=cs["dst"], in0=a[:], in1=bb[:], op=ALU.add),
                                     reads=[ak, bk], writes=[cs["dkey"]])
                            else:
                                kc_ = cs["kc"]
                                S.op("dve", lambda e: e.tensor_tensor(out=kz[0:64, 2 * kc_, blk(j)], in0=a[0:64, :], in1=bb[0:64, :], op=ALU.add),
                                     reads=[ak, bk, cs["dkey"]], writes=[cs["dkey"]])
                                S.op("dve", lambda e: e.tensor_tensor(out=kz[64:128, 2 * kc_ + 1, blk(j)], in0=a[64:128, :], in1=bb[64:128, :],
                                                                      op=ALU.add),
                                     reads=[ak, bk, cs["dkey"]], writes=[cs["dkey"]])

                        wtq, wkq = wget(("q", l))
                        wtk, wkk = None, None
                        chunks = []
                        for j in range(NBLK):
                            for c in range(4):
                                chunks.append(dict(wt=wtq, wk=wkq, col0=c * 128, j=j, dst=qT[:, c, blk(j)], dkey=("q", c, j),
                                                   gain=gq2[l], gkey="gq2%d" % l))
                        for j in range(NBLK):
                            for kc in range(2):
                                chunks.append(dict(wt=wtk, wk=wkk, col0=kc * 128, j=j, dst=None, kc=kc, dkey=("k", kc, j),
                                                   gain=gk2[l], gkey="gk2%d" % l))
                        for i_, cs in enumerate(chunks):
                            cs["i"] = i_
                        nch_ = len(chunks)
                        for step in range(nch_ + 2):
                            if step == 16:
                                wtk, wkk = wget(("kkv", l))
                                for cs in chunks[16:]:
                                    cs["wt"], cs["wk"] = wtk, wkk
                            if step < nch_:
                                stageA(chunks[step])
                            if 0 <= step - 1 < nch_:
                                stageB(chunks[step - 1])
                            if 0 <= step - 2 < nch_:
                                stageC(chunks[step - 2])
                        wt, wk = wtk, wkk
                        for j in range(NBLK):
                            for tt in range(4):
                                t = j * 4 + tt
                                bank = pb[6 + tt % 2]
                                S.ops("pe", [mm(bank[:, 0:128], A[:, k, t * 128:(t + 1) * 128], wt[:, k, 256:384], k == 0, k == 7)
                                             for k in range(8)],
                                      reads=[wk] + [("A", k, j) for k in range(8)], writes=[PB[6 + tt % 2]])
                                dst = v1[:, t, 64:320].rearrange("p (a b) -> p a b", b=128)[:, :, 0:64]
                                src = bank[:, 0:128].rearrange("p (a b) -> p a b", b=64)
                                S.op("act", lambda e, dst=dst, src=src: e.copy(out=dst, in_=src),
                                     reads=[PB[6 + tt % 2]], writes=[("v1", t)])
                        wprefetch()
                        S.barrier()
                    with contextlib.ExitStack() as ph:
                        al = lambda n, s, d: sb(n, s, d, ph)
                        ptr = Rot(al, "a_pt", 4, [128, 512], BF16)
                        recr = Rot(al, "a_rec", 2, [128, 512], F32)
                        seq = [(h, qj, kt) for h in range(8) for qj in range(NBLK) for kt in range(NT)]
                        pend = []

                        def score(idx):
                            h, qj, kt = seq[idx]
                            c, hp, kv = h // 2, h % 2, h // 4
                            bank = pb[idx % 3]
                            rows = slice(hp * 64, hp * 64 + 64)
                            S.ops("pe", [mm(bank[:], kz[:, 2 * kv + hp, kt * 128:(kt + 1) * 128], qT[:, c, blk(qj)], True, True)],
                                  reads=[("k", kv, kt // 4), ("q", c, qj)], writes=[PB[idx % 3]])
                            pt, ptk = ptr.get()
                            S.op("act", lambda e: e.activation(out=pt[:], in_=bank[:], func=AF.Exp),
                                 reads=[PB[idx % 3]], writes=[ptk])
                            return pt, ptk

                        def pv(idx, pt, ptk):
                            h, qj, kt = seq[idx]
                            c, hp, kv = h // 2, h % 2, h // 4
                            g = idx // NT
                            po = pb[4 + g % 2]
                            pok = PB[4 + g % 2]
                            col0 = 128 * kv + (64 if hp == 0 else 0)
                            S.ops("pe", [mm(po[:], v1[:, kt, col0:col0 + 128], pt[:], kt == 0, kt == NT - 1)],
                                  reads=[("v1", kt), ptk], writes=[pok])
                            if kt == NT - 1:
                                rec, reck = recr.get()
                                if hp == 0:
                                    S.op("dve", lambda e: e.reciprocal(out=rec[0:64, :], in_=po[64:128, :]), reads=[pok], writes=[reck])
                                    S.op("dve", lambda e: e.tensor_tensor(out=qT[0:64, c, blk(qj)], in0=po[0:64, :], in1=rec[0:64, :],
                                                                          op=ALU.mult),
                                         reads=[pok, reck], writes=[("q", c, qj)])
                                else:
                                    S.op("dve", lambda e: e.reciprocal(out=rec[64:128, :], in_=po[0:64, :]), reads=[pok], writes=[reck])
                                    S.op("dve", lambda e: e.tensor_tensor(out=qT[64:128, c, blk(qj)], in0=po[64:128, :], in1=rec[64:128, :],
                                                                          op=ALU.mult),
                                         reads=[pok, reck], writes=[("q", c, qj)])

                        n = len(seq)
                        look = 2
                        for i in range(n + look):
                            if i < n:
                                pend.append((i,) + score(i))
                            if i >= look:
                                idx, pt, ptk = pend.pop(0)
                                pv(idx, pt, ptk)
                        S.barrier()
                with contextlib.ExitStack() as ph:
                    al = lambda n, s, d: sb(n, s, d, ph)
                    upad = al("upad", [128, 4, SEQ + 30], BF16)
                    diag = al("diag", [128, 31, 128], BF16)
                    sigr = Rot(al, "c_sig", 2, [128, 512], F32)
                    S.op("dve", lambda e: e.memset(upad[:, :, 0:15], 0.0), writes=["upad_l"])
                    S.op("dve", lambda e: e.memset(upad[:, :, SEQ + 15:SEQ + 30], 0.0), writes=["upad_r"])
                    for c in range(4):
                        wt, wk = wget(("conv", l, c))
                        for j in range(NBLK):
                            ba, bg = pb[j % 2], pb[2 + j % 2]
                            S.ops("pe", [mm(ba[:], wt[:, k, 0:128], A[:, k, blk(j)], k == 0, k == 7) for k in range(8)],
                                  reads=[wk] + [("A", k, j) for k in range(8)], writes=[PB[j % 2]])
                            S.ops("pe", [mm(bg[:], wt[:, k, 128:256], A[:, k, blk(j)], k == 0, k == 7) for k in range(8)],
                                  reads=[wk] + [("A", k, j) for k in range(8)], writes=[PB[2 + j % 2]])
                            sg, sgk = sigr.get()
                            S.op("act", lambda e, sg=sg, bg=bg: e.activation(out=sg[:], in_=bg[:], func=AF.Sigmoid),
                                 reads=[PB[2 + j % 2]], writes=[sgk])
                            S.op("dve", lambda e, sg=sg, ba=ba, c=c, j=j: e.tensor_tensor(
                                out=upad[:, c, 15 + j * 512:15 + (j + 1) * 512], in0=ba[:], in1=sg[:], op=ALU.mult),
                                reads=[PB[j % 2], sgk], writes=[("u", c, j)])
                        wprefetch()
                    S.barrier()
                    for c in range(4):
                        for tp in range(31):
                            S.op("dve", lambda e, tp=tp, c=c: e.tensor_scalar(
                                out=diag[:, tp, :], in0=identb[:], scalar1=wdwT[l][:, c * 31 + tp:c * 31 + tp + 1],
                                scalar2=None, op0=ALU.mult),
                                reads=["identb", "wdwT%d" % l], writes=["diag"])
                        for j in range(NBLK):
                            bank = pb[4 + j % 2]
                            rd = ["diag", "upad_l", "upad_r"] + [("u", c, jj) for jj in range(max(0, j - 1), min(NBLK, j + 2))]
                            S.ops("pe", [mm(bank[:], diag[:, tp, :], upad[:, c, j * 512 + tp:j * 512 + tp + 512],
                                            tp == 0, tp == 30) for tp in range(31)],
                                  reads=rd, writes=[PB[4 + j % 2]])
                            S.op("act", lambda e, bank=bank, c=c, j=j: e.activation(
                                out=A[:, c, blk(j)], in_=bank[:], func=AF.Identity, bias=bdwT[l][:, c:c + 1], scale=1.0),
                                reads=[PB[4 + j % 2], "bdwT%d" % l], writes=[("A", c, j)])
                    S.barrier()
                with contextlib.ExitStack() as ph:
                    al = lambda n, s, d: sb(n, s, d, ph)
                    vsq = Rot(al, "l_vsq", 1, [128, 4, 512], BF16)
                    mur = Rot(al, "l_mu", 1, [128, 512], F32)
                    msr = Rot(al, "l_ms", 1, [128, 512], F32)
                    sdr = Rot(al, "l_sd", 1, [128, 512], F32)
                    tr = Rot(al, "l_t", 2, [128, 512], F32)
                    for j in range(NBLK):
                        vq, vqk = vsq.get()
                        S.op("act", lambda e, vq=vq: e.activation(out=vq[:], in_=A[:, 0:4, blk(j)], func=AF.Square),
                             reads=[("A", c, j) for c in range(4)], writes=[vqk])
                        S.ops("pe", [mm(pb[6][:], onesb[:], A[:, c, blk(j)], c == 0, c == 3) for c in range(4)],
                              reads=[("A", c, j) for c in range(4)] + ["onesb"], writes=[PB[6]])
                        S.ops("pe", [mm(pb[7][:], onesb[:], vq[:, c, :], c == 0, c == 3) for c in range(4)],
                              reads=[vqk, "onesb"], writes=[PB[7]])
                        mu, muk = mur.get()
                        ms, msk = msr.get()
                        sd, sdk = sdr.get()
                        S.op("dve", lambda e, mu=mu: e.tensor_scalar(out=mu[:], in0=pb[6][:], scalar1=1.0 / 512,
                                                                     scalar2=None, op0=ALU.mult),
                             reads=[PB[6]], writes=[muk])
                        S.op("dve", lambda e, mu=mu, ms=ms: e.tensor_tensor(out=ms[:], in0=mu[:], in1=mu[:], op=ALU.mult),
                             reads=[muk], writes=[msk])
                        S.op("dve", lambda e, ms=ms, sd=sd: e.scalar_tensor_tensor(
                            out=sd[:], in0=pb[7][:], scalar=1.0 / 512, in1=ms[:], op0=ALU.mult, op1=ALU.subtract),
                            reads=[PB[7], msk], writes=[sdk])
                        S.op("act", lambda e, sd=sd: e.activation(out=sd[:], in_=sd[:], func=AF.Ln, bias=eps_c, scale=1.0),
                             reads=[sdk, "cvec"], writes=[sdk])
                        S.op("act", lambda e, sd=sd: e.activation(out=sd[:], in_=sd[:], func=AF.Exp, scale=-0.5), reads=[sdk], writes=[sdk])
                        for c in range(4):
                            t, tk = tr.get()
                            S.op("dve", lambda e, t=t, c=c, mu=mu: e.tensor_tensor(
                                out=t[:], in0=A[:, c, blk(j)], in1=mu[:], op=ALU.subtract),
                                reads=[("A", c, j), muk], writes=[tk])
                            S.op("dve", lambda e, t=t, sd=sd: e.tensor_tensor(out=t[:], in0=t[:], in1=sd[:], op=ALU.mult),
                                 reads=[tk, sdk], writes=[tk])
                            S.op("act", lambda e, t=t, c=c: e.activation(
                                out=A[:, c, blk(j)], in_=t[:], func=AF.Silu, bias=bcnT[l][:, c:c + 1],
                                scale=gcnT[l][:, c:c + 1]),
                                reads=[tk, "gcnT%d" % l, "bcnT%d" % l], writes=[("A", c, j)])
                    S.barrier()
                for dh in range(2):
                    wt, wk = wget(("o", l, dh))
                    for j in range(NBLK):
                        for dc in range(4):
                            i = (j * 4 + dc) % 4
                            cch = dh * 4 + dc
                            fns = [mm(pb[i][:], wt[:, k, dc * 128:(dc + 1) * 128],
                                      qT[:, k, blk(j)] if k < 4 else A[:, k - 4, blk(j)], k == 0, k == 7) for k in range(8)]
                            S.ops("pe", fns,
                                  reads=[wk] + [("q", k, j) for k in range(4)] + [("A", k, j) for k in range(4)], writes=[PB[i]])
                            S.op("dve", lambda e, i=i, cch=cch, j=j: e.scalar_tensor_tensor(
                                out=xT[:, cch, blk(j)], in0=pb[i][:], scalar=mod_ap(l, 2, cch, b), in1=xT[:, cch, blk(j)],
                                op0=ALU.mult, op1=ALU.add),
                                reads=[PB[i], ("x", cch, j), ("modT", l, 16 + cch)], writes=[("x", cch, j)])
                    if dh == 0:
                        wprefetch()
                S.barrier()

        TS = 256
        NTILE = 64
        U32 = mybir.dt.uint32
        I32 = mybir.dt.int32

        def moe(b, l):
            w1rows = w1b_d[:, :]
            w2rows = w2b_d[:, :]
            b1rows = b1R_d.rearrange("l r n -> (l r) n")
            b2rows = b2R_d.rearrange("l r n -> (l r) n")
            with contextlib.ExitStack() as ph_g:
                alg = lambda n, s, d: sb(n, s, d, ph_g)
                gate4 = alg("gate4", [128, NT, 4], F32)
                slotu = alg("slotu", [128, NT * 4], U32)
                widxu = alg("widxu", [128, NTILE, 8], U32)
                bidxu = alg("bidxu", [128, NTILE], U32)
                ph_r = contextlib.ExitStack()
                alr = lambda n, s, d: sb(n, s, d, ph_r)
                lgs_all = alr("lgs_all", [128, NT, NE], F32)
                m8_all = alr("m8_all", [128, NT, 8], F32)
                maskb = alr("maskb", [128, NT, NE], BF16)
                slotf = alr("slotf", [128, NT * 4], F32)
                cnt = alr("cnt", [128, NE], F32)
                ntl = alr("ntl", [128, NE], F32)
                incl = alr("incl", [128, NE], F32)
                pst = alr("pst", [128, NE], F32)
                onesf = alr("onesf", [128, NE], F32)
                te = alr("te", [128, NTILE], F32)
                te128 = alr("te128", [128, NTILE], F32)
                tflag = alr("tflag", [128, NTILE], F32)
                widxf = alr("widxf", [128, NTILE, 8], F32)
                bidxf = alr("bidxf", [128, NTILE], F32)
                with contextlib.ExitStack() as ph:
                    al = lambda n, s, d: sb(n, s, d, ph)
                    nmr = Rot(al, "r_nm", 2, [128, 1], F32)
                    ssr = Rot(al, "r_ss", 2, [128, 1], F32)

                    def router(j, h, hk):
                        lg = pb[2]
                        for tt in range(4):
                            fns = [mm(lg[:, tt * NE:(tt + 1) * NE], h[:, k, tt * 128:(tt + 1) * 128], wr32[l][:, k, :], k == 0, False)
                                   for k in range(8)]
                            fns.append(mm(lg[:, tt * NE:(tt + 1) * NE], ones32[0:1, :], brow[l][0:1, :], False, True))
                            S.ops("pe", fns, reads=[(hk, k) for k in range(8)] + ["wr%d" % l, "brow%d" % l, "ones32"],
                                  writes=[(PB[2], tt)])
                        S.op("act", lambda e: e.copy(out=lgs_all[:, 4 * j:4 * j + 4, :],
                                                     in_=lg[:, 0:4 * NE].rearrange("p (t e) -> p t e", e=NE)),
                             reads=[(PB[2], tt) for tt in range(4)], writes=[("lgs", 4 * j + tt) for tt in range(4)])
                        for tt in range(4):
                            t = 4 * j + tt
                            lgt = lgs_all[:, t, :]
                            m8 = m8_all[:, t, :]
                            nm, nmk = nmr.get()
                            ss, ssk = ssr.get()
                            S.op("dve", lambda e: e.max(out=m8, in_=lgt), reads=[("lgs", t)], writes=[("m8", t)])
                            S.op("dve", lambda e: e.tensor_scalar(out=nm[:], in0=m8[:, 0:1], scalar1=-1.0, scalar2=None, op0=ALU.mult),
                                 reads=[("m8", t)], writes=[nmk])
                            S.op("act", lambda e: e.activation(out=gate4[:, t, :], in_=m8[:, 0:4], func=AF.Exp, bias=nm[:, 0:1], scale=1.0),
                                 reads=[("m8", t), nmk], writes=[("g4", t)])
                            S.op("dve", lambda e: e.tensor_reduce(out=ss[:], in_=gate4[:, t, :], axis=mybir.AxisListType.X, op=ALU.add),
                                 reads=[("g4", t)], writes=[ssk])
                            S.op("dve", lambda e: e.reciprocal(out=ss[:], in_=ss[:]), reads=[ssk], writes=[ssk])
                            S.op("dve", lambda e: e.tensor_scalar(out=gate4[:, t, :], in0=gate4[:, t, :], scalar1=ss[:, 0:1], scalar2=None,
                                                                  op0=ALU.mult),
                                 reads=[("g4", t), ssk], writes=[("g4", t)])
                            S.op("dve", lambda e: e.tensor_scalar(out=maskb[:, t, :], in0=lgt, scalar1=m8[:, 3:4], scalar2=None, op0=ALU.is_ge),
                                 reads=[("lgs", t), ("m8", t)], writes=[("mask", t)])

                    norm_mod(b, l, 1, ph, router=router)
                    S.barrier()
                with contextlib.ExitStack() as ph:
                    al = lambda n, s, d: sb(n, s, d, ph)
                    tmpr = Rot(al, "q_tmp", 2, [128, NE], F32)
                    Sr = Rot(al, "q_S", 2, [128, NE], F32)
                    S.ops("pe", [mm(pb[0][:, 0:NE], onesb[:], maskb[:, t, :], t == 0, t == NT - 1) for t in range(NT)],
                          reads=[("mask", t) for t in range(NT)] + ["onesb"], writes=[PB[0]])
                    S.op("dve", lambda e: e.tensor_copy(out=cnt[:], in_=pb[0][:, 0:NE]), reads=[PB[0]], writes=["cnt"])
                    S.op("dve", lambda e: e.tensor_scalar(out=ntl[:], in0=cnt[:], scalar1=0.0, scalar2=None, op0=ALU.is_gt),
                         reads=["cnt"], writes=["ntl"])
                    for jj in range(1, SEQ // TS):
                        S.op("dve", lambda e, jj=jj: e.scalar_tensor_tensor(out=ntl[:], in0=cnt[:], scalar=float(TS * jj), in1=ntl[:],
                                                                            op0=ALU.is_gt, op1=ALU.add),
                             reads=["cnt", "ntl"], writes=["ntl"])
                    S.op("dve", lambda e: e.memset(onesf[:], 1.0), writes=["onesf"])
                    S.op("dve", lambda e: e.tensor_tensor_scan(out=incl[:], data0=onesf[:], data1=ntl[:], initial=0.0,
                                                               op0=ALU.mult, op1=ALU.add),
                         reads=["onesf", "ntl"], writes=["incl"])
                    S.op("dve", lambda e: e.tensor_tensor(out=pst[:], in0=incl[:], in1=ntl[:], op=ALU.subtract),
                         reads=["incl", "ntl"], writes=["pst"])
                    for i in range(NTILE):
                        tmp, tmpk = tmpr.get()
                        S.op("dve", lambda e, i=i, tmp=tmp: e.tensor_scalar(out=tmp[:], in0=incl[:], scalar1=float(i), scalar2=None,
                                                                            op0=ALU.is_le),
                             reads=["incl"], writes=[tmpk])
                        S.op("dve", lambda e, i=i, tmp=tmp: e.tensor_reduce(out=te[:, i:i + 1], in_=tmp[:], axis=mybir.AxisListType.X,
                                                                            op=ALU.add),
                             reads=[tmpk], writes=[("te", i)])
                    tek = [("te", i) for i in range(NTILE)]
                    S.op("dve", lambda e: e.tensor_scalar(out=tflag[:], in0=te[:], scalar1=float(NE), scalar2=bigcol[:, 0:1],
                                                          op0=ALU.is_ge, op1=ALU.mult),
                         reads=tek + ["bigcol"], writes=["tflag"])
                    S.op("dve", lambda e: e.tensor_scalar(out=te[:], in0=te[:], scalar1=float(NE - 1), scalar2=None, op0=ALU.min),
                         reads=tek, writes=["te_all"])
                    S.op("dve", lambda e: e.tensor_scalar(out=te128[:], in0=te[:], scalar1=128.0, scalar2=float(l * NE * 128), op0=ALU.mult, op1=ALU.add),
                         reads=["te_all"], writes=["te128"])
                    S.op("dve", lambda e: e.tensor_scalar(out=bidxf[:], in0=te128[:], scalar1=pcol[:, 0:1], scalar2=None, op0=ALU.add),
                         reads=["te128", "pcol"], writes=["bidxf"])
                    S.op("dve", lambda e: e.tensor_copy(out=bidxu[:], in_=bidxf[:]), reads=["bidxf"], writes=["bidxu"])
                    for c in range(8):
                        S.op("dve", lambda e, c=c: e.tensor_scalar(out=widxf[:, :, c], in0=te128[:], scalar1=8.0, scalar2=basepc[:, c:c + 1],
                                                                   op0=ALU.mult, op1=ALU.add),
                             reads=["te128", "basepc"], writes=[("widxf", c)])

                    S.op("dve", lambda e: e.tensor_copy(out=widxu[:], in_=widxf[:]), reads=[("widxf", c) for c in range(8)], writes=["widxu"])
                    for t in range(NT):
                        bank = pb[1 + t % 2]
                        fns = [mm(bank[:, 0:NE], onesb[:], maskb[:, tp, :], tp == 0, False) for tp in range(t)]
                        fns.append(mm(bank[:, 0:NE], utri[:], maskb[:, t, :], t == 0, True))
                        S.ops("pe", fns, reads=[("mask", tp) for tp in range(t + 1)] + ["onesb", "utri"], writes=[PB[1 + t % 2]])
                        Sv, Sk = Sr.get()
                        S.op("dve", lambda e, Sv=Sv, bank=bank: e.scalar_tensor_tensor(out=Sv[:], in0=pst[:], scalar=float(TS), in1=bank[:, 0:NE],
                                                                                       op0=ALU.mult, op1=ALU.add),
                             reads=["pst", PB[1 + t % 2]], writes=[Sk])
                        for k in range(4):
                            tmp, tmpk = tmpr.get()
                            S.op("dve", lambda e, t=t, k=k, tmp=tmp, Sv=Sv: e.scalar_tensor_tensor(
                                out=tmp[:], in0=lgs_all[:, t, :], scalar=m8_all[:, t, k:k + 1], in1=Sv[:], op0=ALU.is_equal, op1=ALU.mult),
                                reads=[("lgs", t), ("m8", t), Sk], writes=[tmpk])
                            S.op("dve", lambda e, t=t, k=k, tmp=tmp: e.tensor_reduce(out=slotf[:, 4 * t + k:4 * t + k + 1], in_=tmp[:],
                                                                                    axis=mybir.AxisListType.X, op=ALU.add),
                                 reads=[tmpk], writes=[("slotf", t, k)])
                    S.op("dve", lambda e: e.tensor_copy(out=slotu[:], in_=slotf[:]),
                         reads=[("slotf", t, k) for t in range(NT) for k in range(4)], writes=["slotu"])
                    S.barrier()
                ph_r.close()
                with contextlib.ExitStack() as ph:
                    al = lambda n, s, d: sb(n, s, d, ph)
                    htr = Rot(al, "h_tok", 2, [128, D], BF16)
                    hs_ev = {}
                    for t in range(NT):
                        ht, htk = htr.get()
                        bank = pb[t % 2]
                        bview = bank[:].bitcast(BF16)
                        fns = [(lambda e, c=c, bview=bview, t=t: e.transpose(out=bview[:, c * 128:(c + 1) * 128],
                                                                              in_=A[:, c, t * 128:(t + 1) * 128], identity=identb[:]))
                               for c in range(8)]
                        S.ops("pe", fns, reads=[("A", c, t // 4) for c in range(8)] + ["identb"], writes=[PB[t % 2]])
                        if t % 2 == 0:
                            S.op("dve", lambda e, ht=ht, bview=bview: e.tensor_copy(out=ht[:], in_=bview), reads=[PB[t % 2]], writes=[htk])
                        else:
                            S.op("act", lambda e, ht=ht, bview=bview: e.copy(out=ht[:], in_=bview), reads=[PB[t % 2]], writes=[htk])

                        def sc(e, ht=ht, t=t):
                            return [e.indirect_dma_start(out=hs_d[:, :], out_offset=bass.IndirectOffsetOnAxis(
                                ap=slotu[:, 4 * t + k:4 * t + k + 1], axis=0), in_=ht[:, :], in_offset=None) for k in range(4)]
                        hs_ev[htk] = S.dma("pool", sc, "sc_" + htk, reads=[htk, "slotu"], writes=[("HS", t)])
                    S.state["HS"] = {"w": list(hs_ev.values()), "r": []}
                    S.barrier()
                with contextlib.ExitStack() as ph:
                    al = lambda n, s, d: sb(n, s, d, ph)
                    wA0 = al("wA0", [128, 8, 2 * D], BF16)
                    wA1b = al("wA1b", [128, 4, 2 * D], BF16)
                    wAs = [[wA0[:, c, :] for c in range(8)],
                           [A[:, 4 + c, :] for c in range(4)] + [wA1b[:, c, :] for c in range(4)]]
                    wBl = [A[:, c // 2, (c % 2) * D:(c % 2 + 1) * D] for c in range(8)]
                    hgr = Rot(al, "e_hg", 2, [128, 8, TS], BF16)
                    actr = Rot(al, "e_act", 1, [128, 8, TS], BF16)
                    glr = Rot(al, "e_gl", 2, [128, TS], F32)
                    tr = Rot(al, "e_t", 2, [128, TS], F32)
                    b1r = Rot(al, "e_b1", 2, [128, 16], F32)
                    b1pr = Rot(al, "e_b1p", 2, [128, 16], F32)
                    b2r = Rot(al, "e_b2", 2, [128, 8], F32)
                    hsts = [wslot[2][:].rearrange("p a b -> p (a b)").rearrange("p (s d) -> p s d", s=4)[:, 2 * q_:2 * q_ + 2, :]
                            for q_ in range(2)]

                    def hs_load(i_):
                        S.dma("sp", lambda e: e.dma_start(out=hsts[i_ % 2],
                                                          in_=hs_d[i_ * TS:(i_ + 1) * TS, :].rearrange("(s p) d -> p s d", p=128)),
                              "hsld%d" % (i_ % 2), reads=["HS"], writes=[("ws2", i_ % 2)])
                    hs_load(0)
                    yT = wslot[0][:].rearrange("p a b -> p (a b)").bitcast(F32).rearrange("p (c s) -> p c s", c=8)
                    ytok = [wslot[1][:].rearrange("p a b -> p (a b)").bitcast(F32).rearrange("p (s d) -> p s d", s=2)[:, s, :]
                            for s in range(2)]
                    ys_ev = {}
                    cntp = 0
                    for i in range(NTILE):
                        par = i % 2
                        wA = wAs[par]

                        def gw1f(i_, dstl):
                            def f(e):
                                return [e.indirect_dma_start(out=dstl[c], out_offset=None, in_=w1rows,
                                                             in_offset=bass.IndirectOffsetOnAxis(ap=widxu[:, i_, c:c + 1], axis=0)) for c in range(8)]
                            return f

                        def gbf(i_, b1t_, b2t_):
                            def f(e):
                                return [e.indirect_dma_start(out=b1t_[:, :], out_offset=None, in_=b1rows,
                                                             in_offset=bass.IndirectOffsetOnAxis(ap=bidxu[:, i_:i_ + 1], axis=0)),
                                        e.indirect_dma_start(out=b2t_[:, :], out_offset=None, in_=b2rows,
                                                             in_offset=bass.IndirectOffsetOnAxis(ap=bidxu[:, i_:i_ + 1], axis=0))]
                            return f

                        if i == 0:
                            bias_buf = {}
                            b1t, b1k = b1r.get()
                            b2t, b2k = b2r.get()
                            S.dma("pool", gbf(0, b1t, b2t), "gb0", reads=["bidxu"], writes=[b1k, b2k])
                            bias_buf[0] = (b1t, b1k, b2t, b2k)
                            S.dma("pool", gw1f(0, wAs[0]), "gwA0", reads=["widxu", ("wb", l)], writes=[("wA", 0)])

                        def gw2(e, i=i):
                            return [e.indirect_dma_start(out=wBl[c], out_offset=None, in_=w2rows,
                                                         in_offset=bass.IndirectOffsetOnAxis(ap=widxu[:, i, c:c + 1], axis=0)) for c in range(8)]
                        S.dma("pool", gw2, "gwB", reads=["widxu", ("wb", l)], writes=["wB"])
                        if i + 1 < NTILE:
                            nb1t, nb1k = b1r.get()
                            nb2t, nb2k = b2r.get()
                            S.dma("pool", gbf(i + 1, nb1t, nb2t), "gb%d" % ((i + 1) % 2), reads=["bidxu"], writes=[nb1k, nb2k])
                            bias_buf[i + 1] = (nb1t, nb1k, nb2t, nb2k)
                            S.dma("pool", gw1f(i + 1, wAs[1 - par]), "gwA%d" % (1 - par), reads=["widxu", ("wb", l)], writes=[("wA", 1 - par)])
                        b1t, b1k, b2t, b2k = bias_buf.pop(i)
                        b1p, b1pk = b1pr.get()
                        S.op("dve", lambda e, b1p=b1p, b1t=b1t: e.tensor_scalar(out=b1p[:], in0=b1t[:], scalar1=1.0, scalar2=None, op0=ALU.add),
                             reads=[b1k], writes=[b1pk])
                        hstok = hsts[par]
                        if i + 1 < NTILE:
                            hs_load(i + 1)
                        hg, hgk = hgr.get()
                        for half in range(2):
                            bank = pb[4 + half]
                            bview = bank[:].bitcast(BF16)
                            fns = [(lambda e, cc=cc, s=s, bview=bview, half=half: e.transpose(
                                out=bview[:, cc * TS + s * 128:cc * TS + (s + 1) * 128],
                                in_=hstok[:, s, (half * 4 + cc) * 128:(half * 4 + cc + 1) * 128], identity=identb[:]))
                                for cc in range(4) for s in range(2)]
                            S.ops("pe", fns, reads=[("ws2", par), "identb"], writes=[PB[4 + half]])
                            dst = hg[:, half * 4:half * 4 + 4, :]
                            src = bview.rearrange("p (c s) -> p c s", c=4)
                            if half == 0:
                                S.op("dve", lambda e, dst=dst, src=src: e.tensor_copy(out=dst, in_=src), reads=[PB[4 + half]], writes=[(hgk, half)])
                            else:
                                S.op("dve", lambda e, dst=dst, src=src: e.tensor_copy(out=dst, in_=src), reads=[PB[4 + half]], writes=[(hgk, half)])
                        act, actk = actr.get()
                        for nch in range(8):
                            bank = pb[cntp % 2]
                            bk = PB[cntp % 2]
                            cntp += 1
                            fns = [mm(bank[:, 0:TS], wA[k][:, nch * 128:(nch + 1) * 128], hg[:, k, :], k == 0, k == 7) for k in range(8)]
                            fns += [mm(bank[:, TS:2 * TS], wA[k][:, D + nch * 128:D + (nch + 1) * 128], hg[:, k, :], k == 0, k == 7) for k in range(8)]
                            S.ops("pe", fns, reads=[("wA", par), (hgk, 0), (hgk, 1)], writes=[bk])
                            gl, glk = glr.get()
                            tq, tk = tr.get()
                            S.op("dve", lambda e, gl=gl, bank=bank, b1t=b1t, nch=nch: e.tensor_scalar(
                                out=gl[:], in0=bank[:, 0:TS], scalar1=b1t[:, nch:nch + 1], scalar2=7.0, op0=ALU.add, op1=ALU.min),
                                reads=[bk, b1k], writes=[glk])
                            S.op("act", lambda e, gl=gl: e.activation(out=gl[:], in_=gl[:], func=AF.Silu, scale=1.702), reads=[glk], writes=[glk])
                            S.op("dve", lambda e, tq=tq, bank=bank, b1p=b1p, nch=nch: e.tensor_scalar(
                                out=tq[:], in0=bank[:, TS:2 * TS], scalar1=b1p[:, 8 + nch:9 + nch], scalar2=8.0, op0=ALU.add, op1=ALU.min),
                                reads=[bk, b1pk], writes=[tk])
                            S.op("dve", lambda e, tq=tq, gl=gl, act=act, nch=nch: e.scalar_tensor_tensor(
                                out=act[:, nch, :], in0=tq[:], scalar=-6.0, in1=gl[:], op0=ALU.max, op1=ALU.mult),
                                reads=[tk, glk], writes=[(actk, nch)])
                        for dc in range(8):
                            bank = pb[2 + dc % 2]
                            bk = PB[2 + dc % 2]
                            S.ops("pe", [mm(bank[:, 0:TS], wBl[k][:, dc * 128:(dc + 1) * 128], act[:, k, :], k == 0, k == 7)
                                         for k in range(8)],
                                  reads=["wB"] + [(actk, k) for k in range(8)], writes=[bk])
                            S.op("dve", lambda e, bank=bank, dc=dc, b2t=b2t: e.tensor_scalar(
                                out=yT[:, dc, :], in0=bank[:, 0:TS], scalar1=1.0 / 1.702, scalar2=b2t[:, dc:dc + 1], op0=ALU.mult, op1=ALU.add),
                                reads=[bk, b2k], writes=[("ws0", dc)])
                        for s in range(2):
                            for half in range(2):
                                bank = pb[6 + half]
                                fns = [(lambda e, cc=cc, bank=bank, s=s, half=half: e.transpose(
                                    out=bank[:, cc * 128:(cc + 1) * 128], in_=yT[:, half * 4 + cc, s * 128:(s + 1) * 128], identity=ident[:]))
                                    for cc in range(4)]
                                S.ops("pe", fns, reads=[("ws0", half * 4 + cc) for cc in range(4)] + ["ident"], writes=[PB[6 + half]])
                                if half == 0:
                                    S.op("dve", lambda e, bank=bank, s=s: e.tensor_copy(out=ytok[s][:, 0:512], in_=bank[:]),
                                         reads=[PB[6 + half]], writes=[("ws1", s, 0)])
                                else:
                                    S.op("dve", lambda e, bank=bank, s=s: e.tensor_copy(out=ytok[s][:, 512:1024], in_=bank[:]),
                                         reads=[PB[6 + half]], writes=[("ws1", s, 1)])
                            row = i * TS + s * 128
                            ys_ev[s] = S.dma("sp", lambda e, s=s, row=row: e.dma_start(out=ys_d[row:row + 128, :], in_=ytok[s]),
                                             "yst%d" % s, reads=[("ws1", s, 0), ("ws1", s, 1)], writes=[("YS", i, s)])
                    S.state["YS"] = {"w": list(ys_ev.values()), "r": []}
                    S.barrier()
                    for kk in ("ws0", "ws1", "ws2"):
                        evs = []
                        for key, stt in S.state.items():
                            if isinstance(key, tuple) and key[0] == kk:
                                evs += stt["w"] + stt["r"]
                        base = S.state.setdefault(kk, {"w": [], "r": []})
                        base["r"] = base["r"] + evs
                with contextlib.ExitStack() as ph:
                    al = lambda n, s, d: sb(n, s, d, ph)
                    ykr = Rot(al, "c_yk", 8, [128, D], F32)
                    accr = Rot(al, "c_acc", 2, [128, D], F32)
                    gt2 = lambda cch: mod_ap(l, 5, cch, b)
                    for t in range(NT):
                        yks = []
                        for k in range(4):
                            yk, ykk = ykr.get()
                            S.dma("pool", lambda e, yk=yk, t=t, k=k: e.indirect_dma_start(
                                out=yk[:, :], out_offset=None, in_=ys_d[:, :],
                                in_offset=bass.IndirectOffsetOnAxis(ap=slotu[:, 4 * t + k:4 * t + k + 1], axis=0)),
                                "g_" + ykk, reads=["YS", "slotu"], writes=[ykk])
                            yks.append((yk, ykk))
                        acc, acck = accr.get()
                        S.op("dve", lambda e, acc=acc, yk=yks[0][0], t=t: e.tensor_scalar(
                            out=acc[:], in0=yk[:], scalar1=gate4[:, t, 0:1], scalar2=None, op0=ALU.mult),
                            reads=[yks[0][1], ("g4", t)], writes=[acck])
                        for k in range(1, 4):
                            S.op("dve", lambda e, acc=acc, yk=yks[k][0], t=t, k=k: e.scalar_tensor_tensor(
                                out=acc[:], in0=yk[:], scalar=gate4[:, t, k:k + 1], in1=acc[:], op0=ALU.mult, op1=ALU.add),
                                reads=[yks[k][1], ("g4", t), acck], writes=[acck])
                        for half in range(2):
                            bank = pb[(2 * t + half) % 4]
                            bk = PB[(2 * t + half) % 4]
                            fns = [(lambda e, cc=cc, bank=bank, acc=acc, half=half: e.transpose(
                                out=bank[:, cc * 128:(cc + 1) * 128], in_=acc[:, (half * 4 + cc) * 128:(half * 4 + cc + 1) * 128],
                                identity=ident[:])) for cc in range(4)]
                            S.ops("pe", fns, reads=[acck, "ident"], writes=[bk])
                            for cc in range(4):
                                cch = half * 4 + cc
                                S.op("dve", lambda e, bank=bank, cc=cc, cch=cch, t=t: e.scalar_tensor_tensor(
                                    out=xT[:, cch, t * 128:(t + 1) * 128], in0=bank[:, cc * 128:(cc + 1) * 128], scalar=gt2(cch),
                                    in1=xT[:, cch, t * 128:(t + 1) * 128], op0=ALU.mult, op1=ALU.add),
                                    reads=[bk, ("x", cch, t // 4), ("modT", l, 40 + cch)], writes=[("x", cch, t // 4)])
                    S.barrier()

        def cast_layer(l):
            for ex in range(NE):
                r0 = (l * NE + ex) * D

                def f(e, ex=ex, r0=r0):
                    return [e.dma_start(out=w1b_d[r0:r0 + D, :], in_=w1_d[l, ex]),
                            e.dma_start(out=w2b_d[r0:r0 + D, :], in_=w2_d[l, ex])]
                S.dma("pool", f, "cast%d" % l, writes=[("wcast", l, ex)])
            S.state[("wb", l)] = {"w": [(S.dma_sem["cast%d" % l][0], S.dma_sem["cast%d" % l][1])], "r": []}

        cast_layer(0)
        for b in range(NB):
            load_x(b)
            done = (stop == ("load", 0))
            for l in range(NL):
                if done:
                    break
                mixer(b, l)
                if b == 0 and l == 0 and NL > 1:
                    cast_layer(1)
                if stop == ("mixer", l):
                    done = True
                    break
                moe(b, l)
                if stop == ("moe", l):
                    done = True
                    break
            if done:
                store_x(b, False)
                break
            store_x(b, stop is None)
        S.finish([("out", "f_o0"), ("out", "f_o1")])
        build.stats = dict(ticks=dict(S.tick), waits=S.n_wait, ops=S.n_ops, nsem=len(S.sems))
    return nc


def _rope_tables():
    freqs = (np.float32(10000.0) ** (-np.arange(16, dtype=np.float32) / np.float32(16))).astype(np.float32)
    tok = np.arange(SEQ)
    row = (tok // 64).astype(np.float32)
    col = (tok % 64).astype(np.float32)
    cosT = np.zeros((128, SEQ), np.float32)
    sinT = np.zeros((128, SEQ), np.float32)
    for p in range(128):
        d = p % 64
        ang = (row if d < 32 else col) * freqs[d % 16]
        cosT[p] = np.cos(ang.astype(np.float32))
        sinT[p] = np.sin(ang.astype(np.float32))
    pm = np.zeros((128, 128), np.float32)
    for m in range(128):
        i = m % 32
        if i < 16:
            pm[m + 16, m] = -1.0
        else:
            pm[m - 16, m] = 1.0
    return cosT, sinT, pm


def _prep_shared(inp):
    f = lambda a: np.ascontiguousarray(np.asarray(a, dtype=np.float32))
    w_in = f(inp["w_in"])
    q = w_in[:, :, 0:512]
    k0 = w_in[:, :, 512:576]
    k1 = w_in[:, :, 576:640]
    v = w_in[:, :, 640:768]
    ca = w_in[:, :, 768:1280]
    cg = w_in[:, :, 1280:1792]
    parts = [q, k0, k0, k1, k1, v]
    for c in range(4):
        parts += [ca[:, :, c * 128:(c + 1) * 128], cg[:, :, c * 128:(c + 1) * 128]]
    w_in_r = np.ascontiguousarray(np.concatenate(parts, axis=2))
    assert w_in_r.shape[2] == WIN

    def fm(a, nch):
        a = f(a)
        return np.ascontiguousarray(a.reshape(a.shape[0], nch, 128).transpose(0, 2, 1))

    cosT, sinT, pm = _rope_tables()
    sh = dict(
        w_mod=f(inp["w_mod"]), b_modT=fm(inp["b_mod"], 48), g_mixT=fm(inp["g_mix"], 8), g_ffnT=fm(inp["g_ffn"], 8),
        g_finT=fm(f(inp["g_final"])[None], 8)[0], w_in_r=w_in_r,
        gq2=np.ascontiguousarray(np.tile(f(inp["g_q"]), (1, 2))[:, :, None]),
        gk2=np.ascontiguousarray(np.tile(f(inp["g_k"]), (1, 2))[:, :, None]),
        wdwT=np.ascontiguousarray(f(inp["w_dw"]).reshape(2, 31, 4, 128).transpose(0, 3, 2, 1).reshape(2, 128, 124)),
        b_dwT=fm(inp["b_dw"], 4), g_cnT=fm(inp["g_cn"], 4), b_cnT=fm(inp["b_cn"], 4),
        w_out=f(inp["w_out"]), w_router=f(inp["w_router"]), b_router=f(inp["b_router"]),
        w1=f(inp["w1"]),
        b1R=np.ascontiguousarray(f(inp["b1"]).reshape(2, NE, 16, 128).transpose(0, 1, 3, 2).reshape(2, NE * 128, 16)),
        w2=f(inp["w2"]),
        b2R=np.ascontiguousarray(f(inp["b2"]).reshape(2, NE, 8, 128).transpose(0, 1, 3, 2).reshape(2, NE * 128, 8)),
        cosT=cosT, sinT=sinT, pmat=pm,
    )
    return sh


def kernel(**inputs):
    n = 8
    NB = 4
    sh = _prep_shared(inputs)
    x = np.asarray(inputs["x"], dtype=np.float32)
    c = np.asarray(inputs["c"], dtype=np.float32)
    nc = build(NB=NB, NL=2)
    in_maps = []
    for i in range(n):
        m = dict(sh)
        m["x"] = np.ascontiguousarray(x[i * NB:(i + 1) * NB])
        cc = c[i * NB:(i + 1) * NB]
        m["cT"] = np.ascontiguousarray(cc.reshape(NB, 8, 128).transpose(2, 1, 0))
        in_maps.append(m)
    res = run_bass_kernel_spmd(nc, in_maps, core_ids=list(range(n)))
    return np.concatenate([np.asarray(r["out"]) for r in res.results], axis=0).astype(np.float32)
```

```python
import contextlib
import numpy as np
import concourse.bass as bass
import concourse.mybir as mybir
from concourse.bass_utils import run_bass_kernel_spmd

F32 = mybir.dt.float32
BF16 = mybir.dt.bfloat16
AF = mybir.ActivationFunctionType
ALU = mybir.AluOpType

D = 1024
SEQ = 2048
NBLK = 4
NT = 16
NE = 32
EPS = 1e-6
WIN = 1920


class Sched:
    ENG = ("pe", "dve", "act", "pool", "sp")
    LIMIT = 30000

    def __init__(self, nc, stack):
        self.nc = nc
        self.stack = stack
        self.engs = {"pe": nc.tensor, "dve": nc.vector, "act": nc.scalar,
                     "pool": nc.gpsimd, "sp": nc.sync}
        self.tick_sem = {e: stack.enter_context(nc.semaphore("tk_" + e)) for e in self.ENG}
        self.tick = {e: 0 for e in self.ENG}
        self.tick_sid = {e: "tk_" + e for e in self.ENG}
        self.epoch = {}
        self.seen = {e: {} for e in self.ENG}
        self.sems = {}
        for e in self.ENG:
            self.sems["tk_" + e] = self.tick_sem[e]
        self.dma_sem = {}
        self.dma_gen = 0
        self.state = {}
        self.n_wait = 0
        self.n_ops = 0

    def _need(self, e, ev):
        sid, val = ev
        if self.seen[e].get(sid, 0) >= val:
            return
        self.seen[e][sid] = val
        self.engs[e].wait_ge(self.sems[sid], val)
        self.n_wait += 1

    def _deps(self, e, reads, writes):
        for k in reads:
            st = self.state.get(k)
            if st:
                for ev in st["w"]:
                    self._need(e, ev)
        for k in writes:
            st = self.state.get(k)
            if st:
                for ev in st["w"]:
                    self._need(e, ev)
                for ev in st["r"]:
                    self._need(e, ev)

    def _record(self, ev, reads, writes):
        for k in writes:
            self.state[k] = {"w": [ev], "r": []}
        for k in reads:
            st = self.state.setdefault(k, {"w": [], "r": []})
            st["r"].append(ev)
            if len(st["r"]) > 16:
                best = {}
                for s, v in st["r"]:
                    if best.get(s, 0) < v:
                        best[s] = v
                st["r"] = list(best.items())

    def _roll(self, e):
        if self.tick[e] >= self.LIMIT:
            self.epoch[e] = self.epoch.get(e, 0) + 1
            sid = "tk_%s_%d" % (e, self.epoch[e])
            self.sems[sid] = self.stack.enter_context(self.nc.semaphore(sid))
            self.tick_sem[e] = self.sems[sid]
            self.tick_sid[e] = sid
            self.tick[e] = 0

    def barrier(self):
        cur = [(self.tick_sid[e], self.tick[e]) for e in self.ENG if self.tick[e] > 0]
        for e in self.ENG:
            for ev in cur:
                self._need(e, ev)
        for k, st in self.state.items():
            st["w"] = [ev for ev in st["w"] if not ev[0].startswith("tk_")]
            st["r"] = [ev for ev in st["r"] if not ev[0].startswith("tk_")]

    def op(self, e, fn, reads=(), writes=()):
        self._deps(e, reads, writes)
        self._roll(e)
        self.tick[e] += 1
        ev = (self.tick_sid[e], self.tick[e])
        fn(self.engs[e]).then_inc(self.tick_sem[e], 1)
        self._record(ev, reads, writes)
        self.n_ops += 1
        return ev

    def ops(self, e, fns, reads=(), writes=()):
        self._deps(e, reads, writes)
        self._roll(e)
        self.tick[e] += 1
        ev = (self.tick_sid[e], self.tick[e])
        for fn in fns[:-1]:
            fn(self.engs[e])
        fns[-1](self.engs[e]).then_inc(self.tick_sem[e], 1)
        self._record(ev, reads, writes)
        self.n_ops += len(fns)
        return ev

    def dma(self, q, fn, key, reads=(), writes=()):
        self._deps(q, reads, writes)
        if key not in self.dma_sem:
            sid = "dma_" + str(key)
            self.sems[sid] = self.stack.enter_context(self.nc.semaphore(sid))
            self.dma_sem[key] = [sid, 0]
        sid, cnt = self.dma_sem[key]
        if cnt >= self.LIMIT:
            self.dma_gen += 1
            sid = "dma_%s_g%d" % (key, self.dma_gen)
            self.sems[sid] = self.stack.enter_context(self.nc.semaphore(sid))
            self.dma_sem[key] = [sid, 0]
            cnt = 0
        r = fn(self.engs[q])
        if not isinstance(r, (list, tuple)):
            r = [r]
        for ins in r:
            ins.then_inc(self.sems[sid], 16)
        cnt += 16 * len(r)
        self.dma_sem[key][1] = cnt
        ev = (sid, cnt)
        self._record(ev, reads, writes)
        return ev

    def finish(self, keys):
        for k in keys:
            st = self.state.get(k)
            if st:
                for ev in st["w"] + st["r"]:
                    self._need("sp", ev)


class Rot:
    def __init__(self, alloc, name, n, shape, dt):
        self.t = [alloc(name + str(i), shape, dt) for i in range(n)]
        self.k = [name + str(i) for i in range(n)]
        self.i = 0

    def get(self):
        j = self.i % len(self.t)
        self.i += 1
        return self.t[j], self.k[j]


def _plan(NB, NL, NEXP):
    plan = []
    for l in range(NL):
        for j in range(12):
            plan.append(("mod", l, j))
    for b in range(NB):
        for l in range(NL):
            plan.append(("q", l))
            plan.append(("kkv", l))
            for c in range(4):
                plan.append(("conv", l, c))
            for dh in range(2):
                plan.append(("o", l, dh))
    return plan


def build(NB=4, NL=2, NEXP=NE, stop=None):
    nc = bass.Bass("TRN2", target_bir_lowering=False)

    def din(name, shape, dt=F32):
        return nc.dram_tensor(name, list(shape), dt, kind="ExternalInput").ap()

    x_d = din("x", [NB, SEQ, D])
    cT_d = din("cT", [128, 8, NB])
    wmod_d = din("w_mod", [2, D, 6 * D])
    bmodT_d = din("b_modT", [2, 128, 48])
    gmixT_d = din("g_mixT", [2, 128, 8])
    gffnT_d = din("g_ffnT", [2, 128, 8])
    gfinT_d = din("g_finT", [128, 8])
    win_d = din("w_in_r", [2, D, WIN])
    gq2_d = din("gq2", [2, 128, 1])
    gk2_d = din("gk2", [2, 128, 1])
    wdwT_d = din("wdwT", [2, 128, 4 * 31])
    bdwT_d = din("b_dwT", [2, 128, 4])
    gcnT_d = din("g_cnT", [2, 128, 4])
    bcnT_d = din("b_cnT", [2, 128, 4])
    wout_d = din("w_out", [2, D, D])
    wr_d = din("w_router", [2, D, NE])
    br_d = din("b_router", [2, NE])
    w1_d = din("w1", [2, NEXP, D, 2 * D])
    b1R_d = din("b1R", [2, NE * 128, 16])
    b2R_d = din("b2R", [2, NE * 128, 8])
    hs_d = nc.dram_tensor("hs_scratch", [64 * 256, D], BF16, kind="Internal").ap()
    ys_d = nc.dram_tensor("ys_scratch", [64 * 256, D], F32, kind="Internal").ap()
    wmodb_d = nc.dram_tensor("wmod_bf16", [2, D, 6 * D], BF16, kind="Internal").ap()
    winb_d = nc.dram_tensor("win_bf16", [2, D, WIN], BF16, kind="Internal").ap()
    woutb_d = nc.dram_tensor("wout_bf16", [2, D, D], BF16, kind="Internal").ap()
    w1b_d = nc.dram_tensor("w1_bf16", [2 * NE * D, 2 * D], BF16, kind="Internal").ap()
    w2b_d = nc.dram_tensor("w2_bf16", [2 * NE * D, D], BF16, kind="Internal").ap()
    w2_d = din("w2", [2, NEXP, D, D])
    cos_d = din("cosT", [128, SEQ])
    sin_d = din("sinT", [128, SEQ])
    pm_d = din("pmat", [128, 128])
    out_d = nc.dram_tensor("out", [NB, SEQ, D], F32, kind="ExternalOutput").ap()

    with contextlib.ExitStack() as st:
        S = Sched(nc, st)

        uniq = [0]

        def sb(name, shape, dt, stack=st):
            uniq[0] += 1
            return stack.enter_context(nc.sbuf_tensor("%s_s%d" % (name, uniq[0]), list(shape), dt))

        xT = sb("xT", [128, 8, SEQ], F32)
        A = sb("A", [128, 8, SEQ], BF16)
        wslot = [sb("wslot%d" % i, [128, 8, 512], BF16) for i in range(3)]
        cosT = sb("cos", [128, SEQ], BF16)
        sinT = sb("sin", [128, SEQ], BF16)
        pmat = sb("pmat_s", [128, 128], BF16)
        ident = sb("ident", [128, 128], F32)
        identb = sb("identb", [128, 128], BF16)
        onesb = sb("onesb", [128, 128], BF16)
        onesbd = sb("onesbd", [128, 128], BF16)
        ones32 = sb("ones32", [128, 128], F32)
        cvec = sb("cvec", [128, 8], F32)
        cact = sb("cact", [128, 8, NB], F32)
        cactb = sb("cactb", [128, 8, NB], BF16)
        modT = [sb("modT%d" % l, [128, 48, NB], F32) for l in range(NL)]
        bmodT = [sb("bmodT%d" % l, [128, 48], F32) for l in range(NL)]
        gmixT = [sb("gmixT%d" % l, [128, 8], F32) for l in range(NL)]
        gffnT = [sb("gffnT%d" % l, [128, 8], F32) for l in range(NL)]
        gfinT = sb("gfinT", [128, 8], F32)
        gq2 = [sb("gq2_%d" % l, [128, 1], F32) for l in range(NL)]
        gk2 = [sb("gk2_%d" % l, [128, 1], F32) for l in range(NL)]
        wdwT = [sb("wdwT%d" % l, [128, 4 * 31], F32) for l in range(NL)]
        bdwT = [sb("bdwT%d" % l, [128, 4], F32) for l in range(NL)]
        gcnT = [sb("gcnT%d" % l, [128, 4], F32) for l in range(NL)]
        bcnT = [sb("bcnT%d" % l, [128, 4], F32) for l in range(NL)]
        wr32 = [sb("wr32_%d" % l, [128, 8, NE], F32) for l in range(NL)]
        brow = [sb("brow%d" % l, [1, NE], F32) for l in range(NL)]
        utri = sb("utri", [128, 128], BF16)
        utri32 = sb("utri32", [128, 128], F32)
        pcol = sb("pcol", [128, 1], F32)
        bigcol = sb("bigcol", [128, 1], F32)
        basepc = sb("basepc", [128, 8], F32)
        ipc = sb("ipc", [128, 8], mybir.dt.int32)
        gmod = sb("gmod", [128, 8], F32)
        pb = [st.enter_context(nc.psum_tensor("pb%d" % i, [128, 512], F32)) for i in range(8)]
        PB = ["pb%d" % i for i in range(8)]

        const_keys = []

        def cload(q, dst, src, key):
            S.dma(q, lambda e: e.dma_start(out=dst, in_=src), "const", writes=[key])
            const_keys.append(key)

        cload("pool", cosT[:], cos_d, "cos")
        cload("pool", sinT[:], sin_d, "sin")
        cload("pool", pmat[:], pm_d, "pmat")
        cload("sp", cact[:], cT_d, "cact")
        cload("sp", gfinT[:], gfinT_d, "gfinT")
        for l in range(NL):
            cload("sp", bmodT[l][:], bmodT_d[l], "bmodT%d" % l)
            cload("sp", gmixT[l][:], gmixT_d[l], "gmixT%d" % l)
            cload("sp", gffnT[l][:], gffnT_d[l], "gffnT%d" % l)
            cload("sp", gq2[l][:], gq2_d[l], "gq2%d" % l)
            cload("sp", gk2[l][:], gk2_d[l], "gk2%d" % l)
            cload("sp", wdwT[l][:], wdwT_d[l], "wdwT%d" % l)
            cload("sp", bdwT[l][:], bdwT_d[l], "bdwT%d" % l)
            cload("sp", gcnT[l][:], gcnT_d[l], "gcnT%d" % l)
            cload("sp", bcnT[l][:], bcnT_d[l], "bcnT%d" % l)
            cload("sp", wr32[l][:], wr_d[l].rearrange("(c p) n -> p c n", p=128), "wr%d" % l)
            cload("sp", brow[l][:], br_d[l:l + 1, :], "brow%d" % l)
        fin = (S.dma_sem["const"][0], S.dma_sem["const"][1])
        for k in const_keys:
            S.state[k] = {"w": [fin], "r": []}

        S.op("pool", lambda e: e.memset(ident[:], 0.0), writes=["ident"])
        S.op("pool", lambda e: e.affine_select(out=ident[:], in_=ident[:], pattern=[[-1, 128]],
                                               compare_op=ALU.not_equal, fill=1.0, base=0,
                                               channel_multiplier=1),
             reads=["ident"], writes=["ident"])
        S.op("dve", lambda e: e.tensor_copy(out=identb[:], in_=ident[:]), reads=["ident"], writes=["identb"])
        S.op("pool", lambda e: e.memset(utri32[:], 1.0), writes=["utri32"])
        S.op("pool", lambda e: e.affine_select(out=utri32[:], in_=utri32[:], pattern=[[1, 128]],
                                               compare_op=ALU.is_gt, fill=0.0, base=0, channel_multiplier=-1),
             reads=["utri32"], writes=["utri32"])
        S.op("dve", lambda e: e.tensor_copy(out=utri[:], in_=utri32[:]), reads=["utri32"], writes=["utri"])
        S.op("pool", lambda e: e.iota(ipc[:], pattern=[[128, 8]], base=0, channel_multiplier=1), writes=["ipc"])
        S.op("dve", lambda e: e.tensor_copy(out=basepc[:], in_=ipc[:]), reads=["ipc"], writes=["basepc"])
        S.op("dve", lambda e: e.tensor_copy(out=pcol[:], in_=ipc[:, 0:1]), reads=["ipc"], writes=["pcol"])
        S.op("dve", lambda e: e.tensor_scalar(out=bigcol[:], in0=pcol[:], scalar1=0.5, scalar2=1048576.0, op0=ALU.is_gt, op1=ALU.mult),
             reads=["pcol"], writes=["bigcol"])
        S.op("dve", lambda e: e.memset(onesb[:], 1.0), writes=["onesb"])
        S.op("dve", lambda e: e.memset(ones32[:], 1.0), writes=["ones32"])
        S.op("dve", lambda e: e.memset(onesbd[:], 0.0), writes=["onesbd"])
        S.op("dve", lambda e: e.memset(onesbd[0:64, 0:64], 1.0), reads=["onesbd"], writes=["onesbd"])
        S.op("dve", lambda e: e.memset(onesbd[64:128, 64:128], 1.0), reads=["onesbd"], writes=["onesbd"])
        S.op("dve", lambda e: e.memset(cvec[:, 0:1], EPS), writes=["cvec"])
        S.op("dve", lambda e: e.memset(cvec[:, 1:2], 64.0 * EPS), reads=["cvec"], writes=["cvec"])
        S.op("dve", lambda e: e.memset(cvec[:, 2:3], 0.0), reads=["cvec"], writes=["cvec"])
        eps_c = cvec[:, 0:1]
        eps64_c = cvec[:, 1:2]
        zero_c = cvec[:, 2:3]
        S.op("act", lambda e: e.activation(out=cactb[:], in_=cact[:], func=AF.Silu),
             reads=["cact"], writes=["cactb"])
        for l in range(NL):
            S.op("dve", lambda e, l=l: e.tensor_scalar(out=gk2[l][:], in0=gk2[l][:], scalar1=8.0,
                                                       scalar2=None, op0=ALU.mult),
                 reads=["gk2%d" % l], writes=["gk2%d" % l])

        plan = _plan(NB, NL, NEXP)
        wstate = {"next": 0, "cons": 0}
        pgrp = []
        g = 0
        for idx_, d_ in enumerate(plan):
            if d_[0] == "q":
                g += 1
            pgrp.append(g)

        def piece_dma(desc, slot):
            kind = desc[0]
            l = desc[1]
            if kind == "mod":
                j = desc[2]
                src = wmodb_d[l].rearrange("(c p) n -> p c n", p=128)[:, :, j * 512:(j + 1) * 512]
                dst = slot[:, :, 0:512]
            elif kind == "conv":
                c = desc[2]
                src = winb_d[l].rearrange("(c p) n -> p c n", p=128)[:, :, 896 + 256 * c:896 + 256 * (c + 1)]
                dst = slot[:, :, 0:256]
            elif kind == "q":
                src = winb_d[l].rearrange("(c p) n -> p c n", p=128)[:, :, 0:512]
                dst = slot[:, :, 0:512]
            elif kind == "kkv":
                src = winb_d[l].rearrange("(c p) n -> p c n", p=128)[:, :, 512:896]
                dst = slot[:, :, 0:384]
            elif kind == "o":
                dh = desc[2]
                src = woutb_d[l].rearrange("(c p) n -> p c n", p=128)[:, :, dh * 512:(dh + 1) * 512]
                dst = slot[:, :, 0:512]
            elif kind == "w1":
                e, pg = desc[2], desc[3]
                v = w1_d[l, e].rearrange("(c p) n -> p c n", p=128)
                src = [v[:, :, 256 * pg:256 * (pg + 1)], v[:, :, 1024 + 256 * pg:1024 + 256 * (pg + 1)]]
                dst = [slot[:, :, 0:256], slot[:, :, 256:512]]
            elif kind == "w2":
                e, dh = desc[2], desc[3]
                src = w2_d[l, e].rearrange("(c p) n -> p c n", p=128)[:, :, dh * 512:(dh + 1) * 512]
                dst = slot[:, :, 0:512]
            if not isinstance(dst, list):
                dst, src = [dst], [src]
            return dst, src

        def wget(desc):
            i = wstate["cons"]
            assert plan[i] == desc, (plan[i], desc)
            while wstate["next"] < len(plan) and wstate["next"] <= i + 2 and pgrp[wstate["next"]] == pgrp[i]:
                j = wstate["next"]
                dst, src = piece_dma(plan[j], wslot[j % 3])
                S.dma("sp", lambda e, dst=dst, src=src: [e.dma_start(out=d_, in_=s_) for d_, s_ in zip(dst, src)],
                      "ws%d" % (j % 3), reads=["mcast"], writes=["ws%d" % (j % 3)])
                wstate["next"] += 1
            wstate["cons"] += 1
            return wslot[i % 3], "ws%d" % (i % 3)

        def wprefetch():
            i = wstate["cons"]
            while wstate["next"] < len(plan) and wstate["next"] <= i + 2 and pgrp[wstate["next"]] == pgrp[max(i - 1, 0)]:
                j = wstate["next"]
                dst, src = piece_dma(plan[j], wslot[j % 3])
                S.dma("sp", lambda e, dst=dst, src=src: [e.dma_start(out=d_, in_=s_) for d_, s_ in zip(dst, src)],
                      "ws%d" % (j % 3), reads=["mcast"], writes=["ws%d" % (j % 3)])
                wstate["next"] += 1

        def blk(j):
            return slice(j * 512, (j + 1) * 512)

        def mm(out, lhsT, rhs, start, stop):
            return lambda e: e.matmul(out, lhsT=lhsT, rhs=rhs, start=start, stop=stop)

        for l in range(NL):
            S.dma("pool", lambda e, l=l: [e.dma_start(out=wmodb_d[l], in_=wmod_d[l]), e.dma_start(out=winb_d[l], in_=win_d[l]),
                                          e.dma_start(out=woutb_d[l], in_=wout_d[l])], "mcast", writes=[("mcast", l)])
        S.state["mcast"] = {"w": [(S.dma_sem["mcast"][0], S.dma_sem["mcast"][1])], "r": []}

        for l in range(NL):
            for j in range(12):
                wt, wk = wget(("mod", l, j))
                for jj in range(4):
                    col = j * 4 + jj
                    bank = pb[col % 2]
                    fns = [mm(bank[:, 0:NB], wt[:, k, jj * 128:(jj + 1) * 128], cactb[:, k, :], k == 0, k == 7)
                           for k in range(8)]
                    S.ops("pe", fns, reads=[wk, "cactb"], writes=[PB[col % 2]])
                    S.op("dve", lambda e, bank=bank, l=l, col=col: e.tensor_scalar(
                        out=modT[l][:, col, :], in0=bank[:, 0:NB], scalar1=bmodT[l][:, col:col + 1],
                        scalar2=None, op0=ALU.add),
                        reads=[PB[col % 2], "bmodT%d" % l], writes=[("modT", l, col)])
                wprefetch()
        S.barrier()

        def mod_ap(l, which, c, b):
            return modT[l][:, which * 8 + c, b:b + 1]

        def norm_stats(stk, j, sqr, sdr, scale):
            sq, sqk = sqr.get()
            S.op("act", lambda e: e.activation(out=sq[:], in_=xT[:, :, blk(j)], func=AF.Square),
                 reads=[("x", c, j) for c in range(8)], writes=[sqk])
            bank = pb[j % 2]
            S.ops("pe", [mm(bank[:], onesb[:], sq[:, k, :], k == 0, k == 7) for k in range(8)],
                  reads=[sqk, "onesb"], writes=[PB[j % 2]])
            sd, sdk = sdr.get()
            S.op("act", lambda e: e.activation(out=sd[:], in_=bank[:], func=AF.Ln, bias=eps_c, scale=scale),
                 reads=[PB[j % 2], "cvec"], writes=[sdk])
            S.op("act", lambda e: e.activation(out=sd[:], in_=sd[:], func=AF.Exp, scale=-0.5), reads=[sdk], writes=[sdk])
            return sd, sdk

        def load_x(b):
            with contextlib.ExitStack() as ph:
                xin = Rot(lambda n, s, d: sb(n, s, d, ph), "xin", 2, [128, D], F32)
                for t in range(NT):
                    xi, xk = xin.get()
                    S.dma("sp", lambda e, xi=xi, t=t: e.dma_start(out=xi[:], in_=x_d[b, t * 128:(t + 1) * 128, :]),
                          xk, writes=[xk])
                    for half in range(2):
                        bi = (2 * t + half) % 4
                        bank = pb[bi]
                        fns = [(lambda e, cc=cc, bank=bank, xi=xi, half=half: e.transpose(
                            out=bank[:, cc * 128:(cc + 1) * 128],
                            in_=xi[:, (half * 4 + cc) * 128:(half * 4 + cc + 1) * 128], identity=ident[:]))
                            for cc in range(4)]
                        S.ops("pe", fns, reads=[xk, "ident"], writes=[PB[bi]])
                        eng = "dve" if half == 0 else "act"
                        dst = xT[:, half * 4:half * 4 + 4, t * 128:(t + 1) * 128]
                        src = bank[:].rearrange("p (c t) -> p c t", c=4)
                        wr = [("x", half * 4 + cc, t // 4) for cc in range(4)]
                        if eng == "dve":
                            S.op("dve", lambda e, dst=dst, src=src: e.tensor_copy(out=dst, in_=src),
                                 reads=[PB[bi]], writes=wr)
                        else:
                            S.op("act", lambda e, dst=dst, src=src: e.copy(out=dst, in_=src),
                                 reads=[PB[bi]], writes=wr)
                S.barrier()

        def store_x(b, do_norm):
            with contextlib.ExitStack() as ph:
                al = lambda n, s, d: sb(n, s, d, ph)
                sqr = Rot(al, "f_sq", 2, [128, 8, 512], BF16)
                sdr = Rot(al, "f_sd", 2, [128, 512], F32)
                y32 = Rot(al, "f_y", 1, [128, 8, 512], F32)
                ost = Rot(al, "f_o", 2, [128, D], F32)
                for j in range(NBLK):
                    y, yk = y32.get()
                    if do_norm:
                        rs, rsk = norm_stats(ph, j, sqr, sdr, 1.0 / D)
                        for c in range(8):
                            S.op("dve", lambda e, c=c, y=y, rs=rs: e.scalar_tensor_tensor(
                                out=y[:, c, :], in0=xT[:, c, blk(j)], scalar=gfinT[:, c:c + 1], in1=rs[:],
                                op0=ALU.mult, op1=ALU.mult),
                                reads=[("x", c, j), rsk, "gfinT"], writes=[(yk, c)])
                    else:
                        for c in range(8):
                            S.op("dve", lambda e, c=c, y=y: e.tensor_copy(out=y[:, c, :], in_=xT[:, c, blk(j)]),
                                 reads=[("x", c, j)], writes=[(yk, c)])
                    for t in range(4):
                        o, ok = ost.get()
                        for half in range(2):
                            bi = (2 * t + half) % 4
                            bank = pb[bi]
                            fns = [(lambda e, cc=cc, bank=bank, y=y, half=half, t=t: e.transpose(
                                out=bank[:, cc * 128:(cc + 1) * 128],
                                in_=y[:, half * 4 + cc, t * 128:(t + 1) * 128], identity=ident[:]))
                                for cc in range(4)]
                            S.ops("pe", fns, reads=[(yk, half * 4 + cc) for cc in range(4)] + ["ident"],
                                  writes=[PB[bi]])
                            if half == 0:
                                S.op("dve", lambda e, o=o, bank=bank: e.tensor_copy(out=o[:, 0:512], in_=bank[:]),
                                     reads=[PB[bi]], writes=[(ok, 0)])
                            else:
                                S.op("act", lambda e, o=o, bank=bank: e.copy(out=o[:, 512:1024], in_=bank[:]),
                                     reads=[PB[bi]], writes=[(ok, 1)])
                        row = (j * 4 + t) * 128
                        S.dma("sp", lambda e, o=o, row=row: e.dma_start(out=out_d[b, row:row + 128, :], in_=o[:]),
                              "st_" + ok, reads=[(ok, 0), (ok, 1)], writes=[("out", ok)])
                S.barrier()

        def norm_mod(b, l, sub, ph, h32r=None, router=None):
            al = lambda n, s, d: sb(n, s, d, ph)
            sqr = Rot(al, "n_sq", 2, [128, 8, 512], BF16)
            sdr = Rot(al, "n_sd", 2, [128, 512], F32)
            h32r = Rot(al, "n_h", 1, [128, 8, 512], F32)
            gsrc = gmixT[l] if sub == 0 else gffnT[l]
            gkey = ("gmixT%d" if sub == 0 else "gffnT%d") % l
            S.op("dve", lambda e: e.scalar_tensor_tensor(
                out=gmod[:], in0=modT[l][:, (1 + 3 * sub) * 8:(2 + 3 * sub) * 8, b], scalar=1.0, in1=gsrc[:],
                op0=ALU.add, op1=ALU.mult),
                reads=[("modT", l, (1 + 3 * sub) * 8 + c) for c in range(8)] + [gkey], writes=["gmod"])
            for j in range(NBLK):
                rs, rsk = norm_stats(ph, j, sqr, sdr, 1.0 / D)
                h, hk = h32r.get()
                for c in range(8):
                    S.op("dve", lambda e, c=c, h=h, rs=rs: e.scalar_tensor_tensor(
                        out=h[:, c, :], in0=xT[:, c, blk(j)], scalar=gmod[:, c:c + 1], in1=rs[:],
                        op0=ALU.mult, op1=ALU.mult),
                        reads=[("x", c, j), rsk, "gmod"], writes=[(hk, c)])
                    sh = mod_ap(l, 3 * sub, c, b)
                    if router is None:
                        S.op("act", lambda e, c=c, h=h, sh=sh: e.activation(
                            out=A[:, c, blk(j)], in_=h[:, c, :], func=AF.Identity, bias=sh, scale=1.0),
                            reads=[(hk, c), ("modT", l, 3 * sub * 8 + c)], writes=[("A", c, j)])
                    else:
                        S.op("act", lambda e, c=c, h=h, sh=sh: e.activation(
                            out=h[:, c, :], in_=h[:, c, :], func=AF.Identity, bias=sh, scale=1.0),
                            reads=[(hk, c), ("modT", l, 3 * sub * 8 + c)], writes=[(hk, c)])
                        S.op("dve", lambda e, c=c, h=h: e.tensor_copy(out=A[:, c, blk(j)], in_=h[:, c, :]),
                             reads=[(hk, c)], writes=[("A", c, j)])
                if router is not None:
                    router(j, h, hk)

        def mixer(b, l):
            with contextlib.ExitStack() as ph:
                norm_mod(b, l, 0, ph)
                S.barrier()
            with contextlib.ExitStack() as ph_q:
                qT = sb("qT", [128, 4, SEQ], BF16, ph_q)
                with contextlib.ExitStack() as ph_kv:
                    al0 = lambda n, s, d: sb(n, s, d, ph_kv)
                    kz = al0("kz", [128, 4, SEQ], BF16)
                    S.op("dve", lambda e: e.memset(kz[:], 0.0), writes=[("k", kc_, j_) for kc_ in range(2) for j_ in range(NBLK)])
                    v1 = al0("v1", [128, NT, 320], BF16)
                    S.op("dve", lambda e: e.memset(v1[:], 1.0), writes=[("v1", t) for t in range(NT)])
                    with contextlib.ExitStack() as ph:
                        al = lambda n, s, d: sb(n, s, d, ph)
                        sqr = Rot(al, "p_sq", 2, [128, 512], BF16)
                        sdr = Rot(al, "p_sd", 2, [128, 512], F32)
                        qnr = Rot(al, "p_qn", 2, [128, 512], BF16)
                        ar = Rot(al, "p_a", 2, [128, 512], F32)
                        br = Rot(al, "p_b", 2, [128, 512], F32)
                        cnt = [0]

                        def stageA(cs):
                            wt, wk, col0, j = cs["wt"], cs["wk"], cs["col0"], cs["j"]
                            i = cs["i"]
                            pq, kq = pb[i % 2], PB[i % 2]
                            S.ops("pe", [mm(pq[:], wt[:, k, col0:col0 + 128], A[:, k, blk(j)], k == 0, k == 7) for k in range(8)],
                                  reads=[wk] + [("A", k, j) for k in range(8)], writes=[kq])
                            sq, sqk = sqr.get()
                            S.op("act", lambda e: e.activation(out=sq[:], in_=pq[:], func=AF.Square), reads=[kq], writes=[sqk])
                            cs["sq"], cs["sqk"] = sq, sqk

                        def stageB(cs):
                            i = cs["i"]
                            pq, kq = pb[i % 2], PB[i % 2]
                            pss, kss = pb[2 + i % 2], PB[2 + i % 2]
                            sq, sqk = cs["sq"], cs["sqk"]
                            S.ops("pe", [mm(pss[:], onesbd[:], sq[:], True, True)], reads=[sqk, "onesbd"], writes=[kss])
                            sd, sdk = sdr.get()
                            S.op("act", lambda e: e.activation(out=sd[:], in_=pss[:], func=AF.Ln, bias=eps64_c, scale=1.0),
                                 reads=[kss, "cvec"], writes=[sdk])
                            S.op("act", lambda e: e.activation(out=sd[:], in_=sd[:], func=AF.Exp, scale=-0.5), reads=[sdk], writes=[sdk])
                            qn, qnk = qnr.get()
                            gain, gkey = cs["gain"], cs["gkey"]
                            S.op("dve", lambda e: e.scalar_tensor_tensor(out=qn[:], in0=pq[:], scalar=gain[:, 0:1], in1=sd[:],
                                                                         op0=ALU.mult, op1=ALU.mult),
                                 reads=[kq, sdk, gkey], writes=[qnk])
                            cs["qn"], cs["qnk"] = qn, qnk

                        def stageC(cs):
                            i, j = cs["i"], cs["j"]
                            ppq, kpq = pb[4 + i % 2], PB[4 + i % 2]
                            qn, qnk = cs["qn"], cs["qnk"]
                            S.ops("pe", [mm(ppq[:], pmat[:], qn[:], True, True)], reads=[qnk, "pmat"], writes=[kpq])
                            a, ak = ar.get()
                            bb, bk = br.get()
                            S.op("dve", lambda e: e.tensor_tensor(out=a[:], in0=qn[:], in1=cosT[:, blk(j)], op=ALU.mult),
                                 reads=[qnk, "cos"], writes=[ak])
                            S.op("dve", lambda e: e.tensor_tensor(out=bb[:], in0=ppq[:], in1=sinT[:, blk(j)], op=ALU.mult),
                                 reads=[kpq, "sin"], writes=[bk])
                            if cs.get("kc") is None:
                                S.op("dve", lambda e: e.tensor_tensor(out=cs["dst"], in0=a[:], in1=bb[:], op=ALU.add),
                                     reads=[ak, bk], writes=[cs["dkey"]])
                            else:
                                kc_ = cs["kc"]
                                S.op("dve", lambda e: e.tensor_tensor(out=kz[0:64, 2 * kc_, blk(j)], in0=a[0:64, :], in1=bb[0:64, :], op=ALU.add),
                                     reads=[ak, bk, cs["dkey"]], writes=[cs["dkey"]])
                                S.op("dve", lambda e: e.tensor_tensor(out=kz[64:128, 2 * kc_ + 1, blk(j)], in0=a[64:128, :], in1=bb[64:128, :],
                                                                      op=ALU.add),
                                     reads=[ak, bk, cs["dkey"]], writes=[cs["dkey"]])

                        wtq, wkq = wget(("q", l))
                        wtk, wkk = None, None
                        chunks = []
                        for j in range(NBLK):
                            for c in range(4):
                                chunks.append(dict(wt=wtq, wk=wkq, col0=c * 128, j=j, dst=qT[:, c, blk(j)], dkey=("q", c, j),
                                                   gain=gq2[l], gkey="gq2%d" % l))
                        for j in range(NBLK):
                            for kc in range(2):
                                chunks.append(dict(wt=wtk, wk=wkk, col0=kc * 128, j=j, dst=None, kc=kc, dkey=("k", kc, j),
                                                   gain=gk2[l], gkey="gk2%d" % l))
                        for i_, cs in enumerate(chunks):
                            cs["i"] = i_
                        nch_ = len(chunks)
                        for step in range(nch_ + 2):
                            if step == 16:
                                wtk, wkk = wget(("kkv", l))
                                for cs in chunks[16:]:
                                    cs["wt"], cs["wk"] = wtk, wkk
                            if step < nch_:
                                stageA(chunks[step])
                            if 0 <= step - 1 < nch_:
                                stageB(chunks[step - 1])
                            if 0 <= step - 2 < nch_:
                                stageC(chunks[step - 2])
                        wt, wk = wtk, wkk
                        for j in range(NBLK):
                            for tt in range(4):
                                t = j * 4 + tt
                                bank = pb[6 + tt % 2]
                                S.ops("pe", [mm(bank[:, 0:128], A[:, k, t * 128:(t + 1) * 128], wt[:, k, 256:384], k == 0, k == 7)
                                             for k in range(8)],
                                      reads=[wk] + [("A", k, j) for k in range(8)], writes=[PB[6 + tt % 2]])
                                dst = v1[:, t, 64:320].rearrange("p (a b) -> p a b", b=128)[:, :, 0:64]
                                src = bank[:, 0:128].rearrange("p (a b) -> p a b", b=64)
                                S.op("act", lambda e, dst=dst, src=src: e.copy(out=dst, in_=src),
                                     reads=[PB[6 + tt % 2]], writes=[("v1", t)])
                        wprefetch()
                        S.barrier()
                    with contextlib.ExitStack() as ph:
                        al = lambda n, s, d: sb(n, s, d, ph)
                        ptr = Rot(al, "a_pt", 4, [128, 512], BF16)
                        recr = Rot(al, "a_rec", 2, [128, 512], F32)
                        seq = [(h, qj, kt) for h in range(8) for qj in range(NBLK) for kt in range(NT)]
                        pend = []

                        def score(idx):
                            h, qj, kt = seq[idx]
                            c, hp, kv = h // 2, h % 2, h // 4
                            bank = pb[idx % 3]
                            rows = slice(hp * 64, hp * 64 + 64)
                            S.ops("pe", [mm(bank[:], kz[:, 2 * kv + hp, kt * 128:(kt + 1) * 128], qT[:, c, blk(qj)], True, True)],
                                  reads=[("k", kv, kt // 4), ("q", c, qj)], writes=[PB[idx % 3]])
                            pt, ptk = ptr.get()
                            S.op("act", lambda e: e.activation(out=pt[:], in_=bank[:], func=AF.Exp),
                                 reads=[PB[idx % 3]], writes=[ptk])
                            return pt, ptk

                        def pv(idx, pt, ptk):
                            h, qj, kt = seq[idx]
                            c, hp, kv = h // 2, h % 2, h // 4
                            g = idx // NT
                            po = pb[4 + g % 2]
                            pok = PB[4 + g % 2]
                            col0 = 128 * kv + (64 if hp == 0 else 0)
                            S.ops("pe", [mm(po[:], v1[:, kt, col0:col0 + 128], pt[:], kt == 0, kt == NT - 1)],
                                  reads=[("v1", kt), ptk], writes=[pok])
                            if kt == NT - 1:
                                rec, reck = recr.get()
                                if hp == 0:
                                    S.op("dve", lambda e: e.reciprocal(out=rec[0:64, :], in_=po[64:128, :]), reads=[pok], writes=[reck])
                                    S.op("dve", lambda e: e.tensor_tensor(out=qT[0:64, c, blk(qj)], in0=po[0:64, :], in1=rec[0:64, :],
                                                                          op=ALU.mult),
                                         reads=[pok, reck], writes=[("q", c, qj)])
                                else:
                                    S.op("dve", lambda e: e.reciprocal(out=rec[64:128, :], in_=po[0:64, :]), reads=[pok], writes=[reck])
                                    S.op("dve", lambda e: e.tensor_tensor(out=qT[64:128, c, blk(qj)], in0=po[64:128, :], in1=rec[64:128, :],
                                                                          op=ALU.mult),
                                         reads=[pok, reck], writes=[("q", c, qj)])

                        n = len(seq)
                        look = 2
                        for i in range(n + look):
                            if i < n:
                                pend.append((i,) + score(i))
                            if i >= look:
                                idx, pt, ptk = pend.pop(0)
                                pv(idx, pt, ptk)
                        S.barrier()
                with contextlib.ExitStack() as ph:
                    al = lambda n, s, d: sb(n, s, d, ph)
                    upad = al("upad", [128, 4, SEQ + 30], BF16)
                    diag = al("diag", [128, 31, 128], BF16)
                    sigr = Rot(al, "c_sig", 2, [128, 512], F32)
                    S.op("dve", lambda e: e.memset(upad[:, :, 0:15], 0.0), writes=["upad_l"])
                    S.op("dve", lambda e: e.memset(upad[:, :, SEQ + 15:SEQ + 30], 0.0), writes=["upad_r"])
                    for c in range(4):
                        wt, wk = wget(("conv", l, c))
                        for j in range(NBLK):
                            ba, bg = pb[j % 2], pb[2 + j % 2]
                            S.ops("pe", [mm(ba[:], wt[:, k, 0:128], A[:, k, blk(j)], k == 0, k == 7) for k in range(8)],
                                  reads=[wk] + [("A", k, j) for k in range(8)], writes=[PB[j % 2]])
                            S.ops("pe", [mm(bg[:], wt[:, k, 128:256], A[:, k, blk(j)], k == 0, k == 7) for k in range(8)],
                                  reads=[wk] + [("A", k, j) for k in range(8)], writes=[PB[2 + j % 2]])
                            sg, sgk = sigr.get()
                            S.op("act", lambda e, sg=sg, bg=bg: e.activation(out=sg[:], in_=bg[:], func=AF.Sigmoid),
                                 reads=[PB[2 + j % 2]], writes=[sgk])
                            S.op("dve", lambda e, sg=sg, ba=ba, c=c, j=j: e.tensor_tensor(
                                out=upad[:, c, 15 + j * 512:15 + (j + 1) * 512], in0=ba[:], in1=sg[:], op=ALU.mult),
                                reads=[PB[j % 2], sgk], writes=[("u", c, j)])
                        wprefetch()
                    S.barrier()
                    for c in range(4):
                        for tp in range(31):
                            S.op("dve", lambda e, tp=tp, c=c: e.tensor_scalar(
                                out=diag[:, tp, :], in0=identb[:], scalar1=wdwT[l][:, c * 31 + tp:c * 31 + tp + 1],
                                scalar2=None, op0=ALU.mult),
                                reads=["identb", "wdwT%d" % l], writes=["diag"])
                        for j in range(NBLK):
                            bank = pb[4 + j % 2]
                            rd = ["diag", "upad_l", "upad_r"] + [("u", c, jj) for jj in range(max(0, j - 1), min(NBLK, j + 2))]
                            S.ops("pe", [mm(bank[:], diag[:, tp, :], upad[:, c, j * 512 + tp:j * 512 + tp + 512],
                                            tp == 0, tp == 30) for tp in range(31)],
                                  reads=rd, writes=[PB[4 + j % 2]])
                            S.op("act", lambda e, bank=bank, c=c, j=j: e.activation(
                                out=A[:, c, blk(j)], in_=bank[:], func=AF.Identity, bias=bdwT[l][:, c:c + 1], scale=1.0),
                                reads=[PB[4 + j % 2], "bdwT%d" % l], writes=[("A", c, j)])
                    S.barrier()
                with contextlib.ExitStack() as ph:
                    al = lambda n, s, d: sb(n, s, d, ph)
                    vsq = Rot(al, "l_vsq", 1, [128, 4, 512], BF16)
                    mur = Rot(al, "l_mu", 1, [128, 512], F32)
                    msr = Rot(al, "l_ms", 1, [128, 512], F32)
                    sdr = Rot(al, "l_sd", 1, [128, 512], F32)
                    tr = Rot(al, "l_t", 2, [128, 512], F32)
                    for j in range(NBLK):
                        vq, vqk = vsq.get()
                        S.op("act", lambda e, vq=vq: e.activation(out=vq[:], in_=A[:, 0:4, blk(j)], func=AF.Square),
                             reads=[("A", c, j) for c in range(4)], writes=[vqk])
                        S.ops("pe", [mm(pb[6][:], onesb[:], A[:, c, blk(j)], c == 0, c == 3) for c in range(4)],
                              reads=[("A", c, j) for c in range(4)] + ["onesb"], writes=[PB[6]])
                        S.ops("pe", [mm(pb[7][:], onesb[:], vq[:, c, :], c == 0, c == 3) for c in range(4)],
                              reads=[vqk, "onesb"], writes=[PB[7]])
                        mu, muk = mur.get()
                        ms, msk = msr.get()
                        sd, sdk = sdr.get()
                        S.op("dve", lambda e, mu=mu: e.tensor_scalar(out=mu[:], in0=pb[6][:], scalar1=1.0 / 512,
                                                                     scalar2=None, op0=ALU.mult),
                             reads=[PB[6]], writes=[muk])
                        S.op("dve", lambda e, mu=mu, ms=ms: e.tensor_tensor(out=ms[:], in0=mu[:], in1=mu[:], op=ALU.mult),
                             reads=[muk], writes=[msk])
                        S.op("dve", lambda e, ms=ms, sd=sd: e.scalar_tensor_tensor(
                            out=sd[:], in0=pb[7][:], scalar=1.0 / 512, in1=ms[:], op0=ALU.mult, op1=ALU.subtract),
                            reads=[PB[7], msk], writes=[sdk])
                        S.op("act", lambda e, sd=sd: e.activation(out=sd[:], in_=sd[:], func=AF.Ln, bias=eps_c, scale=1.0),
                             reads=[sdk, "cvec"], writes=[sdk])
                        S.op("act", lambda e, sd=sd: e.activation(out=sd[:], in_=sd[:], func=AF.Exp, scale=-0.5), reads=[sdk], writes=[sdk])
                        for c in range(4):
                            t, tk = tr.get()
                            S.op("dve", lambda e, t=t, c=c, mu=mu: e.tensor_tensor(
                                out=t[:], in0=A[:, c, blk(j)], in1=mu[:], op=ALU.subtract),
                                reads=[("A", c, j), muk], writes=[tk])
                            S.op("dve", lambda e, t=t, sd=sd: e.tensor_tensor(out=t[:], in0=t[:], in1=sd[:], op=ALU.mult),
                                 reads=[tk, sdk], writes=[tk])
                            S.op("act", lambda e, t=t, c=c: e.activation(
                                out=A[:, c, blk(j)], in_=t[:], func=AF.Silu, bias=bcnT[l][:, c:c + 1],
                                scale=gcnT[l][:, c:c + 1]),
                                reads=[tk, "gcnT%d" % l, "bcnT%d" % l], writes=[("A", c, j)])
                    S.barrier()
                for dh in range(2):
                    wt, wk = wget(("o", l, dh))
                    for j in range(NBLK):
                        for dc in range(4):
                            i = (j * 4 + dc) % 4
                            cch = dh * 4 + dc
                            fns = [mm(pb[i][:], wt[:, k, dc * 128:(dc + 1) * 128],
                                      qT[:, k, blk(j)] if k < 4 else A[:, k - 4, blk(j)], k == 0, k == 7) for k in range(8)]
                            S.ops("pe", fns,
                                  reads=[wk] + [("q", k, j) for k in range(4)] + [("A", k, j) for k in range(4)], writes=[PB[i]])
                            S.op("dve", lambda e, i=i, cch=cch, j=j: e.scalar_tensor_tensor(
                                out=xT[:, cch, blk(j)], in0=pb[i][:], scalar=mod_ap(l, 2, cch, b), in1=xT[:, cch, blk(j)],
                                op0=ALU.mult, op1=ALU.add),
                                reads=[PB[i], ("x", cch, j), ("modT", l, 16 + cch)], writes=[("x", cch, j)])
                    if dh == 0:
                        wprefetch()
                S.barrier()

        TS = 256
        NTILE = 64
        U32 = mybir.dt.uint32
        I32 = mybir.dt.int32

        def moe(b, l):
            w1rows = w1b_d[:, :]
            w2rows = w2b_d[:, :]
            b1rows = b1R_d.rearrange("l r n -> (l r) n")
            b2rows = b2R_d.rearrange("l r n -> (l r) n")
            with contextlib.ExitStack() as ph_g:
                alg = lambda n, s, d: sb(n, s, d, ph_g)
                gate4 = alg("gate4", [128, NT, 4], F32)
                slotu = alg("slotu", [128, NT * 4], U32)
                widxu = alg("widxu", [128, NTILE, 8], U32)
                bidxu = alg("bidxu", [128, NTILE], U32)
                ph_r = contextlib.ExitStack()
                alr = lambda n, s, d: sb(n, s, d, ph_r)
                lgs_all = alr("lgs_all", [128, NT, NE], F32)
                m8_all = alr("m8_all", [128, NT, 8], F32)
                maskb = alr("maskb", [128, NT, NE], BF16)
                slotf = alr("slotf", [128, NT * 4], F32)
                cnt = alr("cnt", [128, NE], F32)
                ntl = alr("ntl", [128, NE], F32)
                incl = alr("incl", [128, NE], F32)
                pst = alr("pst", [128, NE], F32)
                onesf = alr("onesf", [128, NE], F32)
                te = alr("te", [128, NTILE], F32)
                te128 = alr("te128", [128, NTILE], F32)
                tflag = alr("tflag", [128, NTILE], F32)
                widxf = alr("widxf", [128, NTILE, 8], F32)
                bidxf = alr("bidxf", [128, NTILE], F32)
                with contextlib.ExitStack() as ph:
                    al = lambda n, s, d: sb(n, s, d, ph)
                    nmr = Rot(al, "r_nm", 2, [128, 1], F32)
                    ssr = Rot(al, "r_ss", 2, [128, 1], F32)

                    def router(j, h, hk):
                        lg = pb[2]
                        for tt in range(4):
                            fns = [mm(lg[:, tt * NE:(tt + 1) * NE], h[:, k, tt * 128:(tt + 1) * 128], wr32[l][:, k, :], k == 0, False)
                                   for k in range(8)]
                            fns.append(mm(lg[:, tt * NE:(tt + 1) * NE], ones32[0:1, :], brow[l][0:1, :], False, True))
                            S.ops("pe", fns, reads=[(hk, k) for k in range(8)] + ["wr%d" % l, "brow%d" % l, "ones32"],
                                  writes=[(PB[2], tt)])
                        S.op("act", lambda e: e.copy(out=lgs_all[:, 4 * j:4 * j + 4, :],
                                                     in_=lg[:, 0:4 * NE].rearrange("p (t e) -> p t e", e=NE)),
                             reads=[(PB[2], tt) for tt in range(4)], writes=[("lgs", 4 * j + tt) for tt in range(4)])
                        for tt in range(4):
                            t = 4 * j + tt
                            lgt = lgs_all[:, t, :]
                            m8 = m8_all[:, t, :]
                            nm, nmk = nmr.get()
                            ss, ssk = ssr.get()
                            S.op("dve", lambda e: e.max(out=m8, in_=lgt), reads=[("lgs", t)], writes=[("m8", t)])
                            S.op("dve", lambda e: e.tensor_scalar(out=nm[:], in0=m8[:, 0:1], scalar1=-1.0, scalar2=None, op0=ALU.mult),
                                 reads=[("m8", t)], writes=[nmk])
                            S.op("act", lambda e: e.activation(out=gate4[:, t, :], in_=m8[:, 0:4], func=AF.Exp, bias=nm[:, 0:1], scale=1.0),
                                 reads=[("m8", t), nmk], writes=[("g4", t)])
                            S.op("dve", lambda e: e.tensor_reduce(out=ss[:], in_=gate4[:, t, :], axis=mybir.AxisListType.X, op=ALU.add),
                                 reads=[("g4", t)], writes=[ssk])
                            S.op("dve", lambda e: e.reciprocal(out=ss[:], in_=ss[:]), reads=[ssk], writes=[ssk])
                            S.op("dve", lambda e: e.tensor_scalar(out=gate4[:, t, :], in0=gate4[:, t, :], scalar1=ss[:, 0:1], scalar2=None,
                                                                  op0=ALU.mult),
                                 reads=[("g4", t), ssk], writes=[("g4", t)])
                            S.op("dve", lambda e: e.tensor_scalar(out=maskb[:, t, :], in0=lgt, scalar1=m8[:, 3:4], scalar2=None, op0=ALU.is_ge),
                                 reads=[("lgs", t), ("m8", t)], writes=[("mask", t)])

                    norm_mod(b, l, 1, ph, router=router)
                    S.barrier()
                with contextlib.ExitStack() as ph:
                    al = lambda n, s, d: sb(n, s, d, ph)
                    tmpr = Rot(al, "q_tmp", 2, [128, NE], F32)
                    Sr = Rot(al, "q_S", 2, [128, NE], F32)
                    S.ops("pe", [mm(pb[0][:, 0:NE], onesb[:], maskb[:, t, :], t == 0, t == NT - 1) for t in range(NT)],
                          reads=[("mask", t) for t in range(NT)] + ["onesb"], writes=[PB[0]])
                    S.op("dve", lambda e: e.tensor_copy(out=cnt[:], in_=pb[0][:, 0:NE]), reads=[PB[0]], writes=["cnt"])
                    S.op("dve", lambda e: e.tensor_scalar(out=ntl[:], in0=cnt[:], scalar1=0.0, scalar2=None, op0=ALU.is_gt),
                         reads=["cnt"], writes=["ntl"])
                    for jj in range(1, SEQ // TS):
                        S.op("dve", lambda e, jj=jj: e.scalar_tensor_tensor(out=ntl[:], in0=cnt[:], scalar=float(TS * jj), in1=ntl[:],
                                                                            op0=ALU.is_gt, op1=ALU.add),
                             reads=["cnt", "ntl"], writes=["ntl"])
                    S.op("dve", lambda e: e.memset(onesf[:], 1.0), writes=["onesf"])
                    S.op("dve", lambda e: e.tensor_tensor_scan(out=incl[:], data0=onesf[:], data1=ntl[:], initial=0.0,
                                                               op0=ALU.mult, op1=ALU.add),
                         reads=["onesf", "ntl"], writes=["incl"])
                    S.op("dve", lambda e: e.tensor_tensor(out=pst[:], in0=incl[:], in1=ntl[:], op=ALU.subtract),
                         reads=["incl", "ntl"], writes=["pst"])
                    for i in range(NTILE):
                        tmp, tmpk = tmpr.get()
                        S.op("dve", lambda e, i=i, tmp=tmp: e.tensor_scalar(out=tmp[:], in0=incl[:], scalar1=float(i), scalar2=None,
                                                                            op0=ALU.is_le),
                             reads=["incl"], writes=[tmpk])
                        S.op("dve", lambda e, i=i, tmp=tmp: e.tensor_reduce(out=te[:, i:i + 1], in_=tmp[:], axis=mybir.AxisListType.X,
                                                                            op=ALU.add),
                             reads=[tmpk], writes=[("te", i)])
                    tek = [("te", i) for i in range(NTILE)]
                    S.op("dve", lambda e: e.tensor_scalar(out=tflag[:], in0=te[:], scalar1=float(NE), scalar2=bigcol[:, 0:1],
                                                          op0=ALU.is_ge, op1=ALU.mult),
                         reads=tek + ["bigcol"], writes=["tflag"])
                    S.op("dve", lambda e: e.tensor_scalar(out=te[:], in0=te[:], scalar1=float(NE - 1), scalar2=None, op0=ALU.min),
                         reads=tek, writes=["te_all"])
                    S.op("dve", lambda e: e.tensor_scalar(out=te128[:], in0=te[:], scalar1=128.0, scalar2=float(l * NE * 128), op0=ALU.mult, op1=ALU.add),
                         reads=["te_all"], writes=["te128"])
                    S.op("dve", lambda e: e.tensor_scalar(out=bidxf[:], in0=te128[:], scalar1=pcol[:, 0:1], scalar2=None, op0=ALU.add),
                         reads=["te128", "pcol"], writes=["bidxf"])
                    S.op("dve", lambda e: e.tensor_copy(out=bidxu[:], in_=bidxf[:]), reads=["bidxf"], writes=["bidxu"])
                    for c in range(8):
                        S.op("dve", lambda e, c=c: e.tensor_scalar(out=widxf[:, :, c], in0=te128[:], scalar1=8.0, scalar2=basepc[:, c:c + 1],
                                                                   op0=ALU.mult, op1=ALU.add),
                             reads=["te128", "basepc"], writes=[("widxf", c)])

                    S.op("dve", lambda e: e.tensor_copy(out=widxu[:], in_=widxf[:]), reads=[("widxf", c) for c in range(8)], writes=["widxu"])
                    for t in range(NT):
                        bank = pb[1 + t % 2]
                        fns = [mm(bank[:, 0:NE], onesb[:], maskb[:, tp, :], tp == 0, False) for tp in range(t)]
                        fns.append(mm(bank[:, 0:NE], utri[:], maskb[:, t, :], t == 0, True))
                        S.ops("pe", fns, reads=[("mask", tp) for tp in range(t + 1)] + ["onesb", "utri"], writes=[PB[1 + t % 2]])
                        Sv, Sk = Sr.get()
                        S.op("dve", lambda e, Sv=Sv, bank=bank: e.scalar_tensor_tensor(out=Sv[:], in0=pst[:], scalar=float(TS), in1=bank[:, 0:NE],
                                                                                       op0=ALU.mult, op1=ALU.add),
                             reads=["pst", PB[1 + t % 2]], writes=[Sk])
                        for k in range(4):
                            tmp, tmpk = tmpr.get()
                            S.op("dve", lambda e, t=t, k=k, tmp=tmp, Sv=Sv: e.scalar_tensor_tensor(
                                out=tmp[:], in0=lgs_all[:, t, :], scalar=m8_all[:, t, k:k + 1], in1=Sv[:], op0=ALU.is_equal, op1=ALU.mult),
                                reads=[("lgs", t), ("m8", t), Sk], writes=[tmpk])
                            S.op("dve", lambda e, t=t, k=k, tmp=tmp: e.tensor_reduce(out=slotf[:, 4 * t + k:4 * t + k + 1], in_=tmp[:],
                                                                                    axis=mybir.AxisListType.X, op=ALU.add),
                                 reads=[tmpk], writes=[("slotf", t, k)])
                    S.op("dve", lambda e: e.tensor_copy(out=slotu[:], in_=slotf[:]),
                         reads=[("slotf", t, k) for t in range(NT) for k in range(4)], writes=["slotu"])
                    S.barrier()
                ph_r.close()
                with contextlib.ExitStack() as ph:
                    al = lambda n, s, d: sb(n, s, d, ph)
                    htr = Rot(al, "h_tok", 2, [128, D], BF16)
                    hs_ev = {}
                    for t in range(NT):
                        ht, htk = htr.get()
                        bank = pb[t % 2]
                        bview = bank[:].bitcast(BF16)
                        fns = [(lambda e, c=c, bview=bview, t=t: e.transpose(out=bview[:, c * 128:(c + 1) * 128],
                                                                              in_=A[:, c, t * 128:(t + 1) * 128], identity=identb[:]))
                               for c in range(8)]
                        S.ops("pe", fns, reads=[("A", c, t // 4) for c in range(8)] + ["identb"], writes=[PB[t % 2]])
                        if t % 2 == 0:
                            S.op("dve", lambda e, ht=ht, bview=bview: e.tensor_copy(out=ht[:], in_=bview), reads=[PB[t % 2]], writes=[htk])
                        else:
                            S.op("act", lambda e, ht=ht, bview=bview: e.copy(out=ht[:], in_=bview), reads=[PB[t % 2]], writes=[htk])

                        def sc(e, ht=ht, t=t):
                            return [e.indirect_dma_start(out=hs_d[:, :], out_offset=bass.IndirectOffsetOnAxis(
                                ap=slotu[:, 4 * t + k:4 * t + k + 1], axis=0), in_=ht[:, :], in_offset=None) for k in range(4)]
                        hs_ev[htk] = S.dma("pool", sc, "sc_" + htk, reads=[htk, "slotu"], writes=[("HS", t)])
                    S.state["HS"] = {"w": list(hs_ev.values()), "r": []}
                    S.barrier()
                with contextlib.ExitStack() as ph:
                    al = lambda n, s, d: sb(n, s, d, ph)
                    wA0 = al("wA0", [128, 8, 2 * D], BF16)
                    wA1b = al("wA1b", [128, 4, 2 * D], BF16)
                    wAs = [[wA0[:, c, :] for c in range(8)],
                           [A[:, 4 + c, :] for c in range(4)] + [wA1b[:, c, :] for c in range(4)]]
                    wBl = [A[:, c // 2, (c % 2) * D:(c % 2 + 1) * D] for c in range(8)]
                    hgr = Rot(al, "e_hg", 2, [128, 8, TS], BF16)
                    actr = Rot(al, "e_act", 1, [128, 8, TS], BF16)
                    glr = Rot(al, "e_gl", 2, [128, TS], F32)
                    tr = Rot(al, "e_t", 2, [128, TS], F32)
                    b1r = Rot(al, "e_b1", 2, [128, 16], F32)
                    b1pr = Rot(al, "e_b1p", 2, [128, 16], F32)
                    b2r = Rot(al, "e_b2", 2, [128, 8], F32)
                    hsts = [wslot[2][:].rearrange("p a b -> p (a b)").rearrange("p (s d) -> p s d", s=4)[:, 2 * q_:2 * q_ + 2, :]
                            for q_ in range(2)]

                    def hs_load(i_):
                        S.dma("sp", lambda e: e.dma_start(out=hsts[i_ % 2],
                                                          in_=hs_d[i_ * TS:(i_ + 1) * TS, :].rearrange("(s p) d -> p s d", p=128)),
                              "hsld%d" % (i_ % 2), reads=["HS"], writes=[("ws2", i_ % 2)])
                    hs_load(0)
                    yT = wslot[0][:].rearrange("p a b -> p (a b)").bitcast(F32).rearrange("p (c s) -> p c s", c=8)
                    ytok = [wslot[1][:].rearrange("p a b -> p (a b)").bitcast(F32).rearrange("p (s d) -> p s d", s=2)[:, s, :]
                            for s in range(2)]
                    ys_ev = {}
                    cntp = 0
                    for i in range(NTILE):
                        par = i % 2
                        wA = wAs[par]

                        def gw1f(i_, dstl):
                            def f(e):
                                return [e.indirect_dma_start(out=dstl[c], out_offset=None, in_=w1rows,
                                                             in_offset=bass.IndirectOffsetOnAxis(ap=widxu[:, i_, c:c + 1], axis=0)) for c in range(8)]
                            return f

                        def gbf(i_, b1t_, b2t_):
                            def f(e):
                                return [e.indirect_dma_start(out=b1t_[:, :], out_offset=None, in_=b1rows,
                                                             in_offset=bass.IndirectOffsetOnAxis(ap=bidxu[:, i_:i_ + 1], axis=0)),
                                        e.indirect_dma_start(out=b2t_[:, :], out_offset=None, in_=b2rows,
                                                             in_offset=bass.IndirectOffsetOnAxis(ap=bidxu[:, i_:i_ + 1], axis=0))]
                            return f

                        if i == 0:
                            bias_buf = {}
                            b1t, b1k = b1r.get()
                            b2t, b2k = b2r.get()
                            S.dma("pool", gbf(0, b1t, b2t), "gb0", reads=["bidxu"], writes=[b1k, b2k])
                            bias_buf[0] = (b1t, b1k, b2t, b2k)
                            S.dma("pool", gw1f(0, wAs[0]), "gwA0", reads=["widxu", ("wb", l)], writes=[("wA", 0)])

                        def gw2(e, i=i):
                            return [e.indirect_dma_start(out=wBl[c], out_offset=None, in_=w2rows,
                                                         in_offset=bass.IndirectOffsetOnAxis(ap=widxu[:, i, c:c + 1], axis=0)) for c in range(8)]
                        S.dma("pool", gw2, "gwB", reads=["widxu", ("wb", l)], writes=["wB"])
                        if i + 1 < NTILE:
                            nb1t, nb1k = b1r.get()
                            nb2t, nb2k = b2r.get()
                            S.dma("pool", gbf(i + 1, nb1t, nb2t), "gb%d" % ((i + 1) % 2), reads=["bidxu"], writes=[nb1k, nb2k])
                            bias_buf[i + 1] = (nb1t, nb1k, nb2t, nb2k)
                            S.dma("pool", gw1f(i + 1, wAs[1 - par]), "gwA%d" % (1 - par), reads=["widxu", ("wb", l)], writes=[("wA", 1 - par)])
                        b1t, b1k, b2t, b2k = bias_buf.pop(i)
                        b1p, b1pk = b1pr.get()
                        S.op("dve", lambda e, b1p=b1p, b1t=b1t: e.tensor_scalar(out=b1p[:], in0=b1t[:], scalar1=1.0, scalar2=None, op0=ALU.add),
                             reads=[b1k], writes=[b1pk])
                        hstok = hsts[par]
                        if i + 1 < NTILE:
                            hs_load(i + 1)
                        hg, hgk = hgr.get()
                        for half in range(2):
                            bank = pb[4 + half]
                            bview = bank[:].bitcast(BF16)
                            fns = [(lambda e, cc=cc, s=s, bview=bview, half=half: e.transpose(
                                out=bview[:, cc * TS + s * 128:cc * TS + (s + 1) * 128],
                                in_=hstok[:, s, (half * 4 + cc) * 128:(half * 4 + cc + 1) * 128], identity=identb[:]))
                                for cc in range(4) for s in range(2)]
                            S.ops("pe", fns, reads=[("ws2", par), "identb"], writes=[PB[4 + half]])
                            dst = hg[:, half * 4:half * 4 + 4, :]
                            src = bview.rearrange("p (c s) -> p c s", c=4)
                            if half == 0:
                                S.op("dve", lambda e, dst=dst, src=src: e.tensor_copy(out=dst, in_=src), reads=[PB[4 + half]], writes=[(hgk, half)])
                            else:
                                S.op("dve", lambda e, dst=dst, src=src: e.tensor_copy(out=dst, in_=src), reads=[PB[4 + half]], writes=[(hgk, half)])
                        act, actk = actr.get()
                        for nch in range(8):
                            bank = pb[cntp % 2]
                            bk = PB[cntp % 2]
                            cntp += 1
                            fns = [mm(bank[:, 0:TS], wA[k][:, nch * 128:(nch + 1) * 128], hg[:, k, :], k == 0, k == 7) for k in range(8)]
                            fns += [mm(bank[:, TS:2 * TS], wA[k][:, D + nch * 128:D + (nch + 1) * 128], hg[:, k, :], k == 0, k == 7) for k in range(8)]
                            S.ops("pe", fns, reads=[("wA", par), (hgk, 0), (hgk, 1)], writes=[bk])
                            gl, glk = glr.get()
                            tq, tk = tr.get()
                            S.op("dve", lambda e, gl=gl, bank=bank, b1t=b1t, nch=nch: e.tensor_scalar(
                                out=gl[:], in0=bank[:, 0:TS], scalar1=b1t[:, nch:nch + 1], scalar2=7.0, op0=ALU.add, op1=ALU.min),
                                reads=[bk, b1k], writes=[glk])
                            S.op("act", lambda e, gl=gl: e.activation(out=gl[:], in_=gl[:], func=AF.Silu, scale=1.702), reads=[glk], writes=[glk])
                            S.op("dve", lambda e, tq=tq, bank=bank, b1p=b1p, nch=nch: e.tensor_scalar(
                                out=tq[:], in0=bank[:, TS:2 * TS], scalar1=b1p[:, 8 + nch:9 + nch], scalar2=8.0, op0=ALU.add, op1=ALU.min),
                                reads=[bk, b1pk], writes=[tk])
                            S.op("dve", lambda e, tq=tq, gl=gl, act=act, nch=nch: e.scalar_tensor_tensor(
                                out=act[:, nch, :], in0=tq[:], scalar=-6.0, in1=gl[:], op0=ALU.max, op1=ALU.mult),
                                reads=[tk, glk], writes=[(actk, nch)])
                        for dc in range(8):
                            bank = pb[2 + dc % 2]
                            bk = PB[2 + dc % 2]
                            S.ops("pe", [mm(bank[:, 0:TS], wBl[k][:, dc * 128:(dc + 1) * 128], act[:, k, :], k == 0, k == 7)
                                         for k in range(8)],
                                  reads=["wB"] + [(actk, k) for k in range(8)], writes=[bk])
                            S.op("dve", lambda e, bank=bank, dc=dc, b2t=b2t: e.tensor_scalar(
                                out=yT[:, dc, :], in0=bank[:, 0:TS], scalar1=1.0 / 1.702, scalar2=b2t[:, dc:dc + 1], op0=ALU.mult, op1=ALU.add),
                                reads=[bk, b2k], writes=[("ws0", dc)])
                        for s in range(2):
                            for half in range(2):
                                bank = pb[6 + half]
                                fns = [(lambda e, cc=cc, bank=bank, s=s, half=half: e.transpose(
                                    out=bank[:, cc * 128:(cc + 1) * 128], in_=yT[:, half * 4 + cc, s * 128:(s + 1) * 128], identity=ident[:]))
                                    for cc in range(4)]
                                S.ops("pe", fns, reads=[("ws0", half * 4 + cc) for cc in range(4)] + ["ident"], writes=[PB[6 + half]])
                                if half == 0:
                                    S.op("dve", lambda e, bank=bank, s=s: e.tensor_copy(out=ytok[s][:, 0:512], in_=bank[:]),
                                         reads=[PB[6 + half]], writes=[("ws1", s, 0)])
                                else:
                                    S.op("dve", lambda e, bank=bank, s=s: e.tensor_copy(out=ytok[s][:, 512:1024], in_=bank[:]),
                                         reads=[PB[6 + half]], writes=[("ws1", s, 1)])
                            row = i * TS + s * 128
                            ys_ev[s] = S.dma("sp", lambda e, s=s, row=row: e.dma_start(out=ys_d[row:row + 128, :], in_=ytok[s]),
                                             "yst%d" % s, reads=[("ws1", s, 0), ("ws1", s, 1)], writes=[("YS", i, s)])
                    S.state["YS"] = {"w": list(ys_ev.values()), "r": []}
                    S.barrier()
                    for kk in ("ws0", "ws1", "ws2"):
                        evs = []
                        for key, stt in S.state.items():
                            if isinstance(key, tuple) and key[0] == kk:
                                evs += stt["w"] + stt["r"]
                        base = S.state.setdefault(kk, {"w": [], "r": []})
                        base["r"] = base["r"] + evs
                with contextlib.ExitStack() as ph:
                    al = lambda n, s, d: sb(n, s, d, ph)
                    ykr = Rot(al, "c_yk", 8, [128, D], F32)
                    accr = Rot(al, "c_acc", 2, [128, D], F32)
                    gt2 = lambda cch: mod_ap(l, 5, cch, b)
                    for t in range(NT):
                        yks = []
                        for k in range(4):
                            yk, ykk = ykr.get()
                            S.dma("pool", lambda e, yk=yk, t=t, k=k: e.indirect_dma_start(
                                out=yk[:, :], out_offset=None, in_=ys_d[:, :],
                                in_offset=bass.IndirectOffsetOnAxis(ap=slotu[:, 4 * t + k:4 * t + k + 1], axis=0)),
                                "g_" + ykk, reads=["YS", "slotu"], writes=[ykk])
                            yks.append((yk, ykk))
                        acc, acck = accr.get()
                        S.op("dve", lambda e, acc=acc, yk=yks[0][0], t=t: e.tensor_scalar(
                            out=acc[:], in0=yk[:], scalar1=gate4[:, t, 0:1], scalar2=None, op0=ALU.mult),
                            reads=[yks[0][1], ("g4", t)], writes=[acck])
                        for k in range(1, 4):
                            S.op("dve", lambda e, acc=acc, yk=yks[k][0], t=t, k=k: e.scalar_tensor_tensor(
                                out=acc[:], in0=yk[:], scalar=gate4[:, t, k:k + 1], in1=acc[:], op0=ALU.mult, op1=ALU.add),
                                reads=[yks[k][1], ("g4", t), acck], writes=[acck])
                        for half in range(2):
                            bank = pb[(2 * t + half) % 4]
                            bk = PB[(2 * t + half) % 4]
                            fns = [(lambda e, cc=cc, bank=bank, acc=acc, half=half: e.transpose(
                                out=bank[:, cc * 128:(cc + 1) * 128], in_=acc[:, (half * 4 + cc) * 128:(half * 4 + cc + 1) * 128],
                                identity=ident[:])) for cc in range(4)]
                            S.ops("pe", fns, reads=[acck, "ident"], writes=[bk])
                            for cc in range(4):
                                cch = half * 4 + cc
                                S.op("dve", lambda e, bank=bank, cc=cc, cch=cch, t=t: e.scalar_tensor_tensor(
                                    out=xT[:, cch, t * 128:(t + 1) * 128], in0=bank[:, cc * 128:(cc + 1) * 128], scalar=gt2(cch),
                                    in1=xT[:, cch, t * 128:(t + 1) * 128], op0=ALU.mult, op1=ALU.add),
                                    reads=[bk, ("x", cch, t // 4), ("modT", l, 40 + cch)], writes=[("x", cch, t // 4)])
                    S.barrier()

        def cast_layer(l):
            for ex in range(NE):
                r0 = (l * NE + ex) * D

                def f(e, ex=ex, r0=r0):
                    return [e.dma_start(out=w1b_d[r0:r0 + D, :], in_=w1_d[l, ex]),
                            e.dma_start(out=w2b_d[r0:r0 + D, :], in_=w2_d[l, ex])]
                S.dma("pool", f, "cast%d" % l, writes=[("wcast", l, ex)])
            S.state[("wb", l)] = {"w": [(S.dma_sem["cast%d" % l][0], S.dma_sem["cast%d" % l][1])], "r": []}

        cast_layer(0)
        for b in range(NB):
            load_x(b)
            done = (stop == ("load", 0))
            for l in range(NL):
                if done:
                    break
                mixer(b, l)
                if stop == ("mixer", l):
                    done = True
                    break
                moe(b, l)
                if b == 0 and l == 0 and NL > 1:
                    cast_layer(1)
                if stop == ("moe", l):
                    done = True
                    break
            if done:
                store_x(b, False)
                break
            store_x(b, stop is None)
        S.finish([("out", "f_o0"), ("out", "f_o1")])
        build.stats = dict(ticks=dict(S.tick), waits=S.n_wait, ops=S.n_ops, nsem=len(S.sems))
    return nc


def _rope_tables():
    freqs = (np.float32(10000.0) ** (-np.arange(16, dtype=np.float32) / np.float32(16))).astype(np.float32)
    tok = np.arange(SEQ)
    row = (tok // 64).astype(np.float32)
    col = (tok % 64).astype(np.float32)
    cosT = np.zeros((128, SEQ), np.float32)
    sinT = np.zeros((128, SEQ), np.float32)
    for p in range(128):
        d = p % 64
        ang = (row if d < 32 else col) * freqs[d % 16]
        cosT[p] = np.cos(ang.astype(np.float32))
        sinT[p] = np.sin(ang.astype(np.float32))
    pm = np.zeros((128, 128), np.float32)
    for m in range(128):
        i = m % 32
        if i < 16:
            pm[m + 16, m] = -1.0
        else:
            pm[m - 16, m] = 1.0
    return cosT, sinT, pm


def _prep_shared(inp):
    f = lambda a: np.ascontiguousarray(np.asarray(a, dtype=np.float32))
    w_in = f(inp["w_in"])
    q = w_in[:, :, 0:512]
    k0 = w_in[:, :, 512:576]
    k1 = w_in[:, :, 576:640]
    v = w_in[:, :, 640:768]
    ca = w_in[:, :, 768:1280]
    cg = w_in[:, :, 1280:1792]
    parts = [q, k0, k0, k1, k1, v]
    for c in range(4):
        parts += [ca[:, :, c * 128:(c + 1) * 128], cg[:, :, c * 128:(c + 1) * 128]]
    w_in_r = np.ascontiguousarray(np.concatenate(parts, axis=2))
    assert w_in_r.shape[2] == WIN

    def fm(a, nch):
        a = f(a)
        return np.ascontiguousarray(a.reshape(a.shape[0], nch, 128).transpose(0, 2, 1))

    cosT, sinT, pm = _rope_tables()
    sh = dict(
        w_mod=f(inp["w_mod"]), b_modT=fm(inp["b_mod"], 48), g_mixT=fm(inp["g_mix"], 8), g_ffnT=fm(inp["g_ffn"], 8),
        g_finT=fm(f(inp["g_final"])[None], 8)[0], w_in_r=w_in_r,
        gq2=np.ascontiguousarray(np.tile(f(inp["g_q"]), (1, 2))[:, :, None]),
        gk2=np.ascontiguousarray(np.tile(f(inp["g_k"]), (1, 2))[:, :, None]),
        wdwT=np.ascontiguousarray(f(inp["w_dw"]).reshape(2, 31, 4, 128).transpose(0, 3, 2, 1).reshape(2, 128, 124)),
        b_dwT=fm(inp["b_dw"], 4), g_cnT=fm(inp["g_cn"], 4), b_cnT=fm(inp["b_cn"], 4),
        w_out=f(inp["w_out"]), w_router=f(inp["w_router"]), b_router=f(inp["b_router"]),
        w1=f(inp["w1"]),
        b1R=np.ascontiguousarray(f(inp["b1"]).reshape(2, NE, 16, 128).transpose(0, 1, 3, 2).reshape(2, NE * 128, 16)),
        w2=f(inp["w2"]),
        b2R=np.ascontiguousarray(f(inp["b2"]).reshape(2, NE, 8, 128).transpose(0, 1, 3, 2).reshape(2, NE * 128, 8)),
        cosT=cosT, sinT=sinT, pmat=pm,
    )
    return sh


def kernel(**inputs):
    n = 8
    NB = 4
    sh = _prep_shared(inputs)
    x = np.asarray(inputs["x"], dtype=np.float32)
    c = np.asarray(inputs["c"], dtype=np.float32)
    nc = build(NB=NB, NL=2)
    in_maps = []
    for i in range(n):
        m = dict(sh)
        m["x"] = np.ascontiguousarray(x[i * NB:(i + 1) * NB])
        cc = c[i * NB:(i + 1) * NB]
        m["cT"] = np.ascontiguousarray(cc.reshape(NB, 8, 128).transpose(2, 1, 0))
        in_maps.append(m)
    res = run_bass_kernel_spmd(nc, in_maps, core_ids=list(range(n)))
    return np.concatenate([np.asarray(r["out"]) for r in res.results], axis=0).astype(np.float32)
```

```python
import contextlib
import numpy as np
import concourse.bass as bass
import concourse.mybir as mybir
from concourse.bass_utils import run_bass_kernel_spmd

F32 = mybir.dt.float32
BF16 = mybir.dt.bfloat16
AF = mybir.ActivationFunctionType
ALU = mybir.AluOpType

D = 1024
SEQ = 2048
NBLK = 4
NT = 16
NE = 32
EPS = 1e-6
WIN = 1920


class Sched:
    ENG = ("pe", "dve", "act", "pool", "sp")
    LIMIT = 30000

    def __init__(self, nc, stack):
        self.nc = nc
        self.stack = stack
        self.engs = {"pe": nc.tensor, "dve": nc.vector, "act": nc.scalar,
                     "pool": nc.gpsimd, "sp": nc.sync}
        self.tick_sem = {e: stack.enter_context(nc.semaphore("tk_" + e)) for e in self.ENG}
        self.tick = {e: 0 for e in self.ENG}
        self.tick_sid = {e: "tk_" + e for e in self.ENG}
        self.epoch = {}
        self.seen = {e: {} for e in self.ENG}
        self.sems = {}
        for e in self.ENG:
            self.sems["tk_" + e] = self.tick_sem[e]
        self.dma_sem = {}
        self.dma_gen = 0
        self.state = {}
        self.n_wait = 0
        self.n_ops = 0

    def _need(self, e, ev):
        sid, val = ev
        if self.seen[e].get(sid, 0) >= val:
            return
        self.seen[e][sid] = val
        self.engs[e].wait_ge(self.sems[sid], val)
        self.n_wait += 1

    def _deps(self, e, reads, writes):
        for k in reads:
            st = self.state.get(k)
            if st:
                for ev in st["w"]:
                    self._need(e, ev)
        for k in writes:
            st = self.state.get(k)
            if st:
                for ev in st["w"]:
                    self._need(e, ev)
                for ev in st["r"]:
                    self._need(e, ev)

    def _record(self, ev, reads, writes):
        for k in writes:
            self.state[k] = {"w": [ev], "r": []}
        for k in reads:
            st = self.state.setdefault(k, {"w": [], "r": []})
            st["r"].append(ev)
            if len(st["r"]) > 16:
                best = {}
                for s, v in st["r"]:
                    if best.get(s, 0) < v:
                        best[s] = v
                st["r"] = list(best.items())

    def _roll(self, e):
        if self.tick[e] >= self.LIMIT:
            self.epoch[e] = self.epoch.get(e, 0) + 1
            sid = "tk_%s_%d" % (e, self.epoch[e])
            self.sems[sid] = self.stack.enter_context(self.nc.semaphore(sid))
            self.tick_sem[e] = self.sems[sid]
            self.tick_sid[e] = sid
            self.tick[e] = 0

    def barrier(self):
        cur = [(self.tick_sid[e], self.tick[e]) for e in self.ENG if self.tick[e] > 0]
        for e in self.ENG:
            for ev in cur:
                self._need(e, ev)
        for k, st in self.state.items():
            st["w"] = [ev for ev in st["w"] if not ev[0].startswith("tk_")]
            st["r"] = [ev for ev in st["r"] if not ev[0].startswith("tk_")]

    def op(self, e, fn, reads=(), writes=()):
        self._deps(e, reads, writes)
        self._roll(e)
        self.tick[e] += 1
        ev = (self.tick_sid[e], self.tick[e])
        fn(self.engs[e]).then_inc(self.tick_sem[e], 1)
        self._record(ev, reads, writes)
        self.n_ops += 1
        return ev

    def ops(self, e, fns, reads=(), writes=()):
        self._deps(e, reads, writes)
        self._roll(e)
        self.tick[e] += 1
        ev = (self.tick_sid[e], self.tick[e])
        for fn in fns[:-1]:
            fn(self.engs[e])
        fns[-1](self.engs[e]).then_inc(self.tick_sem[e], 1)
        self._record(ev, reads, writes)
        self.n_ops += len(fns)
        return ev

    def dma(self, q, fn, key, reads=(), writes=()):
        self._deps(q, reads, writes)
        if key not in self.dma_sem:
            sid = "dma_" + str(key)
            self.sems[sid] = self.stack.enter_context(self.nc.semaphore(sid))
            self.dma_sem[key] = [sid, 0]
        sid, cnt = self.dma_sem[key]
        if cnt >= self.LIMIT:
            self.dma_gen += 1
            sid = "dma_%s_g%d" % (key, self.dma_gen)
            self.sems[sid] = self.stack.enter_context(self.nc.semaphore(sid))
            self.dma_sem[key] = [sid, 0]
            cnt = 0
        r = fn(self.engs[q])
        if not isinstance(r, (list, tuple)):
            r = [r]
        for ins in r:
            ins.then_inc(self.sems[sid], 16)
        cnt += 16 * len(r)
        self.dma_sem[key][1] = cnt
        ev = (sid, cnt)
        self._record(ev, reads, writes)
        return ev

    def finish(self, keys):
        for k in keys:
            st = self.state.get(k)
            if st:
                for ev in st["w"] + st["r"]:
                    self._need("sp", ev)


class Rot:
    def __init__(self, alloc, name, n, shape, dt):
        self.t = [alloc(name + str(i), shape, dt) for i in range(n)]
        self.k = [name + str(i) for i in range(n)]
        self.i = 0

    def get(self):
        j = self.i % len(self.t)
        self.i += 1
        return self.t[j], self.k[j]


def _plan(NB, NL, NEXP):
    plan = []
    for l in range(NL):
        for j in range(12):
            plan.append(("mod", l, j))
    for b in range(NB):
        for l in range(NL):
            plan.append(("q", l))
            plan.append(("kkv", l))
            for c in range(4):
                plan.append(("conv", l, c))
            for dh in range(2):
                plan.append(("o", l, dh))
    return plan


def build(NB=4, NL=2, NEXP=NE, stop=None):
    nc = bass.Bass("TRN2", target_bir_lowering=False)

    def din(name, shape, dt=F32):
        return nc.dram_tensor(name, list(shape), dt, kind="ExternalInput").ap()

    x_d = din("x", [NB, SEQ, D])
    cT_d = din("cT", [128, 8, NB])
    wmod_d = din("w_mod", [2, D, 6 * D])
    bmodT_d = din("b_modT", [2, 128, 48])
    gmixT_d = din("g_mixT", [2, 128, 8])
    gffnT_d = din("g_ffnT", [2, 128, 8])
    gfinT_d = din("g_finT", [128, 8])
    win_d = din("w_in_r", [2, D, WIN])
    gq2_d = din("gq2", [2, 128, 1])
    gk2_d = din("gk2", [2, 128, 1])
    wdwT_d = din("wdwT", [2, 128, 4 * 31])
    bdwT_d = din("b_dwT", [2, 128, 4])
    gcnT_d = din("g_cnT", [2, 128, 4])
    bcnT_d = din("b_cnT", [2, 128, 4])
    wout_d = din("w_out", [2, D, D])
    wr_d = din("w_router", [2, D, NE])
    br_d = din("b_router", [2, NE])
    w1_d = din("w1", [2, NEXP, D, 2 * D])
    b1R_d = din("b1R", [2, NE * 128, 16])
    b2R_d = din("b2R", [2, NE * 128, 8])
    b2f_d = din("b2flat", [2 * NE, D])
    hs_d = nc.dram_tensor("hs_scratch", [64 * 256, D], BF16, kind="Internal").ap()
    ys_d = nc.dram_tensor("ys_scratch", [64 * 256, D], F32, kind="Internal").ap()
    wmodb_d = nc.dram_tensor("wmod_bf16", [2, D, 6 * D], BF16, kind="Internal").ap()
    winb_d = nc.dram_tensor("win_bf16", [2, D, WIN], BF16, kind="Internal").ap()
    woutb_d = nc.dram_tensor("wout_bf16", [2, D, D], BF16, kind="Internal").ap()
    w1b_d = nc.dram_tensor("w1_bf16", [2 * NE * 128, 8 * 2 * D], BF16, kind="Internal").ap()
    w2b_d = nc.dram_tensor("w2_bf16", [2 * NE * 128, 8 * D], BF16, kind="Internal").ap()
    w2_d = din("w2", [2, NEXP, D, D])
    cos_d = din("cosT", [128, SEQ])
    sin_d = din("sinT", [128, SEQ])
    pm_d = din("pmat", [128, 128])
    out_d = nc.dram_tensor("out", [NB, SEQ, D], F32, kind="ExternalOutput").ap()

    with contextlib.ExitStack() as st:
        S = Sched(nc, st)

        uniq = [0]

        def sb(name, shape, dt, stack=st):
            uniq[0] += 1
            return stack.enter_context(nc.sbuf_tensor("%s_s%d" % (name, uniq[0]), list(shape), dt))

        xT = sb("xT", [128, 8, SEQ], F32)
        A = sb("A", [128, 8, SEQ], BF16)
        wslot = [sb("wslot%d" % i, [128, 8, 512], BF16) for i in range(3)]
        cosT = sb("cos", [128, SEQ], BF16)
        sinT = sb("sin", [128, SEQ], BF16)
        pmat = sb("pmat_s", [128, 128], BF16)
        ident = sb("ident", [128, 128], F32)
        identb = sb("identb", [128, 128], BF16)
        onesb = sb("onesb", [128, 128], BF16)
        onesbd = sb("onesbd", [128, 128], BF16)
        ones32 = sb("ones32", [128, 128], F32)
        cvec = sb("cvec", [128, 8], F32)
        cact = sb("cact", [128, 8, NB], F32)
        cactb = sb("cactb", [128, 8, NB], BF16)
        modT = [sb("modT%d" % l, [128, 48, NB], F32) for l in range(NL)]
        bmodT = [sb("bmodT%d" % l, [128, 48], F32) for l in range(NL)]
        gmixT = [sb("gmixT%d" % l, [128, 8], F32) for l in range(NL)]
        gffnT = [sb("gffnT%d" % l, [128, 8], F32) for l in range(NL)]
        gfinT = sb("gfinT", [128, 8], F32)
        gq2 = [sb("gq2_%d" % l, [128, 1], F32) for l in range(NL)]
        gk2 = [sb("gk2_%d" % l, [128, 1], F32) for l in range(NL)]
        wdwT = [sb("wdwT%d" % l, [128, 4 * 31], F32) for l in range(NL)]
        bdwT = [sb("bdwT%d" % l, [128, 4], F32) for l in range(NL)]
        gcnT = [sb("gcnT%d" % l, [128, 4], F32) for l in range(NL)]
        bcnT = [sb("bcnT%d" % l, [128, 4], F32) for l in range(NL)]
        wr32 = [sb("wr32_%d" % l, [128, 8, NE], F32) for l in range(NL)]
        brow = [sb("brow%d" % l, [1, NE], F32) for l in range(NL)]
        utri = sb("utri", [128, 128], BF16)
        utri32 = sb("utri32", [128, 128], F32)
        pcol = sb("pcol", [128, 1], F32)
        bigcol = sb("bigcol", [128, 1], F32)
        basepc = sb("basepc", [128, 8], F32)
        ipc = sb("ipc", [128, 8], mybir.dt.int32)
        gmod = sb("gmod", [128, 8], F32)
        pb = [st.enter_context(nc.psum_tensor("pb%d" % i, [128, 512], F32)) for i in range(8)]
        PB = ["pb%d" % i for i in range(8)]

        const_keys = []

        def cload(q, dst, src, key):
            S.dma(q, lambda e: e.dma_start(out=dst, in_=src), "const", writes=[key])
            const_keys.append(key)

        cload("pool", cosT[:], cos_d, "cos")
        cload("pool", sinT[:], sin_d, "sin")
        cload("pool", pmat[:], pm_d, "pmat")
        cload("sp", cact[:], cT_d, "cact")
        cload("sp", gfinT[:], gfinT_d, "gfinT")
        for l in range(NL):
            cload("sp", bmodT[l][:], bmodT_d[l], "bmodT%d" % l)
            cload("sp", gmixT[l][:], gmixT_d[l], "gmixT%d" % l)
            cload("sp", gffnT[l][:], gffnT_d[l], "gffnT%d" % l)
            cload("sp", gq2[l][:], gq2_d[l], "gq2%d" % l)
            cload("sp", gk2[l][:], gk2_d[l], "gk2%d" % l)
            cload("sp", wdwT[l][:], wdwT_d[l], "wdwT%d" % l)
            cload("sp", bdwT[l][:], bdwT_d[l], "bdwT%d" % l)
            cload("sp", gcnT[l][:], gcnT_d[l], "gcnT%d" % l)
            cload("sp", bcnT[l][:], bcnT_d[l], "bcnT%d" % l)
            cload("sp", wr32[l][:], wr_d[l].rearrange("(c p) n -> p c n", p=128), "wr%d" % l)
            cload("sp", brow[l][:], br_d[l:l + 1, :], "brow%d" % l)
        fin = (S.dma_sem["const"][0], S.dma_sem["const"][1])
        for k in const_keys:
            S.state[k] = {"w": [fin], "r": []}

        S.op("pool", lambda e: e.memset(ident[:], 0.0), writes=["ident"])
        S.op("pool", lambda e: e.affine_select(out=ident[:], in_=ident[:], pattern=[[-1, 128]],
                                               compare_op=ALU.not_equal, fill=1.0, base=0,
                                               channel_multiplier=1),
             reads=["ident"], writes=["ident"])
        S.op("dve", lambda e: e.tensor_copy(out=identb[:], in_=ident[:]), reads=["ident"], writes=["identb"])
        S.op("pool", lambda e: e.memset(utri32[:], 1.0), writes=["utri32"])
        S.op("pool", lambda e: e.affine_select(out=utri32[:], in_=utri32[:], pattern=[[1, 128]],
                                               compare_op=ALU.is_gt, fill=0.0, base=0, channel_multiplier=-1),
             reads=["utri32"], writes=["utri32"])
        S.op("dve", lambda e: e.tensor_copy(out=utri[:], in_=utri32[:]), reads=["utri32"], writes=["utri"])
        S.op("pool", lambda e: e.iota(ipc[:], pattern=[[128, 8]], base=0, channel_multiplier=1), writes=["ipc"])
        S.op("dve", lambda e: e.tensor_copy(out=basepc[:], in_=ipc[:]), reads=["ipc"], writes=["basepc"])
        S.op("dve", lambda e: e.tensor_copy(out=pcol[:], in_=ipc[:, 0:1]), reads=["ipc"], writes=["pcol"])
        S.op("dve", lambda e: e.tensor_scalar(out=bigcol[:], in0=pcol[:], scalar1=0.5, scalar2=1048576.0, op0=ALU.is_gt, op1=ALU.mult),
             reads=["pcol"], writes=["bigcol"])
        S.op("dve", lambda e: e.memset(onesb[:], 1.0), writes=["onesb"])
        S.op("dve", lambda e: e.memset(ones32[:], 1.0), writes=["ones32"])
        S.op("dve", lambda e: e.memset(onesbd[:], 0.0), writes=["onesbd"])
        S.op("dve", lambda e: e.memset(onesbd[0:64, 0:64], 1.0), reads=["onesbd"], writes=["onesbd"])
        S.op("dve", lambda e: e.memset(onesbd[64:128, 64:128], 1.0), reads=["onesbd"], writes=["onesbd"])
        S.op("dve", lambda e: e.memset(cvec[:, 0:1], EPS), writes=["cvec"])
        S.op("dve", lambda e: e.memset(cvec[:, 1:2], 64.0 * EPS), reads=["cvec"], writes=["cvec"])
        S.op("dve", lambda e: e.memset(cvec[:, 2:3], 0.0), reads=["cvec"], writes=["cvec"])
        eps_c = cvec[:, 0:1]
        eps64_c = cvec[:, 1:2]
        zero_c = cvec[:, 2:3]
        S.op("act", lambda e: e.activation(out=cactb[:], in_=cact[:], func=AF.Silu),
             reads=["cact"], writes=["cactb"])
        for l in range(NL):
            S.op("dve", lambda e, l=l: e.tensor_scalar(out=gk2[l][:], in0=gk2[l][:], scalar1=8.0,
                                                       scalar2=None, op0=ALU.mult),
                 reads=["gk2%d" % l], writes=["gk2%d" % l])

        plan = _plan(NB, NL, NEXP)
        wstate = {"next": 0, "cons": 0}
        pgrp = []
        g = 0
        for idx_, d_ in enumerate(plan):
            if d_[0] == "q":
                g += 1
            pgrp.append(g)

        def piece_dma(desc, slot):
            kind = desc[0]
            l = desc[1]
            if kind == "mod":
                j = desc[2]
                src = wmodb_d[l].rearrange("(c p) n -> p c n", p=128)[:, :, j * 512:(j + 1) * 512]
                dst = slot[:, :, 0:512]
            elif kind == "conv":
                c = desc[2]
                src = winb_d[l].rearrange("(c p) n -> p c n", p=128)[:, :, 896 + 256 * c:896 + 256 * (c + 1)]
                dst = slot[:, :, 0:256]
            elif kind == "q":
                src = winb_d[l].rearrange("(c p) n -> p c n", p=128)[:, :, 0:512]
                dst = slot[:, :, 0:512]
            elif kind == "kkv":
                src = winb_d[l].rearrange("(c p) n -> p c n", p=128)[:, :, 512:896]
                dst = slot[:, :, 0:384]
            elif kind == "o":
                dh = desc[2]
                src = woutb_d[l].rearrange("(c p) n -> p c n", p=128)[:, :, dh * 512:(dh + 1) * 512]
                dst = slot[:, :, 0:512]
            elif kind == "w1":
                e, pg = desc[2], desc[3]
                v = w1_d[l, e].rearrange("(c p) n -> p c n", p=128)
                src = [v[:, :, 256 * pg:256 * (pg + 1)], v[:, :, 1024 + 256 * pg:1024 + 256 * (pg + 1)]]
                dst = [slot[:, :, 0:256], slot[:, :, 256:512]]
            elif kind == "w2":
                e, dh = desc[2], desc[3]
                src = w2_d[l, e].rearrange("(c p) n -> p c n", p=128)[:, :, dh * 512:(dh + 1) * 512]
                dst = slot[:, :, 0:512]
            if not isinstance(dst, list):
                dst, src = [dst], [src]
            return dst, src

        def wget(desc):
            i = wstate["cons"]
            assert plan[i] == desc, (plan[i], desc)
            while wstate["next"] < len(plan) and wstate["next"] <= i + 2 and pgrp[wstate["next"]] == pgrp[i]:
                j = wstate["next"]
                dst, src = piece_dma(plan[j], wslot[j % 3])
                S.dma("sp", lambda e, dst=dst, src=src: [e.dma_start(out=d_, in_=s_) for d_, s_ in zip(dst, src)],
                      "ws%d" % (j % 3), reads=["mcast"], writes=["ws%d" % (j % 3)])
                wstate["next"] += 1
            wstate["cons"] += 1
            return wslot[i % 3], "ws%d" % (i % 3)

        def wprefetch():
            i = wstate["cons"]
            while wstate["next"] < len(plan) and wstate["next"] <= i + 2 and pgrp[wstate["next"]] == pgrp[max(i - 1, 0)]:
                j = wstate["next"]
                dst, src = piece_dma(plan[j], wslot[j % 3])
                S.dma("sp", lambda e, dst=dst, src=src: [e.dma_start(out=d_, in_=s_) for d_, s_ in zip(dst, src)],
                      "ws%d" % (j % 3), reads=["mcast"], writes=["ws%d" % (j % 3)])
                wstate["next"] += 1

        def blk(j):
            return slice(j * 512, (j + 1) * 512)

        def mm(out, lhsT, rhs, start, stop):
            return lambda e: e.matmul(out, lhsT=lhsT, rhs=rhs, start=start, stop=stop)

        for l in range(NL):
            S.dma("pool", lambda e, l=l: [e.dma_start(out=wmodb_d[l], in_=wmod_d[l]), e.dma_start(out=winb_d[l], in_=win_d[l]),
                                          e.dma_start(out=woutb_d[l], in_=wout_d[l])], "mcast", writes=[("mcast", l)])
        S.state["mcast"] = {"w": [(S.dma_sem["mcast"][0], S.dma_sem["mcast"][1])], "r": []}

        for l in range(NL):
            for j in range(12):
                wt, wk = wget(("mod", l, j))
                for jj in range(4):
                    col = j * 4 + jj
                    bank = pb[col % 2]
                    fns = [mm(bank[:, 0:NB], wt[:, k, jj * 128:(jj + 1) * 128], cactb[:, k, :], k == 0, k == 7)
                           for k in range(8)]
                    S.ops("pe", fns, reads=[wk, "cactb"], writes=[PB[col % 2]])
                    S.op("dve", lambda e, bank=bank, l=l, col=col: e.tensor_scalar(
                        out=modT[l][:, col, :], in0=bank[:, 0:NB], scalar1=bmodT[l][:, col:col + 1],
                        scalar2=None, op0=ALU.add),
                        reads=[PB[col % 2], "bmodT%d" % l], writes=[("modT", l, col)])
                wprefetch()
        S.barrier()

        def mod_ap(l, which, c, b):
            return modT[l][:, which * 8 + c, b:b + 1]

        def norm_stats(stk, j, sqr, sdr, scale):
            sq, sqk = sqr.get()
            S.op("act", lambda e: e.activation(out=sq[:], in_=xT[:, :, blk(j)], func=AF.Square),
                 reads=[("x", c, j) for c in range(8)], writes=[sqk])
            bank = pb[j % 2]
            S.ops("pe", [mm(bank[:], onesb[:], sq[:, k, :], k == 0, k == 7) for k in range(8)],
                  reads=[sqk, "onesb"], writes=[PB[j % 2]])
            sd, sdk = sdr.get()
            S.op("act", lambda e: e.activation(out=sd[:], in_=bank[:], func=AF.Ln, bias=eps_c, scale=scale),
                 reads=[PB[j % 2], "cvec"], writes=[sdk])
            S.op("act", lambda e: e.activation(out=sd[:], in_=sd[:], func=AF.Exp, scale=-0.5), reads=[sdk], writes=[sdk])
            return sd, sdk

        def load_x(b):
            with contextlib.ExitStack() as ph:
                xin = Rot(lambda n, s, d: sb(n, s, d, ph), "xin", 2, [128, D], F32)
                for t in range(NT):
                    xi, xk = xin.get()
                    S.dma("sp", lambda e, xi=xi, t=t: e.dma_start(out=xi[:], in_=x_d[b, t * 128:(t + 1) * 128, :]),
                          xk, writes=[xk])
                    for half in range(2):
                        bi = (2 * t + half) % 4
                        bank = pb[bi]
                        fns = [(lambda e, cc=cc, bank=bank, xi=xi, half=half: e.transpose(
                            out=bank[:, cc * 128:(cc + 1) * 128],
                            in_=xi[:, (half * 4 + cc) * 128:(half * 4 + cc + 1) * 128], identity=ident[:]))
                            for cc in range(4)]
                        S.ops("pe", fns, reads=[xk, "ident"], writes=[PB[bi]])
                        eng = "dve" if half == 0 else "act"
                        dst = xT[:, half * 4:half * 4 + 4, t * 128:(t + 1) * 128]
                        src = bank[:].rearrange("p (c t) -> p c t", c=4)
                        wr = [("x", half * 4 + cc, t // 4) for cc in range(4)]
                        if eng == "dve":
                            S.op("dve", lambda e, dst=dst, src=src: e.tensor_copy(out=dst, in_=src),
                                 reads=[PB[bi]], writes=wr)
                        else:
                            S.op("act", lambda e, dst=dst, src=src: e.copy(out=dst, in_=src),
                                 reads=[PB[bi]], writes=wr)
                S.barrier()

        def store_x(b, do_norm):
            with contextlib.ExitStack() as ph:
                al = lambda n, s, d: sb(n, s, d, ph)
                sqr = Rot(al, "f_sq", 2, [128, 8, 512], BF16)
                sdr = Rot(al, "f_sd", 2, [128, 512], F32)
                y32 = Rot(al, "f_y", 1, [128, 8, 512], F32)
                ost = Rot(al, "f_o", 2, [128, D], F32)
                for j in range(NBLK):
                    y, yk = y32.get()
                    if do_norm:
                        rs, rsk = norm_stats(ph, j, sqr, sdr, 1.0 / D)
                        for c in range(8):
                            S.op("dve", lambda e, c=c, y=y, rs=rs: e.scalar_tensor_tensor(
                                out=y[:, c, :], in0=xT[:, c, blk(j)], scalar=gfinT[:, c:c + 1], in1=rs[:],
                                op0=ALU.mult, op1=ALU.mult),
                                reads=[("x", c, j), rsk, "gfinT"], writes=[(yk, c)])
                    else:
                        for c in range(8):
                            S.op("dve", lambda e, c=c, y=y: e.tensor_copy(out=y[:, c, :], in_=xT[:, c, blk(j)]),
                                 reads=[("x", c, j)], writes=[(yk, c)])
                    for t in range(4):
                        o, ok = ost.get()
                        for half in range(2):
                            bi = (2 * t + half) % 4
                            bank = pb[bi]
                            fns = [(lambda e, cc=cc, bank=bank, y=y, half=half, t=t: e.transpose(
                                out=bank[:, cc * 128:(cc + 1) * 128],
                                in_=y[:, half * 4 + cc, t * 128:(t + 1) * 128], identity=ident[:]))
                                for cc in range(4)]
                            S.ops("pe", fns, reads=[(yk, half * 4 + cc) for cc in range(4)] + ["ident"],
                                  writes=[PB[bi]])
                            if half == 0:
                                S.op("dve", lambda e, o=o, bank=bank: e.tensor_copy(out=o[:, 0:512], in_=bank[:]),
                                     reads=[PB[bi]], writes=[(ok, 0)])
                            else:
                                S.op("act", lambda e, o=o, bank=bank: e.copy(out=o[:, 512:1024], in_=bank[:]),
                                     reads=[PB[bi]], writes=[(ok, 1)])
                        row = (j * 4 + t) * 128
                        S.dma("sp", lambda e, o=o, row=row: e.dma_start(out=out_d[b, row:row + 128, :], in_=o[:]),
                              "st_" + ok, reads=[(ok, 0), (ok, 1)], writes=[("out", ok)])
                S.barrier()

        def norm_mod(b, l, sub, ph, h32r=None, router=None):
            al = lambda n, s, d: sb(n, s, d, ph)
            sqr = Rot(al, "n_sq", 2, [128, 8, 512], BF16)
            sdr = Rot(al, "n_sd", 2, [128, 512], F32)
            h32r = Rot(al, "n_h", 1, [128, 8, 512], F32)
            gsrc = gmixT[l] if sub == 0 else gffnT[l]
            gkey = ("gmixT%d" if sub == 0 else "gffnT%d") % l
            S.op("dve", lambda e: e.scalar_tensor_tensor(
                out=gmod[:], in0=modT[l][:, (1 + 3 * sub) * 8:(2 + 3 * sub) * 8, b], scalar=1.0, in1=gsrc[:],
                op0=ALU.add, op1=ALU.mult),
                reads=[("modT", l, (1 + 3 * sub) * 8 + c) for c in range(8)] + [gkey], writes=["gmod"])
            for j in range(NBLK):
                rs, rsk = norm_stats(ph, j, sqr, sdr, 1.0 / D)
                h, hk = h32r.get()
                for c in range(8):
                    S.op("dve", lambda e, c=c, h=h, rs=rs: e.scalar_tensor_tensor(
                        out=h[:, c, :], in0=xT[:, c, blk(j)], scalar=gmod[:, c:c + 1], in1=rs[:],
                        op0=ALU.mult, op1=ALU.mult),
                        reads=[("x", c, j), rsk, "gmod"], writes=[(hk, c)])
                    sh = mod_ap(l, 3 * sub, c, b)
                    if router is None:
                        S.op("act", lambda e, c=c, h=h, sh=sh: e.activation(
                            out=A[:, c, blk(j)], in_=h[:, c, :], func=AF.Identity, bias=sh, scale=1.0),
                            reads=[(hk, c), ("modT", l, 3 * sub * 8 + c)], writes=[("A", c, j)])
                    else:
                        S.op("act", lambda e, c=c, h=h, sh=sh: e.activation(
                            out=h[:, c, :], in_=h[:, c, :], func=AF.Identity, bias=sh, scale=1.0),
                            reads=[(hk, c), ("modT", l, 3 * sub * 8 + c)], writes=[(hk, c)])
                        S.op("dve", lambda e, c=c, h=h: e.tensor_copy(out=A[:, c, blk(j)], in_=h[:, c, :]),
                             reads=[(hk, c)], writes=[("A", c, j)])
                if router is not None:
                    router(j, h, hk)

        def mixer(b, l):
            with contextlib.ExitStack() as ph:
                norm_mod(b, l, 0, ph)
                S.barrier()
            with contextlib.ExitStack() as ph_q:
                qT = sb("qT", [128, 4, SEQ], BF16, ph_q)
                with contextlib.ExitStack() as ph_kv:
                    al0 = lambda n, s, d: sb(n, s, d, ph_kv)
                    kz = al0("kz", [128, 4, SEQ], BF16)
                    S.op("dve", lambda e: e.memset(kz[:], 0.0), writes=[("k", kc_, j_) for kc_ in range(2) for j_ in range(NBLK)])
                    v1 = al0("v1", [128, NT, 320], BF16)
                    S.op("dve", lambda e: e.memset(v1[:], 1.0), writes=[("v1", t) for t in range(NT)])
                    with contextlib.ExitStack() as ph:
                        al = lambda n, s, d: sb(n, s, d, ph)
                        sqr = Rot(al, "p_sq", 2, [128, 512], BF16)
                        sdr = Rot(al, "p_sd", 2, [128, 512], F32)
                        qnr = Rot(al, "p_qn", 2, [128, 512], BF16)
                        ar = Rot(al, "p_a", 2, [128, 512], F32)
                        br = Rot(al, "p_b", 2, [128, 512], F32)
                        cnt = [0]

                        def stageA(cs):
                            wt, wk, col0, j = cs["wt"], cs["wk"], cs["col0"], cs["j"]
                            i = cs["i"]
                            pq, kq = pb[i % 2], PB[i % 2]
                            S.ops("pe", [mm(pq[:], wt[:, k, col0:col0 + 128], A[:, k, blk(j)], k == 0, k == 7) for k in range(8)],
                                  reads=[wk] + [("A", k, j) for k in range(8)], writes=[kq])
                            sq, sqk = sqr.get()
                            S.op("act", lambda e: e.activation(out=sq[:], in_=pq[:], func=AF.Square), reads=[kq], writes=[sqk])
                            cs["sq"], cs["sqk"] = sq, sqk

                        def stageB(cs):
                            i = cs["i"]
                            pq, kq = pb[i % 2], PB[i % 2]
                            pss, kss = pb[2 + i % 2], PB[2 + i % 2]
                            sq, sqk = cs["sq"], cs["sqk"]
                            S.ops("pe", [mm(pss[:], onesbd[:], sq[:], True, True)], reads=[sqk, "onesbd"], writes=[kss])
                            sd, sdk = sdr.get()
                            S.op("act", lambda e: e.activation(out=sd[:], in_=pss[:], func=AF.Ln, bias=eps64_c, scale=1.0),
                                 reads=[kss, "cvec"], writes=[sdk])
                            S.op("act", lambda e: e.activation(out=sd[:], in_=sd[:], func=AF.Exp, scale=-0.5), reads=[sdk], writes=[sdk])
                            qn, qnk = qnr.get()
                            gain, gkey = cs["gain"], cs["gkey"]
                            S.op("dve", lambda e: e.scalar_tensor_tensor(out=qn[:], in0=pq[:], scalar=gain[:, 0:1], in1=sd[:],
                                                                         op0=ALU.mult, op1=ALU.mult),
                                 reads=[kq, sdk, gkey], writes=[qnk])
                            cs["qn"], cs["qnk"] = qn, qnk

                        def stageC(cs):
                            i, j = cs["i"], cs["j"]
                            ppq, kpq = pb[4 + i % 2], PB[4 + i % 2]
                            qn, qnk = cs["qn"], cs["qnk"]
                            S.ops("pe", [mm(ppq[:], pmat[:], qn[:], True, True)], reads=[qnk, "pmat"], writes=[kpq])
                            a, ak = ar.get()
                            bb, bk = br.get()
                            S.op("dve", lambda e: e.tensor_tensor(out=a[:], in0=qn[:], in1=cosT[:, blk(j)], op=ALU.mult),
                                 reads=[qnk, "cos"], writes=[ak])
                            S.op("dve", lambda e: e.tensor_tensor(out=bb[:], in0=ppq[:], in1=sinT[:, blk(j)], op=ALU.mult),
                                 reads=[kpq, "sin"], writes=[bk])
                            if cs.get("kc") is None:
                                S.op("dve", lambda e: e.tensor_tensor(out=cs["dst"], in0=a[:], in1=bb[:], op=ALU.add),
                                     reads=[ak, bk], writes=[cs["dkey"]])
                            else:
                                kc_ = cs["kc"]
                                S.op("dve", lambda e: e.tensor_tensor(out=kz[0:64, 2 * kc_, blk(j)], in0=a[0:64, :], in1=bb[0:64, :], op=ALU.add),
                                     reads=[ak, bk, cs["dkey"]], writes=[cs["dkey"]])
                                S.op("dve", lambda e: e.tensor_tensor(out=kz[64:128, 2 * kc_ + 1, blk(j)], in0=a[64:128, :], in1=bb[64:128, :],
                                                                      op=ALU.add),
                                     reads=[ak, bk, cs["dkey"]], writes=[cs["dkey"]])

                        wtq, wkq = wget(("q", l))
                        wtk, wkk = None, None
                        chunks = []
                        for j in range(NBLK):
                            for c in range(4):
                                chunks.append(dict(wt=wtq, wk=wkq, col0=c * 128, j=j, dst=qT[:, c, blk(j)], dkey=("q", c, j),
                                                   gain=gq2[l], gkey="gq2%d" % l))
                        for j in range(NBLK):
                            for kc in range(2):
                                chunks.append(dict(wt=wtk, wk=wkk, col0=kc * 128, j=j, dst=None, kc=kc, dkey=("k", kc, j),
                                                   gain=gk2[l], gkey="gk2%d" % l))
                        for i_, cs in enumerate(chunks):
                            cs["i"] = i_
                        nch_ = len(chunks)
                        for step in range(nch_ + 2):
                            if step == 16:
                                wtk, wkk = wget(("kkv", l))
                                for cs in chunks[16:]:
                                    cs["wt"], cs["wk"] = wtk, wkk
                            if step < nch_:
                                stageA(chunks[step])
                            if 0 <= step - 1 < nch_:
                                stageB(chunks[step - 1])
                            if 0 <= step - 2 < nch_:
                                stageC(chunks[step - 2])
                        wt, wk = wtk, wkk
                        for j in range(NBLK):
                            for tt in range(4):
                                t = j * 4 + tt
                                bank = pb[6 + tt % 2]
                                S.ops("pe", [mm(bank[:, 0:128], A[:, k, t * 128:(t + 1) * 128], wt[:, k, 256:384], k == 0, k == 7)
                                             for k in range(8)],
                                      reads=[wk] + [("A", k, j) for k in range(8)], writes=[PB[6 + tt % 2]])
                                dst = v1[:, t, 64:320].rearrange("p (a b) -> p a b", b=128)[:, :, 0:64]
                                src = bank[:, 0:128].rearrange("p (a b) -> p a b", b=64)
                                S.op("act", lambda e, dst=dst, src=src: e.copy(out=dst, in_=src),
                                     reads=[PB[6 + tt % 2]], writes=[("v1", t)])
                        wprefetch()
                        S.barrier()
                    with contextlib.ExitStack() as ph:
                        al = lambda n, s, d: sb(n, s, d, ph)
                        ptr = Rot(al, "a_pt", 4, [128, 512], BF16)
                        recr = Rot(al, "a_rec", 2, [128, 512], F32)
                        seq = [(h, qj, kt) for h in range(8) for qj in range(NBLK) for kt in range(NT)]
                        pend = []

                        def score(idx):
                            h, qj, kt = seq[idx]
                            c, hp, kv = h // 2, h % 2, h // 4
                            bank = pb[idx % 3]
                            rows = slice(hp * 64, hp * 64 + 64)
                            S.ops("pe", [mm(bank[:], kz[:, 2 * kv + hp, kt * 128:(kt + 1) * 128], qT[:, c, blk(qj)], True, True)],
                                  reads=[("k", kv, kt // 4), ("q", c, qj)], writes=[PB[idx % 3]])
                            pt, ptk = ptr.get()
                            S.op("act", lambda e: e.activation(out=pt[:], in_=bank[:], func=AF.Exp),
                                 reads=[PB[idx % 3]], writes=[ptk])
                            return pt, ptk

                        def pv(idx, pt, ptk):
                            h, qj, kt = seq[idx]
                            c, hp, kv = h // 2, h % 2, h // 4
                            g = idx // NT
                            po = pb[4 + g % 2]
                            pok = PB[4 + g % 2]
                            col0 = 128 * kv + (64 if hp == 0 else 0)
                            S.ops("pe", [mm(po[:], v1[:, kt, col0:col0 + 128], pt[:], kt == 0, kt == NT - 1)],
                                  reads=[("v1", kt), ptk], writes=[pok])
                            if kt == NT - 1:
                                rec, reck = recr.get()
                                if hp == 0:
                                    S.op("dve", lambda e: e.reciprocal(out=rec[0:64, :], in_=po[64:128, :]), reads=[pok], writes=[reck])
                                    S.op("dve", lambda e: e.tensor_tensor(out=qT[0:64, c, blk(qj)], in0=po[0:64, :], in1=rec[0:64, :],
                                                                          op=ALU.mult),
                                         reads=[pok, reck], writes=[("q", c, qj)])
                                else:
                                    S.op("dve", lambda e: e.reciprocal(out=rec[64:128, :], in_=po[0:64, :]), reads=[pok], writes=[reck])
                                    S.op("dve", lambda e: e.tensor_tensor(out=qT[64:128, c, blk(qj)], in0=po[64:128, :], in1=rec[64:128, :],
                                                                          op=ALU.mult),
                                         reads=[pok, reck], writes=[("q", c, qj)])

                        n = len(seq)
                        look = 2
                        for i in range(n + look):
                            if i < n:
                                pend.append((i,) + score(i))
                            if i >= look:
                                idx, pt, ptk = pend.pop(0)
                                pv(idx, pt, ptk)
                        S.barrier()
                with contextlib.ExitStack() as ph:
                    al = lambda n, s, d: sb(n, s, d, ph)
                    upad = al("upad", [128, 4, SEQ + 30], BF16)
                    diag = al("diag", [128, 31, 128], BF16)
                    sigr = Rot(al, "c_sig", 2, [128, 512], F32)
                    S.op("dve", lambda e: e.memset(upad[:, :, 0:15], 0.0), writes=["upad_l"])
                    S.op("dve", lambda e: e.memset(upad[:, :, SEQ + 15:SEQ + 30], 0.0), writes=["upad_r"])
                    for c in range(4):
                        wt, wk = wget(("conv", l, c))
                        for j in range(NBLK):
                            ba, bg = pb[j % 2], pb[2 + j % 2]
                            S.ops("pe", [mm(ba[:], wt[:, k, 0:128], A[:, k, blk(j)], k == 0, k == 7) for k in range(8)],
                                  reads=[wk] + [("A", k, j) for k in range(8)], writes=[PB[j % 2]])
                            S.ops("pe", [mm(bg[:], wt[:, k, 128:256], A[:, k, blk(j)], k == 0, k == 7) for k in range(8)],
                                  reads=[wk] + [("A", k, j) for k in range(8)], writes=[PB[2 + j % 2]])
                            sg, sgk = sigr.get()
                            S.op("act", lambda e, sg=sg, bg=bg: e.activation(out=sg[:], in_=bg[:], func=AF.Sigmoid),
                                 reads=[PB[2 + j % 2]], writes=[sgk])
                            S.op("dve", lambda e, sg=sg, ba=ba, c=c, j=j: e.tensor_tensor(
                                out=upad[:, c, 15 + j * 512:15 + (j + 1) * 512], in0=ba[:], in1=sg[:], op=ALU.mult),
                                reads=[PB[j % 2], sgk], writes=[("u", c, j)])
                        wprefetch()
                    S.barrier()
                    for c in range(4):
                        for tp in range(31):
                            S.op("dve", lambda e, tp=tp, c=c: e.tensor_scalar(
                                out=diag[:, tp, :], in0=identb[:], scalar1=wdwT[l][:, c * 31 + tp:c * 31 + tp + 1],
                                scalar2=None, op0=ALU.mult),
                                reads=["identb", "wdwT%d" % l], writes=["diag"])
                        for j in range(NBLK):
                            bank = pb[4 + j % 2]
                            rd = ["diag", "upad_l", "upad_r"] + [("u", c, jj) for jj in range(max(0, j - 1), min(NBLK, j + 2))]
                            S.ops("pe", [mm(bank[:], diag[:, tp, :], upad[:, c, j * 512 + tp:j * 512 + tp + 512],
                                            tp == 0, tp == 30) for tp in range(31)],
                                  reads=rd, writes=[PB[4 + j % 2]])
                            S.op("act", lambda e, bank=bank, c=c, j=j: e.activation(
                                out=A[:, c, blk(j)], in_=bank[:], func=AF.Identity, bias=bdwT[l][:, c:c + 1], scale=1.0),
                                reads=[PB[4 + j % 2], "bdwT%d" % l], writes=[("A", c, j)])
                    S.barrier()
                with contextlib.ExitStack() as ph:
                    al = lambda n, s, d: sb(n, s, d, ph)
                    vsq = Rot(al, "l_vsq", 1, [128, 4, 512], BF16)
                    mur = Rot(al, "l_mu", 1, [128, 512], F32)
                    msr = Rot(al, "l_ms", 1, [128, 512], F32)
                    sdr = Rot(al, "l_sd", 1, [128, 512], F32)
                    tr = Rot(al, "l_t", 2, [128, 512], F32)
                    for j in range(NBLK):
                        vq, vqk = vsq.get()
                        S.op("act", lambda e, vq=vq: e.activation(out=vq[:], in_=A[:, 0:4, blk(j)], func=AF.Square),
                             reads=[("A", c, j) for c in range(4)], writes=[vqk])
                        S.ops("pe", [mm(pb[6][:], onesb[:], A[:, c, blk(j)], c == 0, c == 3) for c in range(4)],
                              reads=[("A", c, j) for c in range(4)] + ["onesb"], writes=[PB[6]])
                        S.ops("pe", [mm(pb[7][:], onesb[:], vq[:, c, :], c == 0, c == 3) for c in range(4)],
                              reads=[vqk, "onesb"], writes=[PB[7]])
                        mu, muk = mur.get()
                        ms, msk = msr.get()
                        sd, sdk = sdr.get()
                        S.op("dve", lambda e, mu=mu: e.tensor_scalar(out=mu[:], in0=pb[6][:], scalar1=1.0 / 512,
                                                                     scalar2=None, op0=ALU.mult),
                             reads=[PB[6]], writes=[muk])
                        S.op("dve", lambda e, mu=mu, ms=ms: e.tensor_tensor(out=ms[:], in0=mu[:], in1=mu[:], op=ALU.mult),
                             reads=[muk], writes=[msk])
                        S.op("dve", lambda e, ms=ms, sd=sd: e.scalar_tensor_tensor(
                            out=sd[:], in0=pb[7][:], scalar=1.0 / 512, in1=ms[:], op0=ALU.mult, op1=ALU.subtract),
                            reads=[PB[7], msk], writes=[sdk])
                        S.op("act", lambda e, sd=sd: e.activation(out=sd[:], in_=sd[:], func=AF.Ln, bias=eps_c, scale=1.0),
                             reads=[sdk, "cvec"], writes=[sdk])
                        S.op("act", lambda e, sd=sd: e.activation(out=sd[:], in_=sd[:], func=AF.Exp, scale=-0.5), reads=[sdk], writes=[sdk])
                        for c in range(4):
                            t, tk = tr.get()
                            S.op("dve", lambda e, t=t, c=c, mu=mu: e.tensor_tensor(
                                out=t[:], in0=A[:, c, blk(j)], in1=mu[:], op=ALU.subtract),
                                reads=[("A", c, j), muk], writes=[tk])
                            S.op("dve", lambda e, t=t, sd=sd: e.tensor_tensor(out=t[:], in0=t[:], in1=sd[:], op=ALU.mult),
                                 reads=[tk, sdk], writes=[tk])
                            S.op("act", lambda e, t=t, c=c: e.activation(
                                out=A[:, c, blk(j)], in_=t[:], func=AF.Silu, bias=bcnT[l][:, c:c + 1],
                                scale=gcnT[l][:, c:c + 1]),
                                reads=[tk, "gcnT%d" % l, "bcnT%d" % l], writes=[("A", c, j)])
                    S.barrier()
                for dh in range(2):
                    wt, wk = wget(("o", l, dh))
                    for j in range(NBLK):
                        for dc in range(4):
                            i = (j * 4 + dc) % 4
                            cch = dh * 4 + dc
                            fns = [mm(pb[i][:], wt[:, k, dc * 128:(dc + 1) * 128],
                                      qT[:, k, blk(j)] if k < 4 else A[:, k - 4, blk(j)], k == 0, k == 7) for k in range(8)]
                            S.ops("pe", fns,
                                  reads=[wk] + [("q", k, j) for k in range(4)] + [("A", k, j) for k in range(4)], writes=[PB[i]])
                            S.op("dve", lambda e, i=i, cch=cch, j=j: e.scalar_tensor_tensor(
                                out=xT[:, cch, blk(j)], in0=pb[i][:], scalar=mod_ap(l, 2, cch, b), in1=xT[:, cch, blk(j)],
                                op0=ALU.mult, op1=ALU.add),
                                reads=[PB[i], ("x", cch, j), ("modT", l, 16 + cch)], writes=[("x", cch, j)])
                    if dh == 0:
                        wprefetch()
                S.barrier()

        TS = 256
        NTILE = 64
        U32 = mybir.dt.uint32
        I32 = mybir.dt.int32

        def moe(b, l):
            w1rows = w1b_d.rearrange("r (h m) -> (r h) m", h=2)
            w2rows = w2b_d[:, :]
            b1rows = b1R_d.rearrange("l r n -> (l r) n")
            b2rows = b2R_d.rearrange("l r n -> (l r) n")
            with contextlib.ExitStack() as ph_g:
                alg = lambda n, s, d: sb(n, s, d, ph_g)
                gate4 = alg("gate4", [128, NT, 4], F32)
                slotu = alg("slotu", [128, NT * 4], U32)
                widxu = alg("widxu", [128, NTILE, 8], U32)
                bidxu = alg("bidxu", [128, NTILE], U32)
                teiu = alg("teiu", [128, NTILE], U32)
                hidxu = [alg("hidxu%d" % h_, [128, NTILE], U32) for h_ in range(2)]
                ph_r = contextlib.ExitStack()
                alr = lambda n, s, d: sb(n, s, d, ph_r)
                lgs_all = alr("lgs_all", [128, NT, NE], F32)
                m8_all = alr("m8_all", [128, NT, 8], F32)
                maskb = alr("maskb", [128, NT, NE], BF16)
                slotf = alr("slotf", [128, NT * 4], F32)
                cnt = alr("cnt", [128, NE], F32)
                ntl = alr("ntl", [128, NE], F32)
                incl = alr("incl", [128, NE], F32)
                pst = alr("pst", [128, NE], F32)
                onesf = alr("onesf", [128, NE], F32)
                te = alr("te", [128, NTILE], F32)
                te128 = alr("te128", [128, NTILE], F32)
                tflag = alr("tflag", [128, NTILE], F32)
                widxf = alr("widxf", [128, NTILE, 8], F32)
                bidxf = alr("bidxf", [128, NTILE], F32)
                with contextlib.ExitStack() as ph:
                    al = lambda n, s, d: sb(n, s, d, ph)
                    nmr = Rot(al, "r_nm", 2, [128, 1], F32)
                    ssr = Rot(al, "r_ss", 2, [128, 1], F32)

                    def router(j, h, hk):
                        lg = pb[2]
                        for tt in range(4):
                            fns = [mm(lg[:, tt * NE:(tt + 1) * NE], h[:, k, tt * 128:(tt + 1) * 128], wr32[l][:, k, :], k == 0, False)
                                   for k in range(8)]
                            fns.append(mm(lg[:, tt * NE:(tt + 1) * NE], ones32[0:1, :], brow[l][0:1, :], False, True))
                            S.ops("pe", fns, reads=[(hk, k) for k in range(8)] + ["wr%d" % l, "brow%d" % l, "ones32"],
                                  writes=[(PB[2], tt)])
                        S.op("act", lambda e: e.copy(out=lgs_all[:, 4 * j:4 * j + 4, :],
                                                     in_=lg[:, 0:4 * NE].rearrange("p (t e) -> p t e", e=NE)),
                             reads=[(PB[2], tt) for tt in range(4)], writes=[("lgs", 4 * j + tt) for tt in range(4)])
                        for tt in range(4):
                            t = 4 * j + tt
                            lgt = lgs_all[:, t, :]
                            m8 = m8_all[:, t, :]
                            nm, nmk = nmr.get()
                            ss, ssk = ssr.get()
                            S.op("dve", lambda e: e.max(out=m8, in_=lgt), reads=[("lgs", t)], writes=[("m8", t)])
                            S.op("dve", lambda e: e.tensor_scalar(out=nm[:], in0=m8[:, 0:1], scalar1=-1.0, scalar2=None, op0=ALU.mult),
                                 reads=[("m8", t)], writes=[nmk])
                            S.op("act", lambda e: e.activation(out=gate4[:, t, :], in_=m8[:, 0:4], func=AF.Exp, bias=nm[:, 0:1], scale=1.0),
                                 reads=[("m8", t), nmk], writes=[("g4", t)])
                            S.op("dve", lambda e: e.tensor_reduce(out=ss[:], in_=gate4[:, t, :], axis=mybir.AxisListType.X, op=ALU.add),
                                 reads=[("g4", t)], writes=[ssk])
                            S.op("dve", lambda e: e.reciprocal(out=ss[:], in_=ss[:]), reads=[ssk], writes=[ssk])
                            S.op("dve", lambda e: e.tensor_scalar(out=gate4[:, t, :], in0=gate4[:, t, :], scalar1=ss[:, 0:1], scalar2=None,
                                                                  op0=ALU.mult),
                                 reads=[("g4", t), ssk], writes=[("g4", t)])
                            S.op("dve", lambda e: e.tensor_scalar(out=maskb[:, t, :], in0=lgt, scalar1=m8[:, 3:4], scalar2=None, op0=ALU.is_ge),
                                 reads=[("lgs", t), ("m8", t)], writes=[("mask", t)])

                    norm_mod(b, l, 1, ph, router=router)
                    S.barrier()
                with contextlib.ExitStack() as ph:
                    al = lambda n, s, d: sb(n, s, d, ph)
                    tmpr = Rot(al, "q_tmp", 2, [128, NE], F32)
                    Sr = Rot(al, "q_S", 2, [128, NE], F32)
                    S.ops("pe", [mm(pb[0][:, 0:NE], onesb[:], maskb[:, t, :], t == 0, t == NT - 1) for t in range(NT)],
                          reads=[("mask", t) for t in range(NT)] + ["onesb"], writes=[PB[0]])
                    S.op("dve", lambda e: e.tensor_copy(out=cnt[:], in_=pb[0][:, 0:NE]), reads=[PB[0]], writes=["cnt"])
                    S.op("dve", lambda e: e.tensor_scalar(out=ntl[:], in0=cnt[:], scalar1=0.0, scalar2=None, op0=ALU.is_gt),
                         reads=["cnt"], writes=["ntl"])
                    for jj in range(1, SEQ // TS):
                        S.op("dve", lambda e, jj=jj: e.scalar_tensor_tensor(out=ntl[:], in0=cnt[:], scalar=float(TS * jj), in1=ntl[:],
                                                                            op0=ALU.is_gt, op1=ALU.add),
                             reads=["cnt", "ntl"], writes=["ntl"])
                    S.op("dve", lambda e: e.memset(onesf[:], 1.0), writes=["onesf"])
                    S.op("dve", lambda e: e.tensor_tensor_scan(out=incl[:], data0=onesf[:], data1=ntl[:], initial=0.0,
                                                               op0=ALU.mult, op1=ALU.add),
                         reads=["onesf", "ntl"], writes=["incl"])
                    S.op("dve", lambda e: e.tensor_tensor(out=pst[:], in0=incl[:], in1=ntl[:], op=ALU.subtract),
                         reads=["incl", "ntl"], writes=["pst"])
                    for i in range(NTILE):
                        tmp, tmpk = tmpr.get()
                        S.op("dve", lambda e, i=i, tmp=tmp: e.tensor_scalar(out=tmp[:], in0=incl[:], scalar1=float(i), scalar2=None,
                                                                            op0=ALU.is_le),
                             reads=["incl"], writes=[tmpk])
                        S.op("dve", lambda e, i=i, tmp=tmp: e.tensor_reduce(out=te[:, i:i + 1], in_=tmp[:], axis=mybir.AxisListType.X,
                                                                            op=ALU.add),
                             reads=[tmpk], writes=[("te", i)])
                    tek = [("te", i) for i in range(NTILE)]
                    S.op("dve", lambda e: e.tensor_scalar(out=tflag[:], in0=te[:], scalar1=float(NE), scalar2=bigcol[:, 0:1],
                                                          op0=ALU.is_ge, op1=ALU.mult),
                         reads=tek + ["bigcol"], writes=["tflag"])
                    S.op("dve", lambda e: e.tensor_scalar(out=te[:], in0=te[:], scalar1=float(NE - 1), scalar2=None, op0=ALU.min),
                         reads=tek, writes=["te_all"])
                    S.op("dve", lambda e: e.tensor_scalar(out=te128[:], in0=te[:], scalar1=128.0, scalar2=float(l * NE * 128), op0=ALU.mult, op1=ALU.add),
                         reads=["te_all"], writes=["te128"])
                    S.op("dve", lambda e: e.tensor_scalar(out=bidxf[:], in0=te128[:], scalar1=pcol[:, 0:1], scalar2=None, op0=ALU.add),
                         reads=["te128", "pcol"], writes=["bidxf"])
                    S.op("dve", lambda e: e.tensor_copy(out=bidxu[:], in_=bidxf[:]), reads=["bidxf"], writes=["bidxu"])
                    for h_ in range(2):
                        S.op("dve", lambda e, h_=h_: e.tensor_scalar(out=tflag[:], in0=bidxf[:], scalar1=2.0, scalar2=float(h_),
                                                                     op0=ALU.mult, op1=ALU.add),
                             reads=["bidxf", "tflag"], writes=["tflag"])
                        S.op("dve", lambda e, h_=h_: e.tensor_copy(out=hidxu[h_][:], in_=tflag[:]), reads=["tflag"], writes=[("hidxu", h_)])
                    S.op("dve", lambda e: e.tensor_scalar(out=bidxf[:], in0=te128[:], scalar1=1.0 / 128.0, scalar2=None, op0=ALU.mult),
                         reads=["te128", "bidxu"], writes=["bidxf"])
                    S.op("dve", lambda e: e.tensor_copy(out=teiu[:], in_=bidxf[:]), reads=["bidxf"], writes=["teiu"])
                    for c in range(8):
                        S.op("dve", lambda e, c=c: e.tensor_scalar(out=widxf[:, :, c], in0=te128[:], scalar1=8.0, scalar2=basepc[:, c:c + 1],
                                                                   op0=ALU.mult, op1=ALU.add),
                             reads=["te128", "basepc"], writes=[("widxf", c)])

                    S.op("dve", lambda e: e.tensor_copy(out=widxu[:], in_=widxf[:]), reads=[("widxf", c) for c in range(8)], writes=["widxu"])
                    for t in range(NT):
                        bank = pb[1 + t % 2]
                        fns = [mm(bank[:, 0:NE], onesb[:], maskb[:, tp, :], tp == 0, False) for tp in range(t)]
                        fns.append(mm(bank[:, 0:NE], utri[:], maskb[:, t, :], t == 0, True))
                        S.ops("pe", fns, reads=[("mask", tp) for tp in range(t + 1)] + ["onesb", "utri"], writes=[PB[1 + t % 2]])
                        Sv, Sk = Sr.get()
                        S.op("dve", lambda e, Sv=Sv, bank=bank: e.scalar_tensor_tensor(out=Sv[:], in0=pst[:], scalar=float(TS), in1=bank[:, 0:NE],
                                                                                       op0=ALU.mult, op1=ALU.add),
                             reads=["pst", PB[1 + t % 2]], writes=[Sk])
                        for k in range(4):
                            tmp, tmpk = tmpr.get()
                            S.op("dve", lambda e, t=t, k=k, tmp=tmp, Sv=Sv: e.scalar_tensor_tensor(
                                out=tmp[:], in0=lgs_all[:, t, :], scalar=m8_all[:, t, k:k + 1], in1=Sv[:], op0=ALU.is_equal, op1=ALU.mult),
                                reads=[("lgs", t), ("m8", t), Sk], writes=[tmpk])
                            S.op("dve", lambda e, t=t, k=k, tmp=tmp: e.tensor_reduce(out=slotf[:, 4 * t + k:4 * t + k + 1], in_=tmp[:],
                                                                                    axis=mybir.AxisListType.X, op=ALU.add),
                                 reads=[tmpk], writes=[("slotf", t, k)])
                    S.op("dve", lambda e: e.tensor_copy(out=slotu[:], in_=slotf[:]),
                         reads=[("slotf", t, k) for t in range(NT) for k in range(4)], writes=["slotu"])
                    S.barrier()
                ph_r.close()
                with contextlib.ExitStack() as ph:
                    al = lambda n, s, d: sb(n, s, d, ph)
                    htr = Rot(al, "h_tok", 2, [128, D], BF16)
                    hs_ev = {}
                    for t in range(NT):
                        ht, htk = htr.get()
                        bank = pb[t % 2]
                        bview = bank[:].bitcast(BF16)
                        fns = [(lambda e, c=c, bview=bview, t=t: e.transpose(out=bview[:, c * 128:(c + 1) * 128],
                                                                              in_=A[:, c, t * 128:(t + 1) * 128], identity=identb[:]))
                               for c in range(8)]
                        S.ops("pe", fns, reads=[("A", c, t // 4) for c in range(8)] + ["identb"], writes=[PB[t % 2]])
                        if t % 2 == 0:
                            S.op("dve", lambda e, ht=ht, bview=bview: e.tensor_copy(out=ht[:], in_=bview), reads=[PB[t % 2]], writes=[htk])
                        else:
                            S.op("act", lambda e, ht=ht, bview=bview: e.copy(out=ht[:], in_=bview), reads=[PB[t % 2]], writes=[htk])

                        def sc(e, ht=ht, t=t):
                            return [e.indirect_dma_start(out=hs_d[:, :], out_offset=bass.IndirectOffsetOnAxis(
                                ap=slotu[:, 4 * t + k:4 * t + k + 1], axis=0), in_=ht[:, :], in_offset=None) for k in range(4)]
                        hs_ev[htk] = S.dma("pool", sc, "sc_" + htk, reads=[htk, "slotu"], writes=[("HS", t)])
                    S.state["HS"] = {"w": list(hs_ev.values()), "r": []}
                    S.barrier()
                with contextlib.ExitStack() as ph:
                    al = lambda n, s, d: sb(n, s, d, ph)
                    wA0 = al("wA0", [128, 8, 2 * D], BF16)
                    wA1b = al("wA1b", [128, 4, 2 * D], BF16)
                    wAs = [[wA0[:, c, :] for c in range(8)],
                           [A[:, 4 + c, :] for c in range(4)] + [wA1b[:, c, :] for c in range(4)]]
                    wBl = [A[:, c // 2, (c % 2) * D:(c % 2 + 1) * D] for c in range(8)]
                    wBflat = A[:, 0:4, :].rearrange("p c n -> p (c n)")
                    wAh = [[wA0[:, 0:4, :].rearrange("p c n -> p (c n)"), wA0[:, 4:8, :].rearrange("p c n -> p (c n)")],
                           [A[:, 4:8, :].rearrange("p c n -> p (c n)"), wA1b[:, :, :].rearrange("p c n -> p (c n)")]]
                    hgr = Rot(al, "e_hg", 2, [128, 8, TS], BF16)
                    actr = Rot(al, "e_act", 1, [128, 8, TS], BF16)
                    glr = Rot(al, "e_gl", 2, [128, TS], F32)
                    tr = Rot(al, "e_t", 2, [128, TS], F32)
                    b1r = Rot(al, "e_b1", 2, [128, 16], F32)
                    b1pr = Rot(al, "e_b1p", 2, [128, 16], F32)
                    b2r = Rot(al, "e_b2", 2, [128, 8], F32)
                    hsts = [wslot[2][:].rearrange("p a b -> p (a b)").rearrange("p (s d) -> p s d", s=4)[:, 2 * q_:2 * q_ + 2, :]
                            for q_ in range(2)]

                    def hs_load(i_):
                        S.dma("sp", lambda e: e.dma_start(out=hsts[i_ % 2],
                                                          in_=hs_d[i_ * TS:(i_ + 1) * TS, :].rearrange("(s p) d -> p s d", p=128)),
                              "hsld%d" % (i_ % 2), reads=["HS"], writes=[("ws2", i_ % 2)])
                    hs_load(0)
                    b2bc = [wslot[0][:].rearrange("p a b -> p (a b)").bitcast(F32)[:, q_ * D:(q_ + 1) * D] for q_ in range(2)]
                    ytok = [wslot[1][:].rearrange("p a b -> p (a b)").bitcast(F32).rearrange("p (s d) -> p s d", s=2)[:, s, :]
                            for s in range(2)]
                    ys_ev = {}
                    cntp = 0
                    for i in range(NTILE):
                        par = i % 2
                        wA = wAs[par]

                        def gw1f(i_, dstl):
                            halves = wAh[0] if dstl is wAs[0] else wAh[1]

                            def f(e):
                                return [e.indirect_dma_start(out=halves[h_], out_offset=None, in_=w1rows,
                                                             in_offset=bass.IndirectOffsetOnAxis(ap=hidxu[h_][:, i_:i_ + 1], axis=0))
                                        for h_ in range(2)]
                            return f

                        def gbf(i_, b1t_, b2t_):
                            def f(e):
                                return [e.indirect_dma_start(out=b1t_[:, :], out_offset=None, in_=b1rows,
                                                             in_offset=bass.IndirectOffsetOnAxis(ap=bidxu[:, i_:i_ + 1], axis=0)),
                                        e.indirect_dma_start(out=b2bc[i_ % 2], out_offset=None, in_=b2f_d[:, :],
                                                             in_offset=bass.IndirectOffsetOnAxis(ap=teiu[:, i_:i_ + 1], axis=0))]
                            return f

                        if i == 0:
                            bias_buf = {}
                            b1t, b1k = b1r.get()
                            b2t, b2k = b2r.get()
                            S.dma("pool", gbf(0, b1t, b2t), "gb0", reads=["bidxu", "teiu"], writes=[b1k, ("ws0", 0)])
                            bias_buf[0] = (b1t, b1k, b2t, b2k)
                            S.dma("pool", gw1f(0, wAs[0]), "gwA0", reads=[("hidxu", 0), ("hidxu", 1), ("wb", l)], writes=[("wA", 0)])

                        def gw2(e, i=i):
                            return [e.indirect_dma_start(out=wBflat, out_offset=None, in_=w2rows,
                                                         in_offset=bass.IndirectOffsetOnAxis(ap=bidxu[:, i:i + 1], axis=0))]
                        S.dma("pool", gw2, "gwB", reads=["bidxu", ("wb", l)], writes=["wB"])
                        if i + 1 < NTILE:
                            nb1t, nb1k = b1r.get()
                            nb2t, nb2k = b2r.get()
                            S.dma("pool", gbf(i + 1, nb1t, nb2t), "gb%d" % ((i + 1) % 2), reads=["bidxu", "teiu"], writes=[nb1k, ("ws0", (i + 1) % 2)])
                            bias_buf[i + 1] = (nb1t, nb1k, nb2t, nb2k)
                            S.dma("pool", gw1f(i + 1, wAs[1 - par]), "gwA%d" % (1 - par), reads=[("hidxu", 0), ("hidxu", 1), ("wb", l)], writes=[("wA", 1 - par)])
                        b1t, b1k, b2t, b2k = bias_buf.pop(i)
                        b1p, b1pk = b1pr.get()
                        S.op("dve", lambda e, b1p=b1p, b1t=b1t: e.tensor_scalar(out=b1p[:], in0=b1t[:], scalar1=1.0, scalar2=None, op0=ALU.add),
                             reads=[b1k], writes=[b1pk])
                        hstok = hsts[par]
                        if i + 1 < NTILE:
                            hs_load(i + 1)
                        hg, hgk = hgr.get()
                        for half in range(2):
                            bank = pb[4 + half]
                            bview = bank[:].bitcast(BF16)
                            fns = [(lambda e, cc=cc, s=s, bview=bview, half=half: e.transpose(
                                out=bview[:, cc * TS + s * 128:cc * TS + (s + 1) * 128],
                                in_=hstok[:, s, (half * 4 + cc) * 128:(half * 4 + cc + 1) * 128], identity=identb[:]))
                                for cc in range(4) for s in range(2)]
                            S.ops("pe", fns, reads=[("ws2", par), "identb"], writes=[PB[4 + half]])
                            dst = hg[:, half * 4:half * 4 + 4, :]
                            src = bview.rearrange("p (c s) -> p c s", c=4)
                            if half == 0:
                                S.op("dve", lambda e, dst=dst, src=src: e.tensor_copy(out=dst, in_=src), reads=[PB[4 + half]], writes=[(hgk, half)])
                            else:
                                S.op("dve", lambda e, dst=dst, src=src: e.tensor_copy(out=dst, in_=src), reads=[PB[4 + half]], writes=[(hgk, half)])
                        act, actk = actr.get()
                        for nch in range(8):
                            bank = pb[cntp % 2]
                            bk = PB[cntp % 2]
                            cntp += 1
                            fns = [mm(bank[:, 0:TS], wA[k][:, nch * 128:(nch + 1) * 128], hg[:, k, :], k == 0, k == 7) for k in range(8)]
                            fns += [mm(bank[:, TS:2 * TS], wA[k][:, D + nch * 128:D + (nch + 1) * 128], hg[:, k, :], k == 0, k == 7) for k in range(8)]
                            S.ops("pe", fns, reads=[("wA", par), (hgk, 0), (hgk, 1)], writes=[bk])
                            gl, glk = glr.get()
                            tq, tk = tr.get()
                            S.op("dve", lambda e, gl=gl, bank=bank, b1t=b1t, nch=nch: e.tensor_scalar(
                                out=gl[:], in0=bank[:, 0:TS], scalar1=b1t[:, nch:nch + 1], scalar2=7.0, op0=ALU.add, op1=ALU.min),
                                reads=[bk, b1k], writes=[glk])
                            S.op("act", lambda e, gl=gl: e.activation(out=gl[:], in_=gl[:], func=AF.Silu, scale=1.702), reads=[glk], writes=[glk])
                            S.op("dve", lambda e, tq=tq, bank=bank, b1p=b1p, nch=nch: e.tensor_scalar(
                                out=tq[:], in0=bank[:, TS:2 * TS], scalar1=b1p[:, 8 + nch:9 + nch], scalar2=8.0, op0=ALU.add, op1=ALU.min),
                                reads=[bk, b1pk], writes=[tk])
                            S.op("dve", lambda e, tq=tq, gl=gl, act=act, nch=nch: e.scalar_tensor_tensor(
                                out=act[:, nch, :], in0=tq[:], scalar=-6.0, in1=gl[:], op0=ALU.max, op1=ALU.mult),
                                reads=[tk, glk], writes=[(actk, nch)])
                        for s in range(2):
                            for dh in range(2):
                                q_ = (2 * s + dh) % 2
                                bank = pb[2 + q_]
                                bk = PB[2 + q_]
                                S.ops("pe", [mm(bank[:], act[:, k, s * 128:(s + 1) * 128], wBl[k][:, dh * 512:(dh + 1) * 512], k == 0, k == 7)
                                             for k in range(8)],
                                      reads=["wB"] + [(actk, k) for k in range(8)], writes=[bk])
                                S.op("dve", lambda e, bank=bank, s=s, dh=dh: e.scalar_tensor_tensor(
                                    out=ytok[s][:, dh * 512:(dh + 1) * 512], in0=bank[:], scalar=1.0 / 1.702,
                                    in1=b2bc[par][:, dh * 512:(dh + 1) * 512], op0=ALU.mult, op1=ALU.add),
                                    reads=[bk, ("ws0", par)], writes=[("ws1", s, dh)])
                            row = i * TS + s * 128
                            ys_ev[s] = S.dma("sp", lambda e, s=s, row=row: e.dma_start(out=ys_d[row:row + 128, :], in_=ytok[s]),
                                             "yst%d" % s, reads=[("ws1", s, 0), ("ws1", s, 1)], writes=[("YS", i, s)])
                    S.state["YS"] = {"w": list(ys_ev.values()), "r": []}
                    S.barrier()
                    for kk in ("ws0", "ws1", "ws2"):
                        evs = []
                        for key, stt in S.state.items():
                            if isinstance(key, tuple) and key[0] == kk:
                                evs += stt["w"] + stt["r"]
                        base = S.state.setdefault(kk, {"w": [], "r": []})
                        base["r"] = base["r"] + evs
                with contextlib.ExitStack() as ph:
                    al = lambda n, s, d: sb(n, s, d, ph)
                    ykr = Rot(al, "c_yk", 8, [128, D], F32)
                    accr = Rot(al, "c_acc", 2, [128, D], F32)
                    gt2 = lambda cch: mod_ap(l, 5, cch, b)
                    for t in range(NT):
                        yks = []
                        for k in range(4):
                            yk, ykk = ykr.get()
                            S.dma("pool", lambda e, yk=yk, t=t, k=k: e.indirect_dma_start(
                                out=yk[:, :], out_offset=None, in_=ys_d[:, :],
                                in_offset=bass.IndirectOffsetOnAxis(ap=slotu[:, 4 * t + k:4 * t + k + 1], axis=0)),
                                "g_" + ykk, reads=["YS", "slotu"], writes=[ykk])
                            yks.append((yk, ykk))
                        acc, acck = accr.get()
                        S.op("dve", lambda e, acc=acc, yk=yks[0][0], t=t: e.tensor_scalar(
                            out=acc[:], in0=yk[:], scalar1=gate4[:, t, 0:1], scalar2=None, op0=ALU.mult),
                            reads=[yks[0][1], ("g4", t)], writes=[acck])
                        for k in range(1, 4):
                            S.op("dve", lambda e, acc=acc, yk=yks[k][0], t=t, k=k: e.scalar_tensor_tensor(
                                out=acc[:], in0=yk[:], scalar=gate4[:, t, k:k + 1], in1=acc[:], op0=ALU.mult, op1=ALU.add),
                                reads=[yks[k][1], ("g4", t), acck], writes=[acck])
                        for half in range(2):
                            bank = pb[(2 * t + half) % 4]
                            bk = PB[(2 * t + half) % 4]
                            fns = [(lambda e, cc=cc, bank=bank, acc=acc, half=half: e.transpose(
                                out=bank[:, cc * 128:(cc + 1) * 128], in_=acc[:, (half * 4 + cc) * 128:(half * 4 + cc + 1) * 128],
                                identity=ident[:])) for cc in range(4)]
                            S.ops("pe", fns, reads=[acck, "ident"], writes=[bk])
                            for cc in range(4):
                                cch = half * 4 + cc
                                S.op("dve", lambda e, bank=bank, cc=cc, cch=cch, t=t: e.scalar_tensor_tensor(
                                    out=xT[:, cch, t * 128:(t + 1) * 128], in0=bank[:, cc * 128:(cc + 1) * 128], scalar=gt2(cch),
                                    in1=xT[:, cch, t * 128:(t + 1) * 128], op0=ALU.mult, op1=ALU.add),
                                    reads=[bk, ("x", cch, t // 4), ("modT", l, 40 + cch)], writes=[("x", cch, t // 4)])
                    S.barrier()

        def cast_layer(l):
            for ex in range(NE):
                r0 = (l * NE + ex) * 128

                def f(e, ex=ex, r0=r0):
                    return [e.dma_start(out=w1b_d[r0:r0 + 128, :].rearrange("p (c n) -> p c n", c=8),
                                        in_=w1_d[l, ex].rearrange("(c p) n -> p c n", p=128)),
                            e.dma_start(out=w2b_d[r0:r0 + 128, :].rearrange("p (c n) -> p c n", c=8),
                                        in_=w2_d[l, ex].rearrange("(c p) n -> p c n", p=128))]
                S.dma("pool", f, "cast%d" % l, writes=[("wcast", l, ex)])
            S.state[("wb", l)] = {"w": [(S.dma_sem["cast%d" % l][0], S.dma_sem["cast%d" % l][1])], "r": []}

        cast_layer(0)
        for b in range(NB):
            load_x(b)
            done = (stop == ("load", 0))
            for l in range(NL):
                if done:
                    break
                mixer(b, l)
                if stop == ("mixer", l):
                    done = True
                    break
                moe(b, l)
                if b == 0 and l == 0 and NL > 1:
                    cast_layer(1)
                if stop == ("moe", l):
                    done = True
                    break
            if done:
                store_x(b, False)
                break
            store_x(b, stop is None)
        S.finish([("out", "f_o0"), ("out", "f_o1")])
        build.stats = dict(ticks=dict(S.tick), waits=S.n_wait, ops=S.n_ops, nsem=len(S.sems))
    return nc


def _rope_tables():
    freqs = (np.float32(10000.0) ** (-np.arange(16, dtype=np.float32) / np.float32(16))).astype(np.float32)
    tok = np.arange(SEQ)
    row = (tok // 64).astype(np.float32)
    col = (tok % 64).astype(np.float32)
    cosT = np.zeros((128, SEQ), np.float32)
    sinT = np.zeros((128, SEQ), np.float32)
    for p in range(128):
        d = p % 64
        ang = (row if d < 32 else col) * freqs[d % 16]
        cosT[p] = np.cos(ang.astype(np.float32))
        sinT[p] = np.sin(ang.astype(np.float32))
    pm = np.zeros((128, 128), np.float32)
    for m in range(128):
        i = m % 32
        if i < 16:
            pm[m + 16, m] = -1.0
        else:
            pm[m - 16, m] = 1.0
    return cosT, sinT, pm


def _prep_shared(inp):
    f = lambda a: np.ascontiguousarray(np.asarray(a, dtype=np.float32))
    w_in = f(inp["w_in"])
    q = w_in[:, :, 0:512]
    k0 = w_in[:, :, 512:576]
    k1 = w_in[:, :, 576:640]
    v = w_in[:, :, 640:768]
    ca = w_in[:, :, 768:1280]
    cg = w_in[:, :, 1280:1792]
    parts = [q, k0, k0, k1, k1, v]
    for c in range(4):
        parts += [ca[:, :, c * 128:(c + 1) * 128], cg[:, :, c * 128:(c + 1) * 128]]
    w_in_r = np.ascontiguousarray(np.concatenate(parts, axis=2))
    assert w_in_r.shape[2] == WIN

    def fm(a, nch):
        a = f(a)
        return np.ascontiguousarray(a.reshape(a.shape[0], nch, 128).transpose(0, 2, 1))

    cosT, sinT, pm = _rope_tables()
    sh = dict(
        w_mod=f(inp["w_mod"]), b_modT=fm(inp["b_mod"], 48), g_mixT=fm(inp["g_mix"], 8), g_ffnT=fm(inp["g_ffn"], 8),
        g_finT=fm(f(inp["g_final"])[None], 8)[0], w_in_r=w_in_r,
        gq2=np.ascontiguousarray(np.tile(f(inp["g_q"]), (1, 2))[:, :, None]),
        gk2=np.ascontiguousarray(np.tile(f(inp["g_k"]), (1, 2))[:, :, None]),
        wdwT=np.ascontiguousarray(f(inp["w_dw"]).reshape(2, 31, 4, 128).transpose(0, 3, 2, 1).reshape(2, 128, 124)),
        b_dwT=fm(inp["b_dw"], 4), g_cnT=fm(inp["g_cn"], 4), b_cnT=fm(inp["b_cn"], 4),
        w_out=f(inp["w_out"]), w_router=f(inp["w_router"]), b_router=f(inp["b_router"]),
        w1=f(inp["w1"]),
        b1R=np.ascontiguousarray(f(inp["b1"]).reshape(2, NE, 16, 128).transpose(0, 1, 3, 2).reshape(2, NE * 128, 16)),
        w2=f(inp["w2"]),
        b2flat=np.ascontiguousarray(f(inp["b2"]).reshape(2 * NE, D)),
        b2R=np.ascontiguousarray(f(inp["b2"]).reshape(2, NE, 8, 128).transpose(0, 1, 3, 2).reshape(2, NE * 128, 8)),
        cosT=cosT, sinT=sinT, pmat=pm,
    )
    return sh


def kernel(**inputs):
    n = 8
    NB = 4
    sh = _prep_shared(inputs)
    x = np.asarray(inputs["x"], dtype=np.float32)
    c = np.asarray(inputs["c"], dtype=np.float32)
    nc = build(NB=NB, NL=2)
    in_maps = []
    for i in range(n):
        m = dict(sh)
        m["x"] = np.ascontiguousarray(x[i * NB:(i + 1) * NB])
        cc = c[i * NB:(i + 1) * NB]
        m["cT"] = np.ascontiguousarray(cc.reshape(NB, 8, 128).transpose(2, 1, 0))
        in_maps.append(m)
    res = run_bass_kernel_spmd(nc, in_maps, core_ids=list(range(n)))
    return np.concatenate([np.asarray(r["out"]) for r in res.results], axis=0).astype(np.float32)
```
